# Optimizing a Trainium2 kernel written in Bass

```python
import math
import jax
import jax.numpy as jnp
from jax import lax
import numpy as np

D_MODEL = 1024
BATCH = 4
SEQ = 4096
DEPTH = 2

CTX_LEN = 256
GRID_W = 64

RW_HEADS = 16
RW_HEAD_DIM = 64
RW_WIDTH = RW_HEADS * RW_HEAD_DIM
RW_DECAY_LORA = 64
RW_AAA_LORA = 64
RW_GATE_LORA = 160
RW_GN_EPS = 64e-5

SSM_EXPAND = 2
SSM_INNER = SSM_EXPAND * D_MODEL
SSM_HEAD_DIM = 64
SSM_HEADS = SSM_INNER // SSM_HEAD_DIM
SSM_GROUPS = 8
SSM_HEADS_PER_GROUP = SSM_HEADS // SSM_GROUPS
SSM_STATE = 128
SSM_CONV = 5
SSM_CHUNK = 128
SSM_XBC = SSM_INNER + 2 * SSM_GROUPS * SSM_STATE
SSM_NORM_EPS = 1e-5

RW_SPLITS = (RW_WIDTH, RW_DECAY_LORA, RW_DECAY_LORA, RW_WIDTH, RW_WIDTH, RW_AAA_LORA, RW_AAA_LORA, RW_GATE_LORA)
RW_COLS = sum(RW_SPLITS)
SSM_SPLITS = (SSM_INNER, SSM_XBC, 2 * SSM_HEADS)
SSM_COLS = sum(SSM_SPLITS)
GATE_COLS = 2 * D_MODEL
IN_COLS = RW_COLS + SSM_COLS + GATE_COLS

N_EXPERTS = 32
N_EXPERT_GROUPS = 4
EXPERTS_PER_GROUP = N_EXPERTS // N_EXPERT_GROUPS
TOP_K = 2
D_EXPERT = 512
MOE_BLOCK = 128

NORM_EPS = 1e-6
F32 = jnp.float32

kernel_name = "hybrid_rwkv7_mamba2_moe_prefix_dit"


def rms_norm(x, gain, eps=NORM_EPS):
    xf = x.astype(F32)
    y = xf * lax.rsqrt(jnp.mean(xf * xf, axis=-1, keepdims=True) + eps)
    return (y * gain.astype(F32)).astype(x.dtype)


def split_cols(t, sizes):
    return jnp.split(t, np.cumsum(sizes)[:-1].tolist(), axis=-1)


def raster_to_colmajor(t, rows):
    b, n, ch = t.shape
    return t.reshape(b, rows, GRID_W, ch).transpose(0, 2, 1, 3).reshape(b, n, ch)


def colmajor_to_raster(t, rows):
    b, n, ch = t.shape
    return t.reshape(b, GRID_W, rows, ch).transpose(0, 2, 1, 3).reshape(b, n, ch)


def centred_token_shift(s, mu):
    pad = jnp.pad(s, ((0, 0), (1, 1), (0, 0)))
    return s + mu * (0.5 * (pad[:, :-2] + pad[:, 2:]) - s)


def wkv7_scan(r, w, k, v, a, b, s0, reverse, emit):
    def step(S, inp):
        r_t, w_t, k_t, v_t, a_t, b_t = inp
        sa = jnp.einsum("bhij,bhj->bhi", S, a_t)
        S = S * w_t[:, :, None, :] + sa[..., None] * b_t[:, :, None, :] + v_t[..., None] * k_t[:, :, None, :]
        y = jnp.einsum("bhij,bhj->bhi", S, r_t) if emit else None
        return S, y
    xs = tuple(jnp.moveaxis(t, 1, 0) for t in (r, w, k, v, a, b))
    S, ys = lax.scan(step, s0, xs, reverse=reverse)
    return S, (jnp.moveaxis(ys, 0, 1) if emit else None)


def rwkv7_branch(s_ctx, s_lat, shift_mu, w0, w2, a0, a2, g2, k_k, k_a, r_k, ln_w, ln_b, with_ctx_out):
    n_ctx = s_ctx.shape[1]
    s = jnp.concatenate([centred_token_shift(s_ctx, shift_mu), centred_token_shift(s_lat, shift_mu)], axis=1)
    r, w_f, w_b, k, v, a_f, a_b, g = split_cols(s, RW_SPLITS)
    bsz = s.shape[0]
    heads = lambda t: t.astype(F32).reshape(bsz, -1, RW_HEADS, RW_HEAD_DIM)
    kk = heads(k * k_k)
    kk = kk / jnp.maximum(jnp.linalg.norm(kk, axis=-1, keepdims=True), 1e-12)
    r_h, k_h, v_h = heads(r), heads(k), heads(v)
    ka_h = k_a.astype(F32).reshape(RW_HEADS, RW_HEAD_DIM)
    s0 = jnp.zeros((bsz, RW_HEADS, RW_HEAD_DIM, RW_HEAD_DIM), F32)
    y = 0.0
    for d, (w_lora, a_lora, reverse) in enumerate(((w_f, a_f, False), (w_b, a_b, True))):
        w_log = -jax.nn.softplus(-(w0[d] + jnp.tanh(w_lora) @ w2[d])) - 0.5
        decay = heads(jnp.exp(-jnp.exp(w_log.astype(F32))))
        a_rate = heads(jax.nn.sigmoid(a0[d] + a_lora @ a2[d]))
        k_d = k_h * (1.0 + (a_rate - 1.0) * ka_h)
        ins = (r_h, decay, k_d, v_h, -kk, kk * a_rate)
        s_c, y_c = wkv7_scan(*[t[:, :n_ctx] for t in ins], s0, reverse, with_ctx_out)
        _, y_l = wkv7_scan(*[t[:, n_ctx:] for t in ins], s_c, reverse, True)
        y = y + (jnp.concatenate([y_c, y_l], axis=1) if with_ctx_out else y_l)
    keep = slice(None) if with_ctx_out else slice(n_ctx, None)
    mean = jnp.mean(y, axis=-1, keepdims=True)
    var = jnp.mean(jnp.square(y - mean), axis=-1, keepdims=True)
    y = ((y - mean) * lax.rsqrt(var + RW_GN_EPS)).reshape(bsz, -1, RW_WIDTH) * ln_w + ln_b
    bonus = jnp.sum(r_h[:, keep] * k_h[:, keep] * r_k, axis=-1, keepdims=True) * v_h[:, keep]
    y = y + bonus.reshape(bsz, -1, RW_WIDTH)
    out_gate = jax.nn.sigmoid(g[:, keep]) @ g2
    return (y * out_gate).astype(s_lat.dtype)


def depthwise_conv_silu(t, w, b):
    half = SSM_CONV // 2
    y = lax.conv_general_dilated(t, w[:, None, :].astype(t.dtype), window_strides=(1,), padding=[(half, half)],
                                 dimension_numbers=("NWC", "WIO", "NWC"), feature_group_count=t.shape[-1])
    return jax.nn.silu(y + b)


def ssd_chunked(x, log_decay, b_in, c_in, s0, emit):
    bsz, n_tok, G, E, P = x.shape
    N = b_in.shape[-1]
    nc, L = n_tok // SSM_CHUNK, SSM_CHUNK
    x = x.reshape(bsz, nc, L, G, E, P)
    b_in = b_in.reshape(bsz, nc, L, G, N)
    c_in = c_in.reshape(bsz, nc, L, G, N)
    cum = jnp.cumsum(log_decay.reshape(bsz, nc, L, G, E).transpose(0, 3, 4, 1, 2), axis=-1)
    decay_to_end = jnp.exp(cum[..., -1:] - cum)
    chunk_states = jnp.einsum("bclgn,bgecl,bclgep->bcgepn", b_in, decay_to_end, x)
    chunk_decay = jnp.exp(cum[..., -1])

    def step(S, inp):
        st, dc = inp
        return S * dc[..., None, None] + st, S

    s_final, s_prev = lax.scan(step, s0, (jnp.moveaxis(chunk_states, 1, 0), jnp.moveaxis(chunk_decay, 3, 0)))
    if not emit:
        return s_final, None
    s_prev = jnp.moveaxis(s_prev, 0, 1)
    lower = jnp.tril(jnp.ones((L, L), dtype=bool))
    seg = cum[..., :, None] - cum[..., None, :]
    lmat = jnp.exp(jnp.where(lower, seg, -jnp.inf))
    cb = jnp.einsum("bclgn,bcsgn->bgcls", c_in, b_in)
    y_diag = jnp.einsum("bgecls,bcsgep->bclgep", cb[:, :, None] * lmat, x)
    y_off = jnp.einsum("bclgn,bcgepn,bgecl->bclgep", c_in, s_prev, jnp.exp(cum))
    return s_final, (y_diag + y_off).reshape(bsz, n_tok, G, E, P)


def ssd_scan(x, log_decay, b_in, c_in, s0, reverse, emit):
    if reverse:
        x, log_decay, b_in, c_in = (jnp.flip(t, axis=1) for t in (x, log_decay, b_in, c_in))
    s_final, y = ssd_chunked(x, log_decay, b_in, c_in, s0, emit)
    if reverse and emit:
        y = jnp.flip(y, axis=1)
    return s_final, y


def ssm_branch(s_ctx, s_lat, rows, conv_w, conv_b, dt_bias, a_log, d_skip, norm_w, with_ctx_out):
    n_ctx = s_ctx.shape[1]
    out_dtype = s_lat.dtype
    s_lat = raster_to_colmajor(s_lat, rows)
    z_c, xbc_c, dt_c = split_cols(s_ctx, SSM_SPLITS)
    z_l, xbc_l, dt_l = split_cols(s_lat, SSM_SPLITS)
    xbc = jnp.concatenate([depthwise_conv_silu(xbc_c, conv_w, conv_b), depthwise_conv_silu(xbc_l, conv_w, conv_b)], axis=1)
    dt_raw = jnp.concatenate([dt_c, dt_l], axis=1).astype(F32)
    z = jnp.concatenate([z_c, z_l], axis=1) if with_ctx_out else z_l
    xs, b_in, c_in = split_cols(xbc.astype(F32), (SSM_INNER, SSM_GROUPS * SSM_STATE, SSM_GROUPS * SSM_STATE))
    bsz, n_tok = xs.shape[:2]
    G, E = SSM_GROUPS, SSM_HEADS_PER_GROUP
    xs = xs.reshape(bsz, n_tok, G, E, SSM_HEAD_DIM)
    b_in = b_in.reshape(bsz, n_tok, G, SSM_STATE)
    c_in = c_in.reshape(bsz, n_tok, G, SSM_STATE)
    dt_raw = dt_raw.reshape(bsz, n_tok, 2, SSM_HEADS)
    s0 = jnp.zeros((bsz, G, E, SSM_HEAD_DIM, SSM_STATE), F32)
    y = 0.0
    for d in range(2):
        reverse = d == 1
        dt = jax.nn.softplus(dt_raw[:, :, d] + dt_bias[d].astype(F32)).reshape(bsz, n_tok, G, E)
        log_decay = dt * (-jnp.exp(a_log[d].astype(F32))).reshape(G, E)
        x_dt = xs * dt[..., None]
        s_c, y_c = ssd_scan(x_dt[:, :n_ctx], log_decay[:, :n_ctx], b_in[:, :n_ctx], c_in[:, :n_ctx], s0, reverse, with_ctx_out)
        _, y_l = ssd_scan(x_dt[:, n_ctx:], log_decay[:, n_ctx:], b_in[:, n_ctx:], c_in[:, n_ctx:], s_c, reverse, True)
        y = y + (jnp.concatenate([y_c, y_l], axis=1) if with_ctx_out else y_l)
    keep = slice(None) if with_ctx_out else slice(n_ctx, None)
    y = y + (d_skip[0] + d_skip[1]).astype(F32).reshape(G, E)[..., None] * xs[:, keep]
    y = y.reshape(bsz, -1, SSM_INNER) * jax.nn.silu(z.astype(F32))
    yg = y.reshape(bsz, -1, SSM_GROUPS, SSM_INNER // SSM_GROUPS)
    yg = yg * lax.rsqrt(jnp.mean(yg * yg, axis=-1, keepdims=True) + SSM_NORM_EPS)
    y = (yg.reshape(bsz, -1, SSM_INNER) * norm_w).astype(out_dtype)
    if with_ctx_out:
        return jnp.concatenate([y[:, :n_ctx], colmajor_to_raster(y[:, n_ctx:], rows)], axis=1)
    return colmajor_to_raster(y, rows)


def mixing_block(h_ctx, h_lat, rows, with_ctx_out, w_in, rw_shift_mu, rw_w0, rw_w2, rw_a0, rw_a2, rw_g2,
                 rw_k_k, rw_k_a, rw_r_k, rw_ln_w, rw_ln_b, ssm_conv_w, ssm_conv_b, ssm_dt_bias, ssm_a_log,
                 ssm_d, ssm_norm_w, w_branch_rw, w_branch_ssm, w_out):
    p_ctx = h_ctx @ w_in
    p_lat = h_lat @ w_in
    rw_c, ssm_c, gate_c = split_cols(p_ctx, (RW_COLS, SSM_COLS, GATE_COLS))
    rw_l, ssm_l, gate_l = split_cols(p_lat, (RW_COLS, SSM_COLS, GATE_COLS))
    y_rw = rwkv7_branch(rw_c, rw_l, rw_shift_mu, rw_w0, rw_w2, rw_a0, rw_a2, rw_g2, rw_k_k, rw_k_a,
                        rw_r_k, rw_ln_w, rw_ln_b, with_ctx_out)
    y_ssm = ssm_branch(ssm_c, ssm_l, rows, ssm_conv_w, ssm_conv_b, ssm_dt_bias, ssm_a_log, ssm_d,
                       ssm_norm_w, with_ctx_out)
    gates = jnp.concatenate([gate_c, gate_l], axis=1) if with_ctx_out else gate_l
    g_rw, g_ssm = split_cols(gates, (D_MODEL, D_MODEL))
    merged = jax.nn.sigmoid(g_rw) * (y_rw @ w_branch_rw) + jax.nn.sigmoid(g_ssm) * (y_ssm @ w_branch_ssm)
    return merged @ w_out


def moe_ffn(h, w_router, b_router, w1, w3, w2):
    n_tok, d = h.shape
    scores = jax.nn.sigmoid(h.astype(F32) @ w_router.astype(F32))
    biased = (scores + b_router.astype(F32)).reshape(n_tok, N_EXPERT_GROUPS, EXPERTS_PER_GROUP)
    group_score = lax.top_k(biased, 2)[0].sum(-1)
    top_group = jnp.argmax(group_score, axis=-1)
    in_group = jnp.take_along_axis(biased, top_group[:, None, None], axis=1)[:, 0]
    _, local = lax.top_k(in_group, TOP_K)
    expert = top_group[:, None] * EXPERTS_PER_GROUP + local
    gate = jnp.take_along_axis(scores, expert, axis=-1)
    gate = gate / jnp.sum(gate, axis=-1, keepdims=True)
    n_assign = n_tok * TOP_K
    flat_e = expert.reshape(-1).astype(jnp.int32)
    order = jnp.argsort(flat_e)
    sorted_e = flat_e[order]
    counts = jnp.bincount(flat_e, length=N_EXPERTS)
    padded = (counts + MOE_BLOCK - 1) // MOE_BLOCK * MOE_BLOCK
    pad_start = jnp.cumsum(padded) - padded
    start = jnp.cumsum(counts) - counts
    dest_sorted = (pad_start[sorted_e] + jnp.arange(n_assign) - start[sorted_e]).astype(jnp.int32)
    dest = jnp.zeros((n_assign,), jnp.int32).at[order].set(dest_sorted)
    n_blocks = -(-n_assign // MOE_BLOCK) + N_EXPERTS
    slot_token = jnp.full((n_blocks * MOE_BLOCK,), n_tok, jnp.int32).at[dest].set(
        jnp.arange(n_assign, dtype=jnp.int32) // TOP_K)
    block_expert = jnp.minimum(jnp.searchsorted(jnp.cumsum(padded) // MOE_BLOCK, jnp.arange(n_blocks), side="right"),
                               N_EXPERTS - 1)
    h_pad = jnp.concatenate([h, jnp.zeros((1, d), h.dtype)], axis=0)
    xb = h_pad[slot_token].reshape(n_blocks, MOE_BLOCK, d)

    def expert_block(args):
        xblk, e = args
        return (jax.nn.silu(xblk @ w1[e]) * (xblk @ w3[e])) @ w2[e]

    yb = lax.map(expert_block, (xb, block_expert)).reshape(-1, d)
    y = yb[dest].reshape(n_tok, TOP_K, d)
    return jnp.einsum("tkd,tk->td", y, gate.astype(y.dtype))


def setup_inputs(seed: int = 0) -> dict:
    key = jax.random.key(seed)
    ks = iter(jax.random.split(key, 48))
    nrm = lambda shape, scale: scale * jax.random.normal(next(ks), shape, F32)
    unif = lambda shape, lo, hi: jax.random.uniform(next(ks), shape, F32, lo, hi)
    dt0 = jnp.exp(unif((DEPTH, 2, SSM_HEADS), math.log(1e-3), math.log(1e-1)))
    return {
        "x": nrm((BATCH, SEQ, D_MODEL), 1.0),
        "c": nrm((BATCH, D_MODEL), 1.0),
        "ctx": nrm((BATCH, CTX_LEN, D_MODEL), 1.0),
        "c_ctx": nrm((D_MODEL,), 1.0),
        "w_mod": nrm((DEPTH, D_MODEL, 6 * D_MODEL), 0.5 * D_MODEL ** -0.5),
        "b_mod": nrm((DEPTH, 6 * D_MODEL), 0.01),
        "norm_mix_g": 1.0 + nrm((DEPTH, D_MODEL), 0.05),
        "w_in": nrm((DEPTH, D_MODEL, IN_COLS), D_MODEL ** -0.5),
        "rw_shift_mu": unif((DEPTH, RW_COLS), 0.0, 1.0),
        "rw_w0": unif((DEPTH, 2, RW_WIDTH), -5.0, -0.5),
        "rw_w2": nrm((DEPTH, 2, RW_DECAY_LORA, RW_WIDTH), 0.1),
        "rw_a0": nrm((DEPTH, 2, RW_WIDTH), 0.1),
        "rw_a2": nrm((DEPTH, 2, RW_AAA_LORA, RW_WIDTH), 0.05),
        "rw_g2": nrm((DEPTH, RW_GATE_LORA, RW_WIDTH), RW_GATE_LORA ** -0.5),
        "rw_k_k": 0.85 + nrm((DEPTH, RW_WIDTH), 0.05),
        "rw_k_a": 1.0 + nrm((DEPTH, RW_WIDTH), 0.05),
        "rw_r_k": nrm((DEPTH, RW_HEADS, RW_HEAD_DIM), 0.1),
        "rw_ln_w": 1.0 + nrm((DEPTH, RW_WIDTH), 0.05),
        "rw_ln_b": nrm((DEPTH, RW_WIDTH), 0.01),
        "ssm_conv_w": nrm((DEPTH, SSM_CONV, SSM_XBC), SSM_CONV ** -0.5),
        "ssm_conv_b": nrm((DEPTH, SSM_XBC), 0.01),
        "ssm_dt_bias": dt0 + jnp.log(-jnp.expm1(-dt0)),
        "ssm_a_log": jnp.log(unif((DEPTH, 2, SSM_HEADS), 1.0, 16.0)),
        "ssm_d": 1.0 + nrm((DEPTH, 2, SSM_HEADS), 0.1),
        "ssm_norm_w": 1.0 + nrm((DEPTH, SSM_INNER), 0.05),
        "w_branch_rw": nrm((DEPTH, RW_WIDTH, D_MODEL), RW_WIDTH ** -0.5),
        "w_branch_ssm": nrm((DEPTH, SSM_INNER, D_MODEL), SSM_INNER ** -0.5),
        "w_out": nrm((DEPTH, D_MODEL, D_MODEL), D_MODEL ** -0.5),
        "norm_ffn_g": 1.0 + nrm((DEPTH, D_MODEL), 0.05),
        "w_router": nrm((D_MODEL, N_EXPERTS), D_MODEL ** -0.5),
        "b_router": nrm((N_EXPERTS,), 0.01),
        "exp_w1": nrm((DEPTH, N_EXPERTS, D_MODEL, D_EXPERT), D_MODEL ** -0.5),
        "exp_w3": nrm((DEPTH, N_EXPERTS, D_MODEL, D_EXPERT), D_MODEL ** -0.5),
        "exp_w2": nrm((DEPTH, N_EXPERTS, D_EXPERT, D_MODEL), D_EXPERT ** -0.5),
        "norm_final_g": 1.0 + nrm((D_MODEL,), 0.05),
    }


def reference(x, c, ctx, c_ctx, w_mod, b_mod, norm_mix_g, w_in, rw_shift_mu, rw_w0, rw_w2, rw_a0, rw_a2,
              rw_g2, rw_k_k, rw_k_a, rw_r_k, rw_ln_w, rw_ln_b, ssm_conv_w, ssm_conv_b, ssm_dt_bias, ssm_a_log,
              ssm_d, ssm_norm_w, w_branch_rw, w_branch_ssm, w_out, norm_ffn_g, w_router, b_router,
              exp_w1, exp_w3, exp_w2, norm_final_g):
    bsz, n_lat, d = x.shape
    rows = n_lat // GRID_W
    n_ctx = ctx.shape[1]
    silu_c = jax.nn.silu(c)
    silu_cc = jax.nn.silu(c_ctx)
    for l in range(DEPTH):
        last = l == DEPTH - 1
        shift_m, scale_m, gate_m, shift_f, scale_f, gate_f = split_cols((silu_c @ w_mod[l] + b_mod[l])[:, None, :], (D_MODEL,) * 6)
        cshift_m, cscale_m, cgate_m, cshift_f, cscale_f, cgate_f = split_cols(silu_cc @ w_mod[l] + b_mod[l], (D_MODEL,) * 6)
        h_lat = rms_norm(x, norm_mix_g[l]) * (1 + scale_m) + shift_m
        h_ctx = rms_norm(ctx, norm_mix_g[l]) * (1 + cscale_m) + cshift_m
        mix = mixing_block(h_ctx, h_lat, rows, not last, w_in[l], rw_shift_mu[l], rw_w0[l], rw_w2[l], rw_a0[l],
                           rw_a2[l], rw_g2[l], rw_k_k[l], rw_k_a[l], rw_r_k[l], rw_ln_w[l], rw_ln_b[l],
                           ssm_conv_w[l], ssm_conv_b[l], ssm_dt_bias[l], ssm_a_log[l], ssm_d[l], ssm_norm_w[l],
                           w_branch_rw[l], w_branch_ssm[l], w_out[l])
        if last:
            x = x + gate_m * mix
            h_lat = rms_norm(x, norm_ffn_g[l]) * (1 + scale_f) + shift_f
            f_lat = moe_ffn(h_lat.reshape(-1, d), w_router, b_router, exp_w1[l], exp_w3[l], exp_w2[l])
            x = x + gate_f * f_lat.reshape(x.shape)
        else:
            x = x + gate_m * mix[:, n_ctx:]
            ctx = ctx + cgate_m * mix[:, :n_ctx]
            h_lat = rms_norm(x, norm_ffn_g[l]) * (1 + scale_f) + shift_f
            h_ctx = rms_norm(ctx, norm_ffn_g[l]) * (1 + cscale_f) + cshift_f
            tok = jnp.concatenate([h_ctx.reshape(-1, d), h_lat.reshape(-1, d)], axis=0)
            f = moe_ffn(tok, w_router, b_router, exp_w1[l], exp_w3[l], exp_w2[l])
            ctx = ctx + cgate_f * f[: bsz * n_ctx].reshape(ctx.shape)
            x = x + gate_f * f[bsz * n_ctx:].reshape(x.shape)
    return rms_norm(x, norm_final_g)
```

```python
import numpy as np
import concourse.bass as bass
import concourse.mybir as mybir

F32 = mybir.dt.float32
BF16 = mybir.dt.bfloat16
I32 = mybir.dt.int32
AF = mybir.ActivationFunctionType
ALU = mybir.AluOpType
AX = mybir.AxisListType

N_DMA_SEMS = 6
N_ENG_SEMS = 8
SAME_ENGINE_SYNC = True


class Trk:
    __slots__ = ("w", "r")

    def __init__(self):
        self.w = None
        self.r = []


class V:
    __slots__ = ("t", "ap")

    def __init__(self, t, ap):
        self.t = t
        self.ap = ap

    def __getitem__(self, key):
        return V(self.t, self.ap[key])

    def re(self, s, **kw):
        return V(self.t, self.ap.rearrange(s, **kw))

    def bc(self, shape):
        return V(self.t, self.ap.to_broadcast(shape))

    def bitcast(self, dt):
        return V(self.t, self.ap.bitcast(dt))


class Fw:
    ENGS = ("pe", "act", "dve", "pool", "sp")

    def __init__(self, nc):
        self.nc = nc
        self.handles = {"pe": nc.tensor, "act": nc.scalar, "dve": nc.vector, "pool": nc.gpsimd, "sp": nc.sync}
        self.ops = {e: [] for e in self.ENGS}
        self.esem = {e: [nc.alloc_semaphore("s_%s%d" % (e, i)) for i in range(N_ENG_SEMS)] for e in self.ENGS}
        self.dsems = {}
        self.dcount = {}
        self.dnext = {}
        for q in ("sp", "pool", "act"):
            self.dsems[q] = [nc.alloc_semaphore("d_%s%d" % (q, i)) for i in range(N_DMA_SEMS)]
            self.dcount[q] = [0] * N_DMA_SEMS
            self.dnext[q] = 0
        self.n_alloc = 0
        self.stacks = []
        self.names = {}
        self.out_events = []

    def sb(self, name, shape, dtype=F32):
        self.n_alloc += 1
        nm = "%s_%d" % (name, self.n_alloc)
        self.names[name] = nm
        if self.stacks:
            h = self.stacks[-1].enter_context(self.nc.sbuf_tensor(nm, list(shape), dtype))
        else:
            h = self.nc.alloc_sbuf_tensor(nm, list(shape), dtype)
        return V(Trk(), h[tuple(slice(None) for _ in shape)])

    def scope(self):
        fw = self

        class _S:
            def __enter__(s):
                import contextlib
                fw.stacks.append(contextlib.ExitStack())
                return s

            def __exit__(s, *a):
                fw.barrier()
                fw.stacks.pop().close()
                return False
        return _S()

    def barrier(self):
        last = {}
        for e in self.ENGS:
            last[e] = -1
            for i in range(len(self.ops[e]) - 1, -1, -1):
                o = self.ops[e][i]
                if o["dma"] is None and o["fn"] is not None:
                    last[e] = i
                    break
        for e in self.ENGS:
            waits = {}
            for e2 in self.ENGS:
                if (e2 != e or (SAME_ENGINE_SYNC and e != 'pe')) and last[e2] >= 0:
                    waits[("e", e2)] = last[e2]
            for q in self.dsems:
                for si in range(N_DMA_SEMS):
                    if self.dcount[q][si] > 0:
                        waits[("d", q, si)] = self.dcount[q][si]
            self.ops[e].append({"waits": waits, "fn": None, "dma": None})

    def ps(self, name, shape, dtype=F32):
        self.n_alloc += 1
        h = self.nc.alloc_psum_tensor("%s_%d" % (name, self.n_alloc), list(shape), dtype)
        return V(Trk(), h[tuple(slice(None) for _ in shape)])

    def dram(self, name, shape, dtype=F32, kind="Internal"):
        h = self.nc.dram_tensor(name, list(shape), dtype, kind=kind)
        return V(Trk(), h.ap())

    def _deps(self, eng, reads, writes):
        deps = []
        for v in reads:
            if v.t.w is not None:
                deps.append(v.t.w)
        for v in writes:
            if v.t.w is not None:
                deps.append(v.t.w)
            deps.extend(v.t.r)
        out = {}
        for ev in deps:
            kind = ev[0]
            if kind == "e":
                _, e, idx = ev
                if e == eng and (not SAME_ENGINE_SYNC or e == "pe"):
                    continue
                k = ("e", e)
                out[k] = max(out.get(k, -1), idx)
            else:
                _, q, si, val = ev
                k = ("d", q, si)
                out[k] = max(out.get(k, -1), val)
        return out

    def _mark(self, ev, reads, writes):
        for v in reads:
            v.t.r.append(ev)
        for v in writes:
            v.t.w = ev
            v.t.r = []

    def op(self, eng, fn, outs, ins):
        outs = [o for o in outs if o is not None]
        ins = [i for i in ins if isinstance(i, V)]
        waits = self._deps(eng, ins, outs)
        idx = len(self.ops[eng])
        self.ops[eng].append({"waits": waits, "fn": fn, "dma": None})
        self._mark(("e", eng, idx), ins, outs)

    def dma(self, q, out, in_, **kw):
        waits = self._deps(q, [in_], [out])
        si = self.dnext[q]
        self.dnext[q] = (si + 1) % N_DMA_SEMS
        if self.dcount[q][si] > 0:
            k = ("d", q, si)
            waits[k] = max(waits.get(k, -1), self.dcount[q][si])
        self.dcount[q][si] += 1
        val = self.dcount[q][si]
        o_ap, i_ap = out.ap, in_.ap
        self.ops[q].append({"waits": waits, "fn": lambda h: h.dma_start(out=o_ap, in_=i_ap, **kw), "dma": (si, val)})
        ev = ("d", q, si, val)
        self._mark(ev, [in_], [out])
        return ev

    def finish(self):
        nc = self.nc
        need = {e: set() for e in self.ENGS}
        for e in self.ENGS:
            for o in self.ops[e]:
                for k, v in o["waits"].items():
                    if k[0] == "e":
                        need[k[1]].add(v)
        rank = {}
        for e in self.ENGS:
            r = 0
            for i in range(len(self.ops[e])):
                if i in need[e]:
                    rank[(e, i)] = (r % N_ENG_SEMS, r // N_ENG_SEMS + 1)
                    r += 1
        final_waits = []
        for q in self.dsems:
            for si in range(N_DMA_SEMS):
                if self.dcount[q][si] > 0:
                    final_waits.append((self.dsems[q][si], 16 * self.dcount[q][si]))
        ops, esem, dsems, handles = self.ops, self.esem, self.dsems, self.handles

        def replay(e, h):
            seen = {}
            for i, o in enumerate(ops[e]):
                for k, v in o["waits"].items():
                    if k[0] == "e":
                        si_, val = rank[(k[1], v)]
                        sem = esem[k[1]][si_]
                        k = ("e", k[1], si_)
                    else:
                        sem, val = dsems[k[1]][k[2]], 16 * v
                    if seen.get(k, -1) >= val:
                        continue
                    seen[k] = val
                    h.wait_ge(sem, val)
                if o["fn"] is None:
                    continue
                ins = o["fn"](h)
                if o["dma"] is not None:
                    ins.then_inc(dsems[e][o["dma"][0]], 16)
                elif (e, i) in rank:
                    ins.then_inc(esem[e][rank[(e, i)][0]], 1)

        with nc.Block() as block:
            @block.tensor
            def _(h):
                replay("pe", h)

            @block.scalar
            def _(h):
                replay("act", h)

            @block.vector
            def _(h):
                replay("dve", h)

            @block.gpsimd
            def _(h):
                replay("pool", h)

            @block.sync
            def _(h):
                replay("sp", h)
                for sem, val in final_waits:
                    h.wait_ge(sem, val)

    def mm(self, out, lhsT, rhs, start=True, stop=True):
        o, l, r = out.ap, lhsT.ap, rhs.ap
        self.op("pe", lambda h: h.matmul(o, l, r, start=start, stop=stop), [out], [lhsT, rhs] + ([] if start else [out]))

    def tr(self, out, in_, ident):
        o, i, d = out.ap, in_.ap, ident.ap
        self.op("pe", lambda h: h.transpose(o, i, d), [out], [in_, ident])

    def act(self, out, in_, func, bias=None, scale=None, accum=None, eng="act"):
        kw = {}
        if bias is not None:
            kw["bias"] = bias.ap if isinstance(bias, V) else bias
        if scale is not None:
            kw["scale"] = scale.ap if isinstance(scale, V) else scale
        if accum is not None:
            kw["accum_out"] = accum.ap
        o, i = out.ap, in_.ap
        self.op("act", lambda h: h.activation(o, i, func, **kw), [out, accum], [in_, bias, scale])

    def tt(self, out, a, b, op, eng="dve"):
        o, x, y = out.ap, a.ap, b.ap
        self.op(eng, lambda h: h.tensor_tensor(o, x, y, op), [out], [a, b])

    def ts(self, out, a, s1, op0, s2=None, op1=None, accum=None, eng="dve"):
        o, x = out.ap, a.ap
        c1 = s1.ap if isinstance(s1, V) else s1
        c2 = s2.ap if isinstance(s2, V) else s2
        kw = {}
        if op1 is not None:
            kw["op1"] = op1
        if accum is not None:
            kw["accum_out"] = accum.ap
        if op1 is None and accum is None:
            self.op(eng, lambda h: h.tensor_single_scalar(o, x, c1, op0), [out], [a, s1])
        else:
            self.op(eng, lambda h: h.tensor_scalar(o, x, c1, c2, op0, **kw), [out, accum], [a, s1, s2])

    def stt(self, out, a, s, b, op0, op1, eng="dve"):
        o, x, y = out.ap, a.ap, b.ap
        c = s.ap if isinstance(s, V) else s
        self.op(eng, lambda h: h.scalar_tensor_tensor(o, x, c, y, op0, op1), [out], [a, s, b])

    def copy(self, out, in_, eng="dve"):
        o, i = out.ap, in_.ap
        if eng == "act":
            self.op("act", lambda h: h.activation(o, i, AF.Identity), [out], [in_])
        else:
            self.op(eng, lambda h: h.tensor_copy(o, i), [out], [in_])

    def memset(self, out, val, eng="dve"):
        o = out.ap
        self.op(eng, lambda h: h.memset(o, val), [out], [])

    def reduce(self, out, in_, op, axis=AX.X, eng="dve"):
        o, i = out.ap, in_.ap
        self.op(eng, lambda h: h.tensor_reduce(o, i, axis, op), [out], [in_])

    def recip(self, out, in_):
        o, i = out.ap, in_.ap
        self.op("dve", lambda h: h.reciprocal(o, i), [out], [in_])

    def max8(self, out, in_):
        o, i = out.ap, in_.ap
        self.op("dve", lambda h: h.max(o, i), [out], [in_])

    def load(self, out, in_, q="sp", **kw):
        return self.dma(q, out, in_, **kw)

    def store(self, out, in_, q="pool", **kw):
        return self.dma(q, out, in_, **kw)


D = 1024
RW_COLS = 3488
SSM_COLS = 6208
IN_COLS = 11744
NCONST = 18
GRID_W = 64


def make_consts():
    c = np.zeros((128, NCONST, 128), np.float32)
    i = np.arange(128)
    s, t = i[:, None], i[None, :]
    c[:, 0] = np.eye(128)
    c[:, 1] = 1.0
    c[:, 2] = (s <= t)
    c[:, 3] = (s < t)
    c[:, 4] = (s >= t)
    c[:, 5] = (s > t)
    c[:, 6] = np.where(t >= s, 0.0, -1e30)
    c[:, 7] = np.where(t <= s, 0.0, -1e30)
    low = (s > t)
    c[:, 8] = low & (s // 8 == t // 8)
    for k in range(1, 5):
        bs = 8 << k
        c[:, 8 + k] = low & (s // bs == t // bs) & (s // (bs // 2) != t // (bs // 2))
    for k in range(5):
        c[:, 13 + k] = c[:, 8 + k].T
    return c


class KB:
    def __init__(self, n_ctx, n_lat, depth=2, stages="all"):
        self.n_ctx, self.n_lat, self.depth = n_ctx, n_lat, depth
        self.T = n_ctx + n_lat
        self.rows = n_lat // GRID_W
        self.NT = self.T // 128
        self.stages = stages
        nc = bass.Bass("TRN2", target_bir_lowering=False)
        self.nc = nc
        f = self.f = Fw(nc)
        T = self.T
        inp = lambda name, shape: f.dram(name, shape, F32, kind="ExternalInput")
        self.x = inp("x", [n_lat, D])
        self.ctx = inp("ctx", [n_ctx, D])
        self.c2 = inp("c2", [2, D])
        self.consts = inp("consts", [128, NCONST, 128])
        L = depth
        self.w_mod = inp("w_mod", [L, D, 6 * D])
        self.b_mod = inp("b_mod", [L, 6 * D])
        self.norm_mix_g = inp("norm_mix_g", [L, D])
        self.w_in = inp("w_in", [L, D, IN_COLS])
        self.rw_shift_mu = inp("rw_shift_mu", [L, RW_COLS])
        self.rw_w0 = inp("rw_w0", [L, 2, 1024])
        self.rw_w2 = inp("rw_w2", [L, 2, 64, 1024])
        self.rw_a0 = inp("rw_a0", [L, 2, 1024])
        self.rw_a2 = inp("rw_a2", [L, 2, 64, 1024])
        self.rw_g2 = inp("rw_g2", [L, 160, 1024])
        self.rw_k_k = inp("rw_k_k", [L, 1024])
        self.rw_k_a = inp("rw_k_a", [L, 1024])
        self.rw_r_k = inp("rw_r_k", [L, 1024])
        self.rw_ln_w = inp("rw_ln_w", [L, 1024])
        self.rw_ln_b = inp("rw_ln_b", [L, 1024])
        self.ssm_conv_w = inp("ssm_conv_w", [L, 5, 4096])
        self.ssm_conv_b = inp("ssm_conv_b", [L, 4096])
        self.ssm_dt_bias = inp("ssm_dt_bias", [L, 64])
        self.ssm_a_log = inp("ssm_a_log", [L, 64])
        self.ssm_d = inp("ssm_d", [L, 64])
        self.ssm_norm_w = inp("ssm_norm_w", [L, 2048])
        self.w_branch_rw = inp("w_branch_rw", [L, 1024, D])
        self.w_branch_ssm = inp("w_branch_ssm", [L, 2048, D])
        self.w_out = inp("w_out", [L, D, D])
        self.norm_ffn_g = inp("norm_ffn_g", [L, D])
        self.w_router = inp("w_router", [D, 32])
        self.b_router = inp("b_router", [32])
        self.exp_w1 = inp("exp_w1", [L, 32, D, 512])
        self.exp_w3 = inp("exp_w3", [L, 32, D, 512])
        self.exp_w2 = inp("exp_w2", [L, 32, 512, D])
        self.norm_final_g = inp("norm_final_g", [D])
        self.out = f.dram("out", [n_lat, D], F32, kind="ExternalOutput")
        self.XS = f.dram("XS", [T, D])
        self.MODS = f.dram("MODS", [2, 6, D])
        self.PRW = f.dram("PRW", [T + 6, RW_COLS])
        self.GS = f.dram("GS", [T, 2048], BF16)
        self.YF = f.dram("YF", [T, 1024])
        self.YRW = f.dram("YRW", [T, 1024], BF16, kind=("ExternalOutput" if stages == "debug" else "Internal"))
        self.cst = f.sb("cst", [128, NCONST, 128])
        self.cstb = f.sb("cstb", [128, NCONST, 128], BF16)
        self.banks = [f.ps("bank%d" % i, [128, 512]) for i in range(8)]
        self.bi = 0
        f.load(self.cst, self.consts)
        f.copy(self.cstb, self.cst)
        self.ident = self.cst[:, 0, :]
        self.identb = self.cstb[:, 0, :]
        self.ones = self.cst[:, 1, :]

    def bank(self):
        b = self.banks[self.bi]
        self.bi = (self.bi + 1) % 8
        return b

    def bcast_load(self, tile, row_ap, n):
        self.f.load(tile, V(row_ap.t, row_ap.ap.partition_broadcast(128)))

    def pad_row(self, t):
        return t + 1 if t < self.n_ctx else t + 3

    def stage_init(self):
        f = self.f
        f.store(self.XS[0:self.n_ctx, :], self.ctx, q="sp")
        f.store(self.XS[self.n_ctx:self.T, :], self.x, q="sp")

    def stage_mod(self, l):
        f = self.f
        with f.scope():
            cT = f.sb("cT", [128, 2, 8])
            scT = f.sb("scT", [128, 2, 8])
            with self.nc.allow_non_contiguous_dma("tiny transposed load"):
                f.load(cT, self.c2.re("r (k p) -> p r k", p=128), allow_slow_non_contiguous=True)
            f.act(scT, cT, AF.Silu)
            rows = [f.sb("mrow%d" % r, [1, 6 * D]) for r in range(2)]
            bm = f.sb("bm", [1, 6 * D])
            f.load(bm, self.b_mod[l:l + 1, :])
            g2 = f.sb("g2", [1, 2, D])
            f.load(g2[:, 0, :], self.norm_mix_g[l:l + 1, :])
            f.load(g2[:, 1, :], self.norm_ffn_g[l:l + 1, :])
            wts = [f.sb("wm%d" % i, [128, 8, 512]) for i in range(2)]
            for cb in range(12):
                wt = wts[cb % 2]
                f.load(wt, self.w_mod[l, :, cb * 512:(cb + 1) * 512].re("(k p) n -> p k n", p=128))
                for r in range(2):
                    pb = self.bank()
                    for k in range(8):
                        f.mm(pb[0:1, :], scT[:, r, k:k + 1], wt[:, k, :], start=(k == 0), stop=(k == 7))
                    f.tt(rows[r][:, cb * 512:(cb + 1) * 512], pb[0:1, :], bm[:, cb * 512:(cb + 1) * 512], ALU.add)
            for r in range(2):
                row = rows[r]
                o = f.sb("orow%d" % r, [1, 6, D])
                f.stt(o[:, 0, :], row[:, D:2 * D], 1.0, g2[:, 0, :], ALU.add, ALU.mult)
                f.copy(o[:, 1, :], row[:, 0:D])
                f.copy(o[:, 2, :], row[:, 2 * D:3 * D])
                f.stt(o[:, 3, :], row[:, 4 * D:5 * D], 1.0, g2[:, 1, :], ALU.add, ALU.mult)
                f.copy(o[:, 4, :], row[:, 3 * D:4 * D])
                f.copy(o[:, 5, :], row[:, 5 * D:6 * D])
                f.store(self.MODS[r:r + 1, :, :], o)

    def emit_h(self, xt, G, S, hT_dst, hf_dst=None):
        f = self.f
        sq, ss, hh = self.h_sq, self.h_ss, self.h_h
        f.act(sq, xt, AF.Square, accum=ss)
        f.ts(ss, ss, 1.0 / D, ALU.mult, 1e-6, ALU.add)
        f.act(ss, ss, AF.Sqrt)
        f.recip(ss, ss)
        f.stt(hh, xt, ss[:, 0:1], G, ALU.mult, ALU.mult)
        f.tt(hh, hh, S, ALU.add)
        for half in range(2):
            pb = self.bank()
            for k in range(4):
                kk = half * 4 + k
                f.tr(pb[:, k * 128:(k + 1) * 128], hh[:, kk * 128:(kk + 1) * 128], self.ident)
            if hf_dst is not None:
                f.copy(hf_dst[:, half * 4:(half + 1) * 4, :], pb.re("p (k n) -> p k n", k=4), eng="dve")
                f.copy(hT_dst[:, half * 4:(half + 1) * 4, :], hf_dst[:, half * 4:(half + 1) * 4, :], eng="act")
            else:
                f.copy(hT_dst[:, half * 4:(half + 1) * 4, :], pb.re("p (k n) -> p k n", k=4), eng="act")

    def alloc_h_tmps(self):
        f = self.f
        self.h_ss = f.sb("h_ss", [128, 1])
        self.h_h = f.sb("h_h", [128, D])
        self.h_sq = self.h_h

    def load_mod_bcast(self, idxs):
        f = self.f
        out = {}
        for r in range(2):
            for i in idxs:
                t = f.sb("mb%d_%d" % (r, i), [128, D])
                self.bcast_load(t, self.MODS[r, i, :], D)
                out[(r, i)] = t
        return out

    def stage_P(self, l):
        f = self.f
        T, NT = self.T, self.NT
        with f.scope():
            self.alloc_h_tmps()
            mb = self.load_mod_bcast([0, 1])
            hT = [f.sb("hT%d" % i, [128, 8, 128], BF16) for i in range(NT)]
            xts = [f.sb("xt%d" % i, [128, D]) for i in range(2)]
            for i in range(NT):
                xt = xts[i % 2]
                f.load(xt, self.XS[i * 128:(i + 1) * 128, :])
                r = 1 if i * 128 < self.n_ctx else 0
                self.emit_h(xt, mb[(r, 0)], mb[(r, 1)], hT[i])
            z = f.sb("zrow", [1, RW_COLS])
            f.memset(z, 0.0)
            for prow in (0, self.n_ctx + 1, self.n_ctx + 2, T + 3):
                f.store(self.PRW[prow:prow + 1, :], z)
            blocks = [(c0, min(512, RW_COLS - c0), "rw") for c0 in range(0, RW_COLS, 512)]
            blocks += [(RW_COLS + SSM_COLS + c0, 512, "gate") for c0 in range(0, 2048, 512)]
            wfs = [f.sb("wf%d" % i, [128, 8, 512]) for i in range(2)]
            wbs = [f.sb("wb%d" % i, [128, 8, 512], BF16) for i in range(2)]
            o32 = [f.sb("o32_%d" % i, [128, 512]) for i in range(2)]
            o16 = [f.sb("o16_%d" % i, [128, 512], BF16) for i in range(2)]
            for bi_, (c0, nb, kind) in enumerate(blocks):
                wf, wb = wfs[bi_ % 2], wbs[bi_ % 2]
                f.load(wf[:, :, 0:nb], self.w_in[l, :, c0:c0 + nb].re("(k p) n -> p k n", p=128))
                f.copy(wb[:, :, 0:nb], wf[:, :, 0:nb], eng="pool")
                for i in range(NT):
                    pb = self.bank()
                    for k in range(8):
                        f.mm(pb[:, 0:nb], hT[i][:, k, :], wb[:, k, 0:nb], start=(k == 0), stop=(k == 7))
                    if kind == "rw":
                        o = o32[i % 2]
                        f.copy(o[:, 0:nb], pb[:, 0:nb], eng="act")
                        pr = self.pad_row(i * 128)
                        f.store(self.PRW[pr:pr + 128, c0:c0 + nb], o[:, 0:nb])
                    else:
                        o = o16[i % 2]
                        f.act(o, pb, AF.Sigmoid)
                        g0 = c0 - RW_COLS - SSM_COLS
                        f.store(self.GS[i * 128:(i + 1) * 128, g0:g0 + 512], o)

    def finish(self):
        self.f.finish()
        return self.nc


def _stage_R(self, l):
    f = self.f
    NT = self.NT
    nctx_t = self.n_ctx // 128
    with f.scope():
        P = {}
        def bc(name, row_ap, n):
            t = f.sb(name, [128, n])
            self.bcast_load(t, row_ap, n)
            return t
        P["MU"] = bc("MU", self.rw_shift_mu[l, :], RW_COLS)
        P["KK"] = bc("KKp", self.rw_k_k[l, :], 1024)
        P["KA"] = bc("KAp", self.rw_k_a[l, :], 1024)
        P["RK"] = bc("RKp", self.rw_r_k[l, :], 1024)
        P["LNW"] = bc("LNW", self.rw_ln_w[l, :], 1024)
        P["LNB"] = bc("LNB", self.rw_ln_b[l, :], 1024)
        g2b = f.sb("g2b", [128, 2, 1024], BF16)
        with f.scope():
            g2f = f.sb("g2f", [128, 2, 1024])
            f.load(g2f[:, 0, :], self.rw_g2[l, 0:128, :])
            f.load(g2f[0:32, 1, :], self.rw_g2[l, 128:160, :])
            f.copy(g2b[:, 0, :], g2f[:, 0, :])
            f.copy(g2b[0:32, 1, :], g2f[0:32, 1, :])
        P["g2b"] = g2b
        W = self.rw_alloc_work()
        for d in range(2):
          with f.scope():
            P["W0"] = bc("W0p%d" % d, self.rw_w0[l, d, :], 1024)
            P["A0"] = bc("A0p%d" % d, self.rw_a0[l, d, :], 1024)
            lb = f.sb("lb%d" % d, [64, 2, 1024], BF16)
            with f.scope():
                lf = f.sb("lf%d" % d, [64, 2, 1024])
                f.load(lf[:, 0, :], self.rw_w2[l, d, :, :])
                f.load(lf[:, 1, :], self.rw_a2[l, d, :, :])
                f.copy(lb, lf)
            P["w2b"], P["a2b"] = lb[:, 0, :], lb[:, 1, :]
            H32 = [f.sb("H32_%d_%d" % (d, g), [64, 4, 64]) for g in range(4)]
            H16 = [f.sb("H16_%d_%d" % (d, g), [64, 4, 64], BF16) for g in range(4)]
            for g in range(4):
                f.memset(H32[g], 0.0)
                f.memset(H16[g], 0.0, eng="pool")
            ctx_chunks = list(range(nctx_t))
            lat_chunks = list(range(nctx_t, NT))
            order = ctx_chunks + lat_chunks if d == 0 else ctx_chunks[::-1] + lat_chunks[::-1]
            for c in order:
                self.rw_chunk(l, c, d, P, W, H32, H16)


def _rw_alloc_work(self):
    f = self.f
    W = {}
    for n in ("cur", "prev", "nxt"):
        W[n] = f.sb("rw_" + n, [128, RW_COLS])
    for n in ("kk", "t1", "t2", "ar", "logw", "E", "y"):
        W[n] = f.sb("rw_" + n, [128, 1024])
    for n in ("Rt", "Kt", "Bt", "At", "V16"):
        W[n] = f.sb("rw_" + n, [128, 1024], BF16)
    for n in ("RT", "KT", "BT", "AT"):
        W[n] = f.sb("rw_" + n, [64, 16, 128], BF16)
    W["s16"] = f.sb("rw_s16", [128, 16])
    W["s16b"] = f.sb("rw_s16b", [128, 16])
    W["tw"] = f.sb("rw_tw", [128, 2, 64])
    W["lT"] = f.sb("rw_lT", [64, 2, 128], BF16)
    W["gC"] = f.sb("rw_gC", [64, 16])
    for n in ("OA", "ON"):
        W[n] = [f.sb("rw_%s%d" % (n, i), [128, 4, 128], BF16) for i in range(5)]
    for n in ("D2s", "DT2s", "E1", "ET1", "E2", "ET2", "E4", "ET4", "Q", "QT", "P1", "P1T"):
        W[n] = f.sb("rw_" + n, [128, 4, 128], BF16)
    for n in ("X", "XT"):
        W[n] = [f.sb("rw_%s%d" % (n, i), [128, 4, 128], BF16) for i in range(2)]
    for n in ("AkT", "RbT", "RkT"):
        W[n] = f.sb("rw_" + n, [128, 4, 128], BF16)
    W["U16"] = [f.sb("rw_U16_%d" % i, [128, 4, 64], BF16) for i in range(2)]
    W["sg"] = f.sb("rw_sg", [128, 160])
    W["sgT"] = f.sb("rw_sgT", [128, 2, 128], BF16)
    W["yo"] = f.sb("rw_yo", [128, 1024], BF16)
    return W


def _rw_chunk(self, l, c, d, P, W, H32, H16):
    f = self.f
    cst, cstb = self.cst, self.cstb
    pr = self.pad_row(c * 128)
    cur, prev, nxt = W["cur"], W["prev"], W["nxt"]
    f.load(cur, self.PRW[pr:pr + 128, :])
    f.load(prev, self.PRW[pr - 1:pr + 127, :])
    f.load(nxt, self.PRW[pr + 1:pr + 129, :])
    f.tt(prev, prev, nxt, ALU.add)
    f.stt(prev, prev, 0.5, cur, ALU.mult, ALU.subtract)
    f.tt(prev, prev, P["MU"], ALU.mult)
    f.tt(cur, cur, prev, ALU.add)
    s = cur
    r, k, v = s[:, 0:1024], s[:, 1152:2176], s[:, 2176:3200]
    wlo = s[:, 1024 + d * 64:1024 + (d + 1) * 64]
    alo = s[:, 3200 + d * 64:3200 + (d + 1) * 64]
    kk, t1, t2, ar, logw, E = W["kk"], W["t1"], W["t2"], W["ar"], W["logw"], W["E"]
    kd = t1
    s16, s16b = W["s16"], W["s16b"]
    h3 = lambda t: t.re("p (h n) -> p h n", h=16)
    f.tt(kk, k, P["KK"], ALU.mult)
    f.tt(t1, kk, kk, ALU.mult)
    f.reduce(s16, h3(t1), ALU.add)
    f.act(s16, s16, AF.Sqrt)
    f.ts(s16, s16, 1e-12, ALU.max)
    f.recip(s16, s16)
    f.tt(h3(kk), h3(kk), s16.re("p (h o) -> p h o", o=1).bc([128, 16, 64]), ALU.mult)
    tw, lT = W["tw"], W["lT"]
    f.act(tw[:, 0, :], wlo, AF.Tanh)
    f.copy(tw[:, 1, :], alo)
    pb = self.bank()
    for i in range(2):
        f.tr(pb[0:64, i * 128:(i + 1) * 128], tw[:, i, :], self.ident)
    f.copy(lT, pb[0:64, 0:256].re("p (i n) -> p i n", i=2), eng="act")
    for half in range(2):
        pb = self.bank()
        f.mm(pb, lT[:, 0, :], P["w2b"][:, half * 512:(half + 1) * 512])
        f.tt(t1[:, half * 512:(half + 1) * 512], pb, P["W0"][:, half * 512:(half + 1) * 512], ALU.add)
    f.act(t1, t1, AF.Sigmoid)
    f.ts(logw, t1, -0.6065306597126334, ALU.mult)
    for half in range(2):
        pb = self.bank()
        f.mm(pb, lT[:, 1, :], P["a2b"][:, half * 512:(half + 1) * 512])
        f.tt(ar[:, half * 512:(half + 1) * 512], pb, P["A0"][:, half * 512:(half + 1) * 512], ALU.add)
    f.act(ar, ar, AF.Sigmoid)
    f.stt(t2, ar, -1.0, P["KA"], ALU.add, ALU.mult)
    f.stt(kd, t2, 1.0, k, ALU.add, ALU.mult)
    incl = cst[:, 2 if d == 0 else 4, :]
    strict = cst[:, 3 if d == 0 else 5, :]
    Rt, Kt, Bt, At, V16 = W["Rt"], W["Kt"], W["Bt"], W["At"], W["V16"]
    cb = [self.bank(), self.bank()]
    for half in range(2):
        f.mm(cb[half], incl, logw[:, half * 512:(half + 1) * 512])
    for half in range(2):
        hs = slice(half * 512, (half + 1) * 512)
        f.act(E[:, hs], cb[half], AF.Exp)
        f.tt(Rt[:, hs], r[:, hs], E[:, hs], ALU.mult)
    for half in range(2):
        hs = slice(half * 512, (half + 1) * 512)
        f.act(E[:, hs], cb[half], AF.Exp, scale=-1.0)
    f.tt(Kt, kd, E, ALU.mult)
    f.tt(t2, kk, ar, ALU.mult)
    f.tt(Bt, t2, E, ALU.mult)
    cb = [self.bank(), self.bank()]
    for half in range(2):
        f.mm(cb[half], strict, logw[:, half * 512:(half + 1) * 512])
    for half in range(2):
        hs = slice(half * 512, (half + 1) * 512)
        f.act(E[:, hs], cb[half], AF.Exp)
    f.stt(At, kk, -1.0, E, ALU.mult, ALU.mult)
    f.copy(V16, v, eng="pool")
    gC = W["gC"]
    pb = self.bank()
    for h in range(16):
        f.mm(pb[0:64, h:h + 1], logw[:, h * 64:(h + 1) * 64], self.ones[:, 0:1])
    f.act(gC, pb[0:64, 0:16], AF.Exp)
    for src, dst in ((Rt, W["RT"]), (Kt, W["KT"]), (Bt, W["BT"]), (At, W["AT"])):
        for half in range(2):
            pbb = self.bank().bitcast(BF16)
            for hh in range(8):
                h = half * 8 + hh
                f.tr(pbb[0:64, hh * 128:(hh + 1) * 128], src[:, h * 64:(h + 1) * 64], self.identb)
            f.copy(dst[:, half * 8:(half + 1) * 8, :], pbb[0:64, :].re("p (h n) -> p h n", h=8), eng="act")
    RT, KT, BT, AT = W["RT"], W["KT"], W["BT"], W["AT"]
    m_strict_st = cst[:, 3 if d == 0 else 5, :]
    m_strict_ts = cst[:, 5 if d == 0 else 3, :]
    m_incl_st = cst[:, 2 if d == 0 else 4, :]
    y = W["y"]
    bc4 = lambda m: m.re("p (o n) -> p o n", o=1).bc([128, 4, 128])
    b4 = lambda b: b.re("p (h n) -> p h n", h=4)
    for g in range(4):
        heads = [g * 4 + hh for hh in range(4)]
        mA = [cst[:, (8 if d == 0 else 13) + q, :] for q in range(5)]
        mN = [cst[:, (13 if d == 0 else 8) + q, :] for q in range(5)]
        idb4 = bc4(self.identb)

        def mm4(pb, L_, R_, start=True, stop=True):
            for hh in range(4):
                f.mm(pb[:, hh * 128:(hh + 1) * 128], L_[:, hh, :], R_[:, hh, :], start=start, stop=stop)

        def mm4I(pb, L_, R_, R2_):
            for hh in range(4):
                f.mm(pb[:, hh * 128:(hh + 1) * 128], L_[:, hh, :], R_[:, hh, :], start=True, stop=False)
                f.mm(pb[:, hh * 128:(hh + 1) * 128], self.identb, R2_[:, hh, :], start=False, stop=True)

        pa, pn = self.bank(), self.bank()
        for hh, h in enumerate(heads):
            f.mm(pa[:, hh * 128:(hh + 1) * 128], AT[:, h, :], BT[:, h, :])
            f.mm(pn[:, hh * 128:(hh + 1) * 128], BT[:, h, :], AT[:, h, :])
        for lv in range(5):
            f.tt(W["OA"][lv], b4(pa), bc4(mA[lv]), ALU.mult)
            f.tt(W["ON"][lv], b4(pn), bc4(mN[lv]), ALU.mult)
        for (dst, L_, R_, msk) in ((W["AkT"], KT, AT, m_strict_st), (W["RbT"], BT, RT, m_incl_st),
                                   (W["RkT"], KT, RT, m_incl_st)):
            pb = self.bank()
            for hh, h in enumerate(heads):
                f.mm(pb[:, hh * 128:(hh + 1) * 128], L_[:, h, :], R_[:, h, :])
            f.tt(dst, b4(pb), bc4(msk), ALU.mult)
        Dm, DTm = W["OA"][0], W["ON"][0]
        f.tt(W["E1"], Dm, idb4, ALU.add, eng="pool")
        f.tt(W["ET1"], DTm, idb4, ALU.add, eng="pool")
        p1, p2 = self.bank(), self.bank()
        mm4(p1, DTm, Dm)
        mm4(p2, Dm, DTm)
        f.copy(W["D2s"], b4(p1), eng="act")
        f.copy(W["DT2s"], b4(p2), eng="act")
        f.tt(W["E2"], b4(p1), idb4, ALU.add)
        f.tt(W["ET2"], b4(p2), idb4, ALU.add)
        p1, p2 = self.bank(), self.bank()
        mm4(p1, W["DT2s"], W["D2s"])
        mm4(p2, W["D2s"], W["DT2s"])
        f.tt(W["E4"], b4(p1), idb4, ALU.add)
        f.tt(W["ET4"], b4(p2), idb4, ALU.add)
        p1, p2 = self.bank(), self.bank()
        mm4(p1, W["ET2"], W["E4"])
        mm4(p2, W["E2"], W["ET4"])
        f.copy(W["Q"], b4(p1), eng="act")
        f.copy(W["QT"], b4(p2), eng="dve")
        p1, p2 = self.bank(), self.bank()
        mm4(p1, W["ET1"], W["Q"])
        mm4(p2, W["E1"], W["QT"])
        xi = 0
        X, XT = W["X"][0], W["XT"][0]
        f.copy(X, b4(p1), eng="act")
        f.copy(XT, b4(p2), eng="dve")
        for lv in range(1, 5):
            O, OT = W["OA"][lv], W["ON"][lv]
            p1 = self.bank()
            mm4(p1, OT, X)
            f.copy(W["P1"], b4(p1), eng="act")
            Xn, XTn = W["X"][xi ^ 1], W["XT"][xi ^ 1]
            if lv < 4:
                p2 = self.bank()
                mm4(p2, X, OT)
                f.copy(W["P1T"], b4(p2), eng="dve")
                p3 = self.bank()
                mm4I(p3, XT, W["P1"], X)
                f.copy(Xn, b4(p3), eng="act")
                p4 = self.bank()
                mm4I(p4, W["P1"], XT, XT)
                f.copy(XTn, b4(p4), eng="dve")
            else:
                p4 = self.bank()
                mm4I(p4, W["P1"], XT, XT)
                f.copy(XTn, b4(p4), eng="dve")
            xi ^= 1
            X, XT = Xn, XTn
        pb = self.bank()
        for hh, h in enumerate(heads):
            f.mm(pb[:, hh * 64:(hh + 1) * 64], AT[:, h, :], H16[g][:, hh, :], start=True, stop=False)
            f.mm(pb[:, hh * 64:(hh + 1) * 64], W["AkT"][:, hh, :], V16[:, h * 64:(h + 1) * 64], start=False, stop=True)
        f.copy(W["U16"][0], pb[:, 0:256].re("p (h n) -> p h n", h=4), eng="act")
        pb = self.bank()
        for hh in range(4):
            f.mm(pb[:, hh * 64:(hh + 1) * 64], XT[:, hh, :], W["U16"][0][:, hh, :])
        U = W["U16"][1]
        f.copy(U, pb[:, 0:256].re("p (h n) -> p h n", h=4), eng="act")
        pb = self.bank()
        for hh, h in enumerate(heads):
            f.mm(pb[:, hh * 64:(hh + 1) * 64], RT[:, h, :], H16[g][:, hh, :], start=True, stop=False)
            f.mm(pb[:, hh * 64:(hh + 1) * 64], W["RbT"][:, hh, :], U[:, hh, :], start=False, stop=False)
            f.mm(pb[:, hh * 64:(hh + 1) * 64], W["RkT"][:, hh, :], V16[:, h * 64:(h + 1) * 64], start=False, stop=True)
        f.copy(y[:, g * 256:(g + 1) * 256], pb[:, 0:256], eng="dve")
        pb = self.bank()
        for hh, h in enumerate(heads):
            f.mm(pb[0:64, hh * 64:(hh + 1) * 64], Bt[:, h * 64:(h + 1) * 64], U[:, hh, :], start=True, stop=False)
            f.mm(pb[0:64, hh * 64:(hh + 1) * 64], Kt[:, h * 64:(h + 1) * 64], V16[:, h * 64:(h + 1) * 64], start=False, stop=True)
        f.tt(H32[g], H32[g], pb[0:64, 0:256].re("p (h n) -> p h n", h=4), ALU.add)
        f.tt(H32[g], H32[g], gC[:, g * 4:(g + 1) * 4].re("p (h o) -> p h o", o=1).bc([64, 4, 64]), ALU.mult)
        f.copy(H16[g], H32[g], eng="pool")
    if d == 0:
        f.store(self.YF[c * 128:(c + 1) * 128, :], y)
        return
    yf = t1
    f.load(yf, self.YF[c * 128:(c + 1) * 128, :])
    f.tt(y, y, yf, ALU.add)
    f.reduce(s16, h3(y), ALU.add)
    f.ts(s16, s16, 1.0 / 64, ALU.mult)
    f.tt(h3(y), h3(y), s16.re("p (h o) -> p h o", o=1).bc([128, 16, 64]), ALU.subtract)
    f.tt(t2, y, y, ALU.mult)
    f.reduce(s16b, h3(t2), ALU.add)
    f.ts(s16b, s16b, 1.0 / 64, ALU.mult, 64e-5, ALU.add)
    f.act(s16b, s16b, AF.Sqrt)
    f.recip(s16b, s16b)
    f.tt(h3(y), h3(y), s16b.re("p (h o) -> p h o", o=1).bc([128, 16, 64]), ALU.mult)
    f.tt(y, y, P["LNW"], ALU.mult)
    f.tt(y, y, P["LNB"], ALU.add)
    f.tt(t2, r, k, ALU.mult)
    f.tt(t2, t2, P["RK"], ALU.mult)
    f.reduce(s16, h3(t2), ALU.add)
    f.tt(h3(t2), h3(v), s16.re("p (h o) -> p h o", o=1).bc([128, 16, 64]), ALU.mult)
    f.tt(y, y, t2, ALU.add)
    sg, sgT = W["sg"], W["sgT"]
    f.act(sg, s[:, 3328:3488], AF.Sigmoid)
    pb = self.bank()
    f.tr(pb[:, 0:128], sg[:, 0:128], self.ident)
    f.tr(pb[0:32, 128:256], sg[:, 128:160], self.ident)
    f.copy(sgT[:, 0, :], pb[:, 0:128], eng="act")
    f.copy(sgT[0:32, 1, :], pb[0:32, 128:256], eng="act")
    for half in range(2):
        hs = slice(half * 512, (half + 1) * 512)
        pb = self.bank()
        f.mm(pb, sgT[:, 0, :], P["g2b"][:, 0, hs], start=True, stop=False)
        f.mm(pb, sgT[0:32, 1, :], P["g2b"][0:32, 1, hs], start=False, stop=True)
        f.tt(W["yo"][:, hs], y[:, hs], pb, ALU.mult)
    f.store(self.YRW[c * 128:(c + 1) * 128, :], W["yo"])


KB.stage_R = _stage_R
KB.rw_alloc_work = _rw_alloc_work
KB.rw_chunk = _rw_chunk


def _stage_final(self):
    f = self.f
    with f.scope():
        G = f.sb("fin_g", [128, D])
        self.bcast_load(G, self.norm_final_g, D)
        xts = [f.sb("fin_x%d" % i, [128, D]) for i in range(2)]
        sqs = f.sb("fin_sq", [128, D])
        ss = [f.sb("fin_ss%d" % i, [128, 1]) for i in range(2)]
        for i in range(self.n_lat // 128):
            xt, s1 = xts[i % 2], ss[i % 2]
            t0 = self.n_ctx + i * 128
            f.load(xt, self.XS[t0:t0 + 128, :])
            f.act(sqs, xt, AF.Square, accum=s1)
            f.ts(s1, s1, 1.0 / D, ALU.mult, 1e-6, ALU.add)
            f.act(s1, s1, AF.Sqrt)
            f.recip(s1, s1)
            f.stt(xt, xt, s1[:, 0:1], G, ALU.mult, ALU.mult)
            f.store(self.out[i * 128:(i + 1) * 128, :], xt)


KB.stage_final = _stage_final


ZOFF = RW_COLS
XOFF = RW_COLS + 2048
DOFF = RW_COLS + 2048 + 4096


def _ssm_rows(self, i):
    nctx_t = self.n_ctx // 128
    if i < nctx_t:
        return [(0, 128, i * 128, 1)]
    q = i - nctx_t
    rows = self.rows
    cpc = 128 // rows
    return [(ci * rows, rows, self.n_ctx + (q * cpc + ci), GRID_W) for ci in range(cpc)]


def _ssm_dram_rows(self, dram, i, cols=slice(None)):
    out = []
    for (p0, n, r0, stride) in self.ssm_rows(i):
        if stride == 1:
            out.append((slice(p0, p0 + n), dram[r0:r0 + n, cols]))
        else:
            lat = dram[self.n_ctx:self.T, cols].re("(r c) d -> c r d", c=GRID_W)
            out.append((slice(p0, p0 + n), lat[r0 - self.n_ctx]))
    return out


def _stage_S1(self, l):
    f = self.f
    T, NT, n_ctx = self.T, self.NT, self.n_ctx
    if not hasattr(self, "ZS"):
        self.ZS = f.dram("ZS", [T, 2048], BF16)
        self.DTA = f.dram("DTA", [T, 64])
        self.XSM = f.dram("XSM", [T, 2048], BF16)
        self.BTM = f.dram("BTM", [T, 1024], BF16)
        self.BCT = f.dram("BCT", [16, 128, T], BF16)
        self.YSF = f.dram("YSF", [T, 2048])
        self.YSSM = f.dram("YSSM", [T, 2048], BF16)
    groups = []
    nctx_t = n_ctx // 128
    i = 0
    while i < NT:
        lim = nctx_t if i < nctx_t else NT
        n = min(4, lim - i)
        groups.append((i, n))
        i += n
    with f.scope():
        self.alloc_h_tmps()
        mb = self.load_mod_bcast([0, 1])
        hTg = [f.sb("hTg%d" % gi, [128, 8, 512], BF16) for gi in range(len(groups))]
        xts = [f.sb("sxt%d" % j, [128, D]) for j in range(2)]
        for gi, (i0, n) in enumerate(groups):
            for j in range(n):
                i = i0 + j
                xt = xts[i % 2]
                for (ps_, ap) in self.ssm_dram_rows(self.XS, i):
                    f.load(xt[ps_, :], ap)
                r = 1 if i < nctx_t else 0
                self.emit_h(xt, mb[(r, 0)], mb[(r, 1)], hTg[gi][:, :, j * 128:(j + 1) * 128])
        zscope = f.scope()
        zscope.__enter__()
        wfs = [f.sb("swf%d" % j, [128, 8, 512]) for j in range(2)]
        wbs = [f.sb("swb%d" % j, [128, 8, 512], BF16) for j in range(2)]
        o16 = [f.sb("so16_%d" % j, [128, 512], BF16) for j in range(2)]
        o32 = [f.sb("so32_%d" % j, [128, 64]) for j in range(2)]
        blocks = [(ZOFF + c0, 512, "z") for c0 in range(0, 2048, 512)] + [(DOFF, 64, "dt")]
        for bi_, (c0, nb, kind) in enumerate(blocks):
            wf, wb = wfs[bi_ % 2], wbs[bi_ % 2]
            f.load(wf[:, :, 0:nb], self.w_in[l, :, c0:c0 + nb].re("(k p) n -> p k n", p=128))
            f.copy(wb[:, :, 0:nb], wf[:, :, 0:nb], eng="pool")
            for gi, (i0, n) in enumerate(groups):
                for j in range(n):
                    i = i0 + j
                    pb = self.bank()
                    for k in range(8):
                        f.mm(pb[:, 0:nb], hTg[gi][:, k, j * 128:(j + 1) * 128], wb[:, k, 0:nb], start=(k == 0), stop=(k == 7))
                    if kind == "z":
                        o = o16[i % 2]
                        f.act(o, pb, AF.Silu)
                        f.store(self.ZS[i * 128:(i + 1) * 128, c0 - ZOFF:c0 - ZOFF + 512], o)
                    else:
                        o = o32[i % 2]
                        f.copy(o, pb[:, 0:64], eng="act")
                        f.store(self.DTA[i * 128:(i + 1) * 128, :], o)
        zscope.__exit__(None, None, None)
        Lp = T + 8
        lat_off = n_ctx + 6
        XB = f.sb("XB", [128, Lp])
        CV = f.sb("CV", [128, Lp - 4])
        CVb = f.sb("CVb", [128, Lp - 4], BF16)
        f.memset(XB, 0.0)
        cws = [f.sb("cw%d" % j, [128, 6]) for j in range(2)]
        xwf = [f.sb("xwf%d" % j, [128, 8, 128]) for j in range(2)]
        xwb = [f.sb("xwb%d" % j, [128, 8, 128], BF16) for j in range(2)]
        stg = [f.sb("stg%d" % j, [128, 8, 128], BF16) for j in range(2)]
        si = 0
        for blk in range(32):
            wf, wb, cw = xwf[blk % 2], xwb[blk % 2], cws[blk % 2]
            c0 = XOFF + blk * 128
            f.load(wf, self.w_in[l, :, c0:c0 + 128].re("(k p) n -> p k n", p=128))
            f.copy(wb, wf, eng="pool")
            f.load(cw[:, 0:5], self.ssm_conv_w[l, :, blk * 128:(blk + 1) * 128].re("k c -> c k"), allow_slow_non_contiguous=True)
            f.load(cw[:, 5:6], self.ssm_conv_b[l, blk * 128:(blk + 1) * 128].re("(c o) -> c o", o=1), allow_slow_non_contiguous=True)
            for gi, (i0, n) in enumerate(groups):
                pb = self.bank()
                ng = n * 128
                for k in range(8):
                    f.mm(pb[:, 0:ng], wb[:, k, :], hTg[gi][:, k, 0:ng], start=(k == 0), stop=(k == 7))
                off = (2 if i0 < nctx_t else 6) + i0 * 128
                f.copy(XB[:, off:off + ng], pb[:, 0:ng], eng="act")
            Lc = Lp - 4
            f.ts(CV, XB[:, 0:Lc], cw[:, 0:1], ALU.mult, cw[:, 5:6], ALU.add)
            for kq in range(1, 5):
                f.stt(CV, XB[:, kq:kq + Lc], cw[:, kq:kq + 1], CV, ALU.mult, ALU.add)
            f.act(CVb, CV, AF.Silu)
            def cpos(i):
                return i * 128 if i < nctx_t else i * 128 + 4
            if blk < 24:
                dst = self.XSM if blk < 16 else self.BTM
                cc = (blk if blk < 16 else blk - 16) * 128
                for gi, (i0, n) in enumerate(groups):
                    if gi % 2 == 0:
                        pbb = self.bank().bitcast(BF16)
                        st = stg[si % 2]
                        si += 1
                    slot0 = (gi % 2) * 4
                    for j in range(n):
                        i = i0 + j
                        f.tr(pbb[:, (slot0 + j) * 128:(slot0 + j + 1) * 128], CVb[:, cpos(i):cpos(i) + 128], self.identb)
                    f.copy(st[:, slot0:slot0 + n, :], pbb[:, slot0 * 128:(slot0 + n) * 128].re("p (c n) -> p c n", n=128))
                    f.store(dst[i0 * 128:(i0 + n) * 128, cc:cc + 128].re("(c p) n -> p c n", p=128), st[:, slot0:slot0 + n, :])
            if blk >= 16:
                gidx = blk - 16
                f.store(self.BCT[gidx, :, 0:n_ctx], CVb[:, 0:n_ctx])
                f.store(self.BCT[gidx, :, n_ctx:T], CVb[:, n_ctx + 4:T + 4])


KB.ssm_rows = _ssm_rows
KB.ssm_dram_rows = _ssm_dram_rows
KB.stage_S1 = _stage_S1


def _stage_S2(self, l):
    f = self.f
    T, NT, n_ctx = self.T, self.NT, self.n_ctx
    nctx_t = n_ctx // 128
    cst = self.cst
    with f.scope():
        def bc(name, row_ap, n):
            t = f.sb(name, [128, n])
            self.bcast_load(t, row_ap, n)
            return t
        NW = bc("ssNW", self.ssm_norm_w[l, :], 2048)
        DTB = bc("ssDTB", self.ssm_dt_bias[l, :], 64)
        AL = bc("ssAL", self.ssm_a_log[l, :], 64)
        DS = bc("ssDS", self.ssm_d[l, :], 64)
        Aneg = f.sb("ssA", [128, 64])
        f.act(Aneg, AL, AF.Exp)
        f.ts(Aneg, Aneg, -1.0, ALU.mult)
        Dsk = f.sb("ssDsk", [128, 32])
        f.tt(Dsk, DS[:, 0:32], DS[:, 32:64], ALU.add)
        xs = f.sb("ss_xs", [128, 32, 64], BF16)
        Btm = f.sb("ss_Btm", [128, 1024], BF16)
        BT = f.sb("ss_BT", [128, 8, 128], BF16)
        CT = f.sb("ss_CT", [128, 8, 128], BF16)
        dtr = f.sb("ss_dtr", [128, 64])
        zs = f.sb("ss_zs", [128, 2048], BF16)
        ysf = f.sb("ss_ysf", [128, 2048])
        y = f.sb("ss_y", [128, 2048])
        yo = f.sb("ss_yo", [128, 2048], BF16)
        LT = f.sb("ss_LT", [128, 32, 128])
        xdt = f.sb("ss_xdt", [128, 32, 64], BF16)
        xe = f.sb("ss_xe", [128, 32, 64], BF16)
        dt = f.sb("ss_dt", [128, 32])
        ld = f.sb("ss_ld", [128, 32])
        cumT = f.sb("ss_cumT", [128, 32])
        dte = f.sb("ss_dte", [128, 32])
        cd = f.sb("ss_cd", [128, 32])
        seg = f.sb("ss_seg", [128, 4, 128])
        Lm = f.sb("ss_L", [128, 4, 128])
        Ec = f.sb("ss_Ec", [128, 4, 128])
        CBs = f.sb("ss_CBs", [128, 128])
        MT = f.sb("ss_MT", [128, 4, 128], BF16)
        CsT = f.sb("ss_CsT", [128, 4, 128], BF16)
        g8 = f.sb("ss_g8", [128, 8])
        S32 = [f.sb("ss_S32_%d" % g, [128, 4, 64]) for g in range(8)]
        S16 = [f.sb("ss_S16_%d" % g, [128, 4, 64], BF16) for g in range(8)]
        bcl = lambda t: t.re("p (h o) -> p h o", o=1)
        for d in range(2):
            for g in range(8):
                f.memset(S32[g], 0.0)
                f.memset(S16[g], 0.0, eng="pool")
            ctx_chunks = list(range(nctx_t))
            lat_chunks = list(range(nctx_t, NT))
            order = ctx_chunks + lat_chunks if d == 0 else ctx_chunks[::-1] + lat_chunks[::-1]
            incl = cst[:, 2 if d == 0 else 4, :]
            negm = cst[:, 6 if d == 0 else 7, :]
            for i in order:
                u0 = i * 128
                f.load(xs, self.XSM[u0:u0 + 128, :].re("p (h n) -> p h n", h=32))
                f.load(Btm, self.BTM[u0:u0 + 128, :])
                f.load(BT, self.BCT[0:8, :, u0:u0 + 128].re("g n t -> n g t"))
                f.load(CT, self.BCT[8:16, :, u0:u0 + 128].re("g n t -> n g t"))
                f.load(dtr, self.DTA[u0:u0 + 128, :])
                if d == 1:
                    f.load(zs, self.ZS[u0:u0 + 128, :])
                    f.load(ysf, self.YSF[u0:u0 + 128, :])
                f.tt(dt, dtr[:, d * 32:(d + 1) * 32], DTB[:, d * 32:(d + 1) * 32], ALU.add)
                f.act(dt, dt, AF.Exp)
                f.ts(dt, dt, 1.0, ALU.add)
                f.act(dt, dt, AF.Ln)
                f.tt(ld, dt, Aneg[:, d * 32:(d + 1) * 32], ALU.mult)
                pb = self.bank()
                f.mm(pb[:, 0:32], incl, ld)
                f.mm(pb[:, 32:64], self.ones, ld)
                f.copy(cumT, pb[:, 0:32])
                f.tt(dte, pb[:, 32:64], cumT, ALU.subtract)
                f.act(dte, dte, AF.Exp)
                f.act(cd, pb[:, 32:64], AF.Exp)
                f.tt(LT, incl.re("p (o n) -> p o n", o=1).bc([128, 32, 128]), bcl(ld).bc([128, 32, 128]), ALU.mult)
                f.tt(xdt, xs, bcl(dt).bc([128, 32, 64]), ALU.mult)
                f.tt(xe, xdt, bcl(dte).bc([128, 32, 64]), ALU.mult)
                for g in range(8):
                    hs = slice(4 * g, 4 * g + 4)
                    pc = self.bank()
                    f.mm(pc, self.ones, LT[:, hs, :].re("p h n -> p (h n)"))
                    pc4 = pc.re("p (h n) -> p h n", h=4)
                    f.tt(seg, pc4, negm.re("p (o n) -> p o n", o=1).bc([128, 4, 128]), ALU.add)
                    f.tt(seg, seg, bcl(cumT[:, hs]).bc([128, 4, 128]), ALU.subtract)
                    f.act(Lm, seg, AF.Exp)
                    f.act(Ec, pc4, AF.Exp)
                    pcb = self.bank()
                    f.mm(pcb[:, 0:128], BT[:, g, :], CT[:, g, :])
                    f.copy(CBs, pcb[:, 0:128], eng="act")
                    f.tt(MT, Lm, CBs.re("p (o n) -> p o n", o=1).bc([128, 4, 128]), ALU.mult)
                    f.tt(CsT, Ec, CT[:, g, :].re("p (o n) -> p o n", o=1).bc([128, 4, 128]), ALU.mult)
                    py = self.bank()
                    for hh in range(4):
                        h = 4 * g + hh
                        f.mm(py[:, hh * 64:(hh + 1) * 64], MT[:, hh, :], xdt[:, h, :], start=True, stop=False)
                        f.mm(py[:, hh * 64:(hh + 1) * 64], CsT[:, hh, :], S16[g][:, hh, :], start=False, stop=True)
                    if d == 0:
                        f.copy(y[:, g * 256:(g + 1) * 256], py[:, 0:256], eng="act")
                    else:
                        f.tt(y[:, g * 256:(g + 1) * 256], py[:, 0:256], ysf[:, g * 256:(g + 1) * 256], ALU.add)
                    pst = self.bank()
                    f.mm(pst[:, 0:256], Btm[:, g * 128:(g + 1) * 128], xe[:, hs, :].re("p h n -> p (h n)"))
                    f.tt(S32[g], S32[g], bcl(cd[:, hs]).bc([128, 4, 64]), ALU.mult)
                    f.tt(S32[g], S32[g], pst[:, 0:256].re("p (h n) -> p h n", h=4), ALU.add)
                    f.copy(S16[g], S32[g], eng="pool")
                if d == 0:
                    f.store(self.YSF[u0:u0 + 128, :], y)
                    continue
                y3 = y.re("p (h n) -> p h n", h=32)
                t3 = ysf.re("p (h n) -> p h n", h=32)
                f.tt(t3, xs, bcl(Dsk).bc([128, 32, 64]), ALU.mult)
                f.tt(y, y, ysf, ALU.add)
                f.tt(y, y, zs, ALU.mult)
                f.tt(ysf, y, y, ALU.mult)
                f.reduce(g8, ysf.re("p (g n) -> p g n", g=8), ALU.add)
                f.ts(g8, g8, 1.0 / 256, ALU.mult, 1e-5, ALU.add)
                f.act(g8, g8, AF.Sqrt)
                f.recip(g8, g8)
                f.tt(y.re("p (g n) -> p g n", g=8), y.re("p (g n) -> p g n", g=8), g8.re("p (g o) -> p g o", o=1).bc([128, 8, 256]), ALU.mult)
                f.tt(yo, y, NW, ALU.mult)
                for (ps_, ap) in self.ssm_dram_rows(self.YSSM, i):
                    f.store(ap, yo[ps_, :])


KB.stage_S2 = _stage_S2


def _load_w_bf16(self, dst, src_ap, nk, ncols, stage_tiles):
    f = self.f
    j = 0
    for k0 in range(0, nk, 8):
        kn = min(8, nk - k0)
        for c0 in range(0, ncols, 512):
            st = stage_tiles[j % 2]
            j += 1
            f.load(st[:, 0:kn, :], src_ap[k0 * 128:(k0 + kn) * 128, c0:c0 + 512].re("(k p) n -> p k n", p=128))
            f.copy(dst[:, k0:k0 + kn, c0:c0 + 512], st[:, 0:kn, :], eng="pool")


def _stage_G(self, l):
    f = self.f
    NT, n_ctx = self.NT, self.n_ctx
    with f.scope():
        Wrw = f.sb("gWrw", [128, 8, 1024], BF16)
        Wss = f.sb("gWss", [128, 16, 1024], BF16)
        Wo = f.sb("gWo", [128, 8, 1024], BF16)
        with f.scope():
            st = [f.sb("gst%d" % j, [128, 8, 512]) for j in range(2)]
            self.load_w_bf16(Wrw, self.w_branch_rw[l], 8, 1024, st)
            self.load_w_bf16(Wss, self.w_branch_ssm[l], 16, 1024, st)
            self.load_w_bf16(Wo, self.w_out[l], 8, 1024, st)
        gate = {}
        for r in range(2):
            t = f.sb("gGate%d" % r, [128, D])
            self.bcast_load(t, self.MODS[r, 2, :], D)
            gate[r] = t
        yin = [f.sb("g_yin%d" % j, [128, 3072], BF16) for j in range(2)]
        gsb = [f.sb("g_gs%d" % j, [128, 2048], BF16) for j in range(2)]
        xts = [f.sb("g_xt%d" % j, [128, D]) for j in range(2)]
        yT = f.sb("g_yT", [128, 24, 128], BF16)
        t1 = f.sb("g_t1", [128, 1024])
        t2 = f.sb("g_t2", [128, 1024])
        mg = f.sb("g_mg", [128, 1024], BF16)
        mT = f.sb("g_mT", [128, 8, 128], BF16)
        for i in range(NT):
            r = 1 if i * 128 < n_ctx else 0
            yi, gs, xt = yin[i % 2], gsb[i % 2], xts[i % 2]
            rs = slice(i * 128, (i + 1) * 128)
            f.load(yi[:, 0:1024], self.YRW[rs, :])
            f.load(yi[:, 1024:3072], self.YSSM[rs, :])
            f.load(gs, self.GS[rs, :])
            f.load(xt, self.XS[rs, :])
            for b3 in range(3):
                pbb = self.bank().bitcast(BF16)
                for j in range(8):
                    kq = b3 * 8 + j
                    f.tr(pbb[:, j * 128:(j + 1) * 128], yi[:, kq * 128:(kq + 1) * 128], self.identb)
                f.copy(yT[:, b3 * 8:(b3 + 1) * 8, :], pbb.re("p (k n) -> p k n", k=8), eng=("act" if b3 % 2 else "dve"))
            for half in range(2):
                hs = slice(half * 512, (half + 1) * 512)
                p1 = self.bank()
                for kq in range(8):
                    f.mm(p1, yT[:, kq, :], Wrw[:, kq, hs], start=(kq == 0), stop=(kq == 7))
                p2 = self.bank()
                for kq in range(16):
                    f.mm(p2, yT[:, 8 + kq, :], Wss[:, kq, hs], start=(kq == 0), stop=(kq == 15))
                f.tt(t1[:, hs], p1, gs[:, hs], ALU.mult)
                f.tt(t2[:, hs], p2, gs[:, 1024 + half * 512:1024 + (half + 1) * 512], ALU.mult)
            f.tt(mg, t1, t2, ALU.add)
            pbb = self.bank().bitcast(BF16)
            for j in range(8):
                f.tr(pbb[:, j * 128:(j + 1) * 128], mg[:, j * 128:(j + 1) * 128], self.identb)
            f.copy(mT, pbb.re("p (k n) -> p k n", k=8), eng="act")
            for half in range(2):
                hs = slice(half * 512, (half + 1) * 512)
                p1 = self.bank()
                for kq in range(8):
                    f.mm(p1, mT[:, kq, :], Wo[:, kq, hs], start=(kq == 0), stop=(kq == 7))
                f.tt(t1[:, hs], p1, gate[r][:, hs], ALU.mult)
            f.tt(xt, xt, t1, ALU.add)
            f.store(self.XS[rs, :], xt)


def _stage_F(self, l, tiles_per_pass=12):
    FDBG = ''
    f = self.f
    NT, n_ctx = self.NT, self.n_ctx
    with f.scope():
        mb = self.load_mod_bcast([3, 4, 5])
        BR = f.sb("fBR", [128, 32])
        self.bcast_load(BR, self.b_router, 32)
        wr = f.sb("fwr", [128, 8, 32])
        f.load(wr, self.w_router.re("(k p) n -> p k n", p=128))
        self.alloc_h_tmps()
        w13f = [f.sb("f_w13f%d" % j, [128, 8, 512]) for j in range(2)]
        w2f = f.sb("f_w2f", [128, 4, 1024])
        w1b = f.sb("f_w1b", [128, 8, 512], BF16)
        w3b = f.sb("f_w3b", [128, 8, 512], BF16)
        w2b = f.sb("f_w2b", [128, 4, 1024], BF16)
        sil = f.sb("f_sil", [128, 512], BF16)
        gT = f.sb("f_gT", [128, 4, 512], BF16)
        hf = f.sb("f_hf", [128, 8, 128])
        sc = f.sb("f_sc", [128, 32])
        bi = f.sb("f_bi", [128, 32])
        m8 = f.sb("f_m8", [128, 4, 8])
        gsx = f.sb("f_gsx", [128, 4])
        gmx = f.sb("f_gmx", [128, 1])
        gm = f.sb("f_gm", [128, 4])
        tq = f.sb("f_tq", [128, 4])
        thr = f.sb("f_thr", [128, 1])
        em = f.sb("f_em", [128, 32])
        den = f.sb("f_den", [128, 1])
        xts = [f.sb("f_xt%d" % j, [128, D]) for j in range(2)]
        for p0 in range(0, NT, tiles_per_pass):
            tiles = list(range(p0, min(NT, p0 + tiles_per_pass)))
            ng = (len(tiles) + 3) // 4
            with f.scope():
                hTg = [f.sb("f_hT%d" % gi, [128, 8, 512], BF16) for gi in range(ng)]
                acc = [f.sb("f_acc%d" % j, [128, D]) for j in range(len(tiles))]
                gw = [f.sb("f_gw%d" % j, [128, 32]) for j in range(len(tiles))]
                for j, i in enumerate(tiles):
                    xt = xts[j % 2]
                    r = 1 if i * 128 < n_ctx else 0
                    f.load(xt, self.XS[i * 128:(i + 1) * 128, :])
                    self.emit_h(xt, mb[(r, 3)], mb[(r, 4)], hTg[j // 4][:, :, (j % 4) * 128:(j % 4 + 1) * 128], hf_dst=hf)
                    f.memset(acc[j], 0.0, eng="pool")
                    pb = self.bank()
                    for k in range(8):
                        f.mm(pb[:, 0:32], hf[:, k, :], wr[:, k, :], start=(k == 0), stop=(k == 7))
                    f.act(sc, pb[:, 0:32], AF.Sigmoid)
                    f.tt(bi, sc, BR, ALU.add)
                    for g in range(4):
                        f.max8(m8[:, g, :], bi[:, g * 8:(g + 1) * 8])
                    f.tt(gsx, m8[:, :, 0], m8[:, :, 1], ALU.add)
                    f.reduce(gmx, gsx, ALU.max)
                    f.ts(gm, gsx, gmx[:, 0:1], ALU.is_equal)
                    f.tt(tq, gm, m8[:, :, 1], ALU.mult)
                    f.reduce(thr, tq, ALU.add)
                    f.ts(em, bi, thr[:, 0:1], ALU.is_ge)
                    f.tt(em.re("p (g n) -> p g n", g=4), em.re("p (g n) -> p g n", g=4),
                         gm.re("p (g o) -> p g o", o=1).bc([128, 4, 8]), ALU.mult)
                    f.tt(gw[j], sc, em, ALU.mult)
                    f.reduce(den, gw[j], ALU.add)
                    f.recip(den, den)
                    f.ts(gw[j], gw[j], den[:, 0:1], ALU.mult)
                for e in range(0 if 'noexp' in FDBG else (2 if 'exp2' in FDBG else 32)):
                    f.load(w13f[0], self.exp_w1[l, e].re("(k p) n -> p k n", p=128))
                    f.copy(w1b, w13f[0], eng="pool")
                    f.load(w13f[1], self.exp_w3[l, e].re("(k p) n -> p k n", p=128))
                    f.copy(w3b, w13f[1], eng="pool")
                    f.load(w2f, self.exp_w2[l, e].re("(k p) n -> p k n", p=128))
                    f.copy(w2b, w2f, eng="pool")
                    for gi in range(ng):
                        nt_g = min(4, len(tiles) - gi * 4)
                        ntok = nt_g * 128
                        for fc in range(4):
                            fs = slice(fc * 128, (fc + 1) * 128)
                            pa, pb = self.bank(), self.bank()
                            for k in range(8):
                                f.mm(pa[:, 0:ntok], w1b[:, k, fs], hTg[gi][:, k, 0:ntok], start=(k == 0), stop=(k == 7))
                            for k in range(8):
                                f.mm(pb[:, 0:ntok], w3b[:, k, fs], hTg[gi][:, k, 0:ntok], start=(k == 0), stop=(k == 7))
                            f.act(sil[:, 0:ntok], pa[:, 0:ntok], AF.Silu)
                            f.tt(gT[:, fc, 0:ntok], sil[:, 0:ntok], pb[:, 0:ntok], ALU.mult)
                        for jj in range(nt_g):
                            j = gi * 4 + jj
                            for half in range(2):
                                hs = slice(half * 512, (half + 1) * 512)
                                po = self.bank()
                                for fc in range(4):
                                    f.mm(po, gT[:, fc, jj * 128:(jj + 1) * 128], w2b[:, fc, hs], start=(fc == 0), stop=(fc == 3))
                                f.stt(acc[j][:, hs], po, gw[j][:, e:e + 1], acc[j][:, hs], ALU.mult, ALU.add)
                for j, i in enumerate(tiles):
                    xt = xts[j % 2]
                    r = 1 if i * 128 < n_ctx else 0
                    f.load(xt, self.XS[i * 128:(i + 1) * 128, :])
                    f.tt(acc[j], acc[j], mb[(r, 5)], ALU.mult)
                    f.tt(xt, xt, acc[j], ALU.add)
                    f.store(self.XS[i * 128:(i + 1) * 128, :], xt)


KB.load_w_bf16 = _load_w_bf16
KB.stage_G = _stage_G
KB.stage_F = _stage_F


def build_program(n_ctx, n_lat, depth):
    K = KB(n_ctx, n_lat, depth)
    K.stage_init()
    for l in range(depth):
        K.stage_mod(l)
        K.stage_P(l)
        K.stage_R(l)
        K.stage_S1(l)
        K.stage_S2(l)
        K.stage_G(l)
        K.stage_F(l)
    K.stage_final()
    return K.finish()


def kernel(**inputs):
    from concourse.bass_utils import run_bass_kernel_spmd
    inp = {k: np.asarray(v) for k, v in inputs.items()}
    B, n_lat, _ = inp["x"].shape
    n_ctx = inp["ctx"].shape[1]
    L = inp["w_mod"].shape[0]
    nc = build_program(n_ctx, n_lat, L)
    consts = make_consts()
    n_cores = 8
    in_maps = []
    for core in range(n_cores):
        b = core // 2
        m = {}
        for k, v in inp.items():
            if k in ("x", "ctx"):
                m[k] = np.ascontiguousarray(v[b])
            elif k in ("c", "c_ctx"):
                continue
            else:
                m[k] = np.ascontiguousarray(v)
        m["c2"] = np.ascontiguousarray(np.stack([inp["c"][b], inp["c_ctx"]]))
        m["consts"] = consts
        m["rw_r_k"] = np.ascontiguousarray(inp["rw_r_k"].reshape(L, 1024))
        m["ssm_dt_bias"] = np.ascontiguousarray(inp["ssm_dt_bias"].reshape(L, 64))
        m["ssm_a_log"] = np.ascontiguousarray(inp["ssm_a_log"].reshape(L, 64))
        m["ssm_d"] = np.ascontiguousarray(inp["ssm_d"].reshape(L, 64))
        in_maps.append(m)
    res = run_bass_kernel_spmd(nc, in_maps, core_ids=list(range(n_cores)))
    out = np.stack([np.asarray(res.results[2 * b]["out"]) for b in range(B)], axis=0)
    return out.astype(np.float32)
```

```python
import numpy as np
import concourse.bass as bass
import concourse.mybir as mybir

F32 = mybir.dt.float32
BF16 = mybir.dt.bfloat16
I32 = mybir.dt.int32
AF = mybir.ActivationFunctionType
ALU = mybir.AluOpType
AX = mybir.AxisListType

N_DMA_SEMS = 6
N_ENG_SEMS = 8
SAME_ENGINE_SYNC = True


class Trk:
    __slots__ = ("w", "r")

    def __init__(self):
        self.w = None
        self.r = []


class V:
    __slots__ = ("t", "ap")

    def __init__(self, t, ap):
        self.t = t
        self.ap = ap

    def __getitem__(self, key):
        return V(self.t, self.ap[key])

    def re(self, s, **kw):
        return V(self.t, self.ap.rearrange(s, **kw))

    def bc(self, shape):
        return V(self.t, self.ap.to_broadcast(shape))

    def bitcast(self, dt):
        return V(self.t, self.ap.bitcast(dt))


class Fw:
    ENGS = ("pe", "act", "dve", "pool", "sp")

    def __init__(self, nc):
        self.nc = nc
        self.handles = {"pe": nc.tensor, "act": nc.scalar, "dve": nc.vector, "pool": nc.gpsimd, "sp": nc.sync}
        self.ops = {e: [] for e in self.ENGS}
        self.esem = {e: [nc.alloc_semaphore("s_%s%d" % (e, i)) for i in range(N_ENG_SEMS)] for e in self.ENGS}
        self.dsems = {}
        self.dcount = {}
        self.dnext = {}
        for q in ("sp", "pool", "act"):
            self.dsems[q] = [nc.alloc_semaphore("d_%s%d" % (q, i)) for i in range(N_DMA_SEMS)]
            self.dcount[q] = [0] * N_DMA_SEMS
            self.dnext[q] = 0
        self.n_alloc = 0
        self.stacks = []
        self.names = {}
        self.out_events = []

    def sb(self, name, shape, dtype=F32):
        self.n_alloc += 1
        nm = "%s_%d" % (name, self.n_alloc)
        self.names[name] = nm
        if self.stacks:
            h = self.stacks[-1].enter_context(self.nc.sbuf_tensor(nm, list(shape), dtype))
        else:
            h = self.nc.alloc_sbuf_tensor(nm, list(shape), dtype)
        return V(Trk(), h[tuple(slice(None) for _ in shape)])

    def scope(self):
        fw = self

        class _S:
            def __enter__(s):
                import contextlib
                fw.stacks.append(contextlib.ExitStack())
                return s

            def __exit__(s, *a):
                fw.barrier()
                fw.stacks.pop().close()
                return False
        return _S()

    def barrier(self):
        last = {}
        for e in self.ENGS:
            last[e] = -1
            for i in range(len(self.ops[e]) - 1, -1, -1):
                o = self.ops[e][i]
                if o["dma"] is None and o["fn"] is not None:
                    last[e] = i
                    break
        for e in self.ENGS:
            waits = {}
            for e2 in self.ENGS:
                if (e2 != e or (SAME_ENGINE_SYNC and e != 'pe')) and last[e2] >= 0:
                    waits[("e", e2)] = last[e2]
            for q in self.dsems:
                for si in range(N_DMA_SEMS):
                    if self.dcount[q][si] > 0:
                        waits[("d", q, si)] = self.dcount[q][si]
            self.ops[e].append({"waits": waits, "fn": None, "dma": None})

    def ps(self, name, shape, dtype=F32):
        self.n_alloc += 1
        h = self.nc.alloc_psum_tensor("%s_%d" % (name, self.n_alloc), list(shape), dtype)
        return V(Trk(), h[tuple(slice(None) for _ in shape)])

    def dram(self, name, shape, dtype=F32, kind="Internal"):
        h = self.nc.dram_tensor(name, list(shape), dtype, kind=kind)
        return V(Trk(), h.ap())

    def _deps(self, eng, reads, writes):
        deps = []
        for v in reads:
            if v.t.w is not None:
                deps.append(v.t.w)
        for v in writes:
            if v.t.w is not None:
                deps.append(v.t.w)
            deps.extend(v.t.r)
        out = {}
        for ev in deps:
            kind = ev[0]
            if kind == "e":
                _, e, idx = ev
                if e == eng and (not SAME_ENGINE_SYNC or e == "pe"):
                    continue
                k = ("e", e)
                out[k] = max(out.get(k, -1), idx)
            else:
                _, q, si, val = ev
                k = ("d", q, si)
                out[k] = max(out.get(k, -1), val)
        return out

    def _mark(self, ev, reads, writes):
        for v in reads:
            v.t.r.append(ev)
        for v in writes:
            v.t.w = ev
            v.t.r = []

    def op(self, eng, fn, outs, ins):
        outs = [o for o in outs if o is not None]
        ins = [i for i in ins if isinstance(i, V)]
        waits = self._deps(eng, ins, outs)
        idx = len(self.ops[eng])
        self.ops[eng].append({"waits": waits, "fn": fn, "dma": None})
        self._mark(("e", eng, idx), ins, outs)

    def dma(self, q, out, in_, **kw):
        waits = self._deps(q, [in_], [out])
        si = self.dnext[q]
        self.dnext[q] = (si + 1) % N_DMA_SEMS
        if self.dcount[q][si] > 0:
            k = ("d", q, si)
            waits[k] = max(waits.get(k, -1), self.dcount[q][si])
        self.dcount[q][si] += 1
        val = self.dcount[q][si]
        o_ap, i_ap = out.ap, in_.ap
        self.ops[q].append({"waits": waits, "fn": lambda h: h.dma_start(out=o_ap, in_=i_ap, **kw), "dma": (si, val)})
        ev = ("d", q, si, val)
        self._mark(ev, [in_], [out])
        return ev

    def finish(self):
        nc = self.nc
        need = {e: set() for e in self.ENGS}
        for e in self.ENGS:
            for o in self.ops[e]:
                for k, v in o["waits"].items():
                    if k[0] == "e":
                        need[k[1]].add(v)
        rank = {}
        for e in self.ENGS:
            r = 0
            for i in range(len(self.ops[e])):
                if i in need[e]:
                    rank[(e, i)] = (r % N_ENG_SEMS, r // N_ENG_SEMS + 1)
                    r += 1
        final_waits = []
        for q in self.dsems:
            for si in range(N_DMA_SEMS):
                if self.dcount[q][si] > 0:
                    final_waits.append((self.dsems[q][si], 16 * self.dcount[q][si]))
        ops, esem, dsems, handles = self.ops, self.esem, self.dsems, self.handles

        def replay(e, h):
            seen = {}
            for i, o in enumerate(ops[e]):
                for k, v in o["waits"].items():
                    if k[0] == "e":
                        si_, val = rank[(k[1], v)]
                        sem = esem[k[1]][si_]
                        k = ("e", k[1], si_)
                    else:
                        sem, val = dsems[k[1]][k[2]], 16 * v
                    if seen.get(k, -1) >= val:
                        continue
                    seen[k] = val
                    h.wait_ge(sem, val)
                if o["fn"] is None:
                    continue
                ins = o["fn"](h)
                if o["dma"] is not None:
                    ins.then_inc(dsems[e][o["dma"][0]], 16)
                elif (e, i) in rank:
                    ins.then_inc(esem[e][rank[(e, i)][0]], 1)

        with nc.Block() as block:
            @block.tensor
            def _(h):
                replay("pe", h)

            @block.scalar
            def _(h):
                replay("act", h)

            @block.vector
            def _(h):
                replay("dve", h)

            @block.gpsimd
            def _(h):
                replay("pool", h)

            @block.sync
            def _(h):
                replay("sp", h)
                for sem, val in final_waits:
                    h.wait_ge(sem, val)

    def mm(self, out, lhsT, rhs, start=True, stop=True):
        o, l, r = out.ap, lhsT.ap, rhs.ap
        self.op("pe", lambda h: h.matmul(o, l, r, start=start, stop=stop), [out], [lhsT, rhs] + ([] if start else [out]))

    def tr(self, out, in_, ident):
        o, i, d = out.ap, in_.ap, ident.ap
        self.op("pe", lambda h: h.transpose(o, i, d), [out], [in_, ident])

    def act(self, out, in_, func, bias=None, scale=None, accum=None, eng="act"):
        kw = {}
        if bias is not None:
            kw["bias"] = bias.ap if isinstance(bias, V) else bias
        if scale is not None:
            kw["scale"] = scale.ap if isinstance(scale, V) else scale
        if accum is not None:
            kw["accum_out"] = accum.ap
        o, i = out.ap, in_.ap
        self.op("act", lambda h: h.activation(o, i, func, **kw), [out, accum], [in_, bias, scale])

    def tt(self, out, a, b, op, eng="dve"):
        o, x, y = out.ap, a.ap, b.ap
        self.op(eng, lambda h: h.tensor_tensor(o, x, y, op), [out], [a, b])

    def ts(self, out, a, s1, op0, s2=None, op1=None, accum=None, eng="dve"):
        o, x = out.ap, a.ap
        c1 = s1.ap if isinstance(s1, V) else s1
        c2 = s2.ap if isinstance(s2, V) else s2
        kw = {}
        if op1 is not None:
            kw["op1"] = op1
        if accum is not None:
            kw["accum_out"] = accum.ap
        if op1 is None and accum is None:
            self.op(eng, lambda h: h.tensor_single_scalar(o, x, c1, op0), [out], [a, s1])
        else:
            self.op(eng, lambda h: h.tensor_scalar(o, x, c1, c2, op0, **kw), [out, accum], [a, s1, s2])

    def stt(self, out, a, s, b, op0, op1, eng="dve"):
        o, x, y = out.ap, a.ap, b.ap
        c = s.ap if isinstance(s, V) else s
        self.op(eng, lambda h: h.scalar_tensor_tensor(o, x, c, y, op0, op1), [out], [a, s, b])

    def copy(self, out, in_, eng="dve"):
        o, i = out.ap, in_.ap
        if eng == "act":
            self.op("act", lambda h: h.activation(o, i, AF.Identity), [out], [in_])
        else:
            self.op(eng, lambda h: h.tensor_copy(o, i), [out], [in_])

    def memset(self, out, val, eng="dve"):
        o = out.ap
        self.op(eng, lambda h: h.memset(o, val), [out], [])

    def reduce(self, out, in_, op, axis=AX.X, eng="dve"):
        o, i = out.ap, in_.ap
        self.op(eng, lambda h: h.tensor_reduce(o, i, axis, op), [out], [in_])

    def recip(self, out, in_):
        o, i = out.ap, in_.ap
        self.op("dve", lambda h: h.reciprocal(o, i), [out], [in_])

    def max8(self, out, in_):
        o, i = out.ap, in_.ap
        self.op("dve", lambda h: h.max(o, i), [out], [in_])

    def load(self, out, in_, q="sp", **kw):
        return self.dma(q, out, in_, **kw)

    def store(self, out, in_, q="pool", **kw):
        return self.dma(q, out, in_, **kw)


D = 1024
RW_COLS = 3488
SSM_COLS = 6208
IN_COLS = 11744
NCONST = 18
GRID_W = 64


def make_consts():
    c = np.zeros((128, NCONST, 128), np.float32)
    i = np.arange(128)
    s, t = i[:, None], i[None, :]
    c[:, 0] = np.eye(128)
    c[:, 1] = 1.0
    c[:, 2] = (s <= t)
    c[:, 3] = (s < t)
    c[:, 4] = (s >= t)
    c[:, 5] = (s > t)
    c[:, 6] = np.where(t >= s, 0.0, -1e30)
    c[:, 7] = np.where(t <= s, 0.0, -1e30)
    low = (s > t)
    c[:, 8] = low & (s // 8 == t // 8)
    for k in range(1, 5):
        bs = 8 << k
        c[:, 8 + k] = low & (s // bs == t // bs) & (s // (bs // 2) != t // (bs // 2))
    for k in range(5):
        c[:, 13 + k] = c[:, 8 + k].T
    return c


class KB:
    def __init__(self, n_ctx, n_lat, depth=2, stages="all"):
        self.n_ctx, self.n_lat, self.depth = n_ctx, n_lat, depth
        self.T = n_ctx + n_lat
        self.rows = n_lat // GRID_W
        self.NT = self.T // 128
        self.stages = stages
        nc = bass.Bass("TRN2", target_bir_lowering=False)
        self.nc = nc
        f = self.f = Fw(nc)
        T = self.T
        inp = lambda name, shape: f.dram(name, shape, F32, kind="ExternalInput")
        self.x = inp("x", [n_lat, D])
        self.ctx = inp("ctx", [n_ctx, D])
        self.c2 = inp("c2", [2, D])
        self.consts = inp("consts", [128, NCONST, 128])
        L = depth
        self.w_mod = inp("w_mod", [L, D, 6 * D])
        self.b_mod = inp("b_mod", [L, 6 * D])
        self.norm_mix_g = inp("norm_mix_g", [L, D])
        self.w_in = inp("w_in", [L, D, IN_COLS])
        self.rw_shift_mu = inp("rw_shift_mu", [L, RW_COLS])
        self.rw_w0 = inp("rw_w0", [L, 2, 1024])
        self.rw_w2 = inp("rw_w2", [L, 2, 64, 1024])
        self.rw_a0 = inp("rw_a0", [L, 2, 1024])
        self.rw_a2 = inp("rw_a2", [L, 2, 64, 1024])
        self.rw_g2 = inp("rw_g2", [L, 160, 1024])
        self.rw_k_k = inp("rw_k_k", [L, 1024])
        self.rw_k_a = inp("rw_k_a", [L, 1024])
        self.rw_r_k = inp("rw_r_k", [L, 1024])
        self.rw_ln_w = inp("rw_ln_w", [L, 1024])
        self.rw_ln_b = inp("rw_ln_b", [L, 1024])
        self.ssm_conv_w = inp("ssm_conv_w", [L, 5, 4096])
        self.ssm_conv_b = inp("ssm_conv_b", [L, 4096])
        self.ssm_dt_bias = inp("ssm_dt_bias", [L, 64])
        self.ssm_a_log = inp("ssm_a_log", [L, 64])
        self.ssm_d = inp("ssm_d", [L, 64])
        self.ssm_norm_w = inp("ssm_norm_w", [L, 2048])
        self.w_branch_rw = inp("w_branch_rw", [L, 1024, D])
        self.w_branch_ssm = inp("w_branch_ssm", [L, 2048, D])
        self.w_out = inp("w_out", [L, D, D])
        self.norm_ffn_g = inp("norm_ffn_g", [L, D])
        self.w_router = inp("w_router", [D, 32])
        self.b_router = inp("b_router", [32])
        self.exp_w1 = inp("exp_w1", [L, 32, D, 512])
        self.exp_w3 = inp("exp_w3", [L, 32, D, 512])
        self.exp_w2 = inp("exp_w2", [L, 32, 512, D])
        self.norm_final_g = inp("norm_final_g", [D])
        self.out = f.dram("out", [n_lat, D], F32, kind="ExternalOutput")
        self.XS = f.dram("XS", [T, D])
        self.MODS = f.dram("MODS", [2, 6, D])
        self.PRW = f.dram("PRW", [T + 6, RW_COLS])
        self.GS = f.dram("GS", [T, 2048], BF16)
        self.YF = f.dram("YF", [T, 1024])
        self.YRW = f.dram("YRW", [T, 1024], BF16, kind=("ExternalOutput" if stages == "debug" else "Internal"))
        self.cst = f.sb("cst", [128, NCONST, 128])
        self.cstb = f.sb("cstb", [128, NCONST, 128], BF16)
        self.banks = [f.ps("bank%d" % i, [128, 512]) for i in range(8)]
        self.bi = 0
        f.load(self.cst, self.consts)
        f.copy(self.cstb, self.cst)
        self.ident = self.cst[:, 0, :]
        self.identb = self.cstb[:, 0, :]
        self.ones = self.cst[:, 1, :]

    def bank(self):
        b = self.banks[self.bi]
        self.bi = (self.bi + 1) % 8
        return b

    def bcast_load(self, tile, row_ap, n):
        self.f.load(tile, V(row_ap.t, row_ap.ap.partition_broadcast(128)))

    def pad_row(self, t):
        return t + 1 if t < self.n_ctx else t + 3

    def stage_init(self):
        f = self.f
        f.store(self.XS[0:self.n_ctx, :], self.ctx, q="sp")
        f.store(self.XS[self.n_ctx:self.T, :], self.x, q="sp")

    def stage_mod(self, l):
        f = self.f
        with f.scope():
            cT = f.sb("cT", [128, 2, 8])
            scT = f.sb("scT", [128, 2, 8])
            with self.nc.allow_non_contiguous_dma("tiny transposed load"):
                f.load(cT, self.c2.re("r (k p) -> p r k", p=128), allow_slow_non_contiguous=True)
            f.act(scT, cT, AF.Silu)
            rows = [f.sb("mrow%d" % r, [1, 6 * D]) for r in range(2)]
            bm = f.sb("bm", [1, 6 * D])
            f.load(bm, self.b_mod[l:l + 1, :])
            g2 = f.sb("g2", [1, 2, D])
            f.load(g2[:, 0, :], self.norm_mix_g[l:l + 1, :])
            f.load(g2[:, 1, :], self.norm_ffn_g[l:l + 1, :])
            wts = [f.sb("wm%d" % i, [128, 8, 512]) for i in range(2)]
            for cb in range(12):
                wt = wts[cb % 2]
                f.load(wt, self.w_mod[l, :, cb * 512:(cb + 1) * 512].re("(k p) n -> p k n", p=128))
                for r in range(2):
                    pb = self.bank()
                    for k in range(8):
                        f.mm(pb[0:1, :], scT[:, r, k:k + 1], wt[:, k, :], start=(k == 0), stop=(k == 7))
                    f.tt(rows[r][:, cb * 512:(cb + 1) * 512], pb[0:1, :], bm[:, cb * 512:(cb + 1) * 512], ALU.add)
            for r in range(2):
                row = rows[r]
                o = f.sb("orow%d" % r, [1, 6, D])
                f.stt(o[:, 0, :], row[:, D:2 * D], 1.0, g2[:, 0, :], ALU.add, ALU.mult)
                f.copy(o[:, 1, :], row[:, 0:D])
                f.copy(o[:, 2, :], row[:, 2 * D:3 * D])
                f.stt(o[:, 3, :], row[:, 4 * D:5 * D], 1.0, g2[:, 1, :], ALU.add, ALU.mult)
                f.copy(o[:, 4, :], row[:, 3 * D:4 * D])
                f.copy(o[:, 5, :], row[:, 5 * D:6 * D])
                f.store(self.MODS[r:r + 1, :, :], o)

    def emit_h(self, xt, G, S, hT_dst, hf_dst=None):
        f = self.f
        sq, ss, hh = self.h_sq, self.h_ss, self.h_h
        f.act(sq, xt, AF.Square, accum=ss)
        f.ts(ss, ss, 1.0 / D, ALU.mult, 1e-6, ALU.add)
        f.act(ss, ss, AF.Sqrt)
        f.recip(ss, ss)
        f.stt(hh, xt, ss[:, 0:1], G, ALU.mult, ALU.mult)
        f.tt(hh, hh, S, ALU.add)
        for half in range(2):
            pb = self.bank()
            for k in range(4):
                kk = half * 4 + k
                f.tr(pb[:, k * 128:(k + 1) * 128], hh[:, kk * 128:(kk + 1) * 128], self.ident)
            if hf_dst is not None:
                f.copy(hf_dst[:, half * 4:(half + 1) * 4, :], pb.re("p (k n) -> p k n", k=4), eng="dve")
                f.copy(hT_dst[:, half * 4:(half + 1) * 4, :], hf_dst[:, half * 4:(half + 1) * 4, :], eng="act")
            else:
                f.copy(hT_dst[:, half * 4:(half + 1) * 4, :], pb.re("p (k n) -> p k n", k=4), eng="act")

    def alloc_h_tmps(self):
        f = self.f
        self.h_ss = f.sb("h_ss", [128, 1])
        self.h_h = f.sb("h_h", [128, D])
        self.h_sq = self.h_h

    def load_mod_bcast(self, idxs):
        f = self.f
        out = {}
        for r in range(2):
            for i in idxs:
                t = f.sb("mb%d_%d" % (r, i), [128, D])
                self.bcast_load(t, self.MODS[r, i, :], D)
                out[(r, i)] = t
        return out

    def stage_P(self, l):
        f = self.f
        T, NT = self.T, self.NT
        with f.scope():
            self.alloc_h_tmps()
            mb = self.load_mod_bcast([0, 1])
            hT = [f.sb("hT%d" % i, [128, 8, 128], BF16) for i in range(NT)]
            xts = [f.sb("xt%d" % i, [128, D]) for i in range(2)]
            for i in range(NT):
                xt = xts[i % 2]
                f.load(xt, self.XS[i * 128:(i + 1) * 128, :])
                r = 1 if i * 128 < self.n_ctx else 0
                self.emit_h(xt, mb[(r, 0)], mb[(r, 1)], hT[i])
            z = f.sb("zrow", [1, RW_COLS])
            f.memset(z, 0.0)
            for prow in (0, self.n_ctx + 1, self.n_ctx + 2, T + 3):
                f.store(self.PRW[prow:prow + 1, :], z)
            blocks = [(c0, min(512, RW_COLS - c0), "rw") for c0 in range(0, RW_COLS, 512)]
            blocks += [(RW_COLS + SSM_COLS + c0, 512, "gate") for c0 in range(0, 2048, 512)]
            wfs = [f.sb("wf%d" % i, [128, 8, 512]) for i in range(2)]
            wbs = [f.sb("wb%d" % i, [128, 8, 512], BF16) for i in range(2)]
            o32 = [f.sb("o32_%d" % i, [128, 512]) for i in range(2)]
            o16 = [f.sb("o16_%d" % i, [128, 512], BF16) for i in range(2)]
            for bi_, (c0, nb, kind) in enumerate(blocks):
                wf, wb = wfs[bi_ % 2], wbs[bi_ % 2]
                f.load(wf[:, :, 0:nb], self.w_in[l, :, c0:c0 + nb].re("(k p) n -> p k n", p=128))
                f.copy(wb[:, :, 0:nb], wf[:, :, 0:nb], eng="pool")
                for i in range(NT):
                    pb = self.bank()
                    for k in range(8):
                        f.mm(pb[:, 0:nb], hT[i][:, k, :], wb[:, k, 0:nb], start=(k == 0), stop=(k == 7))
                    if kind == "rw":
                        o = o32[i % 2]
                        f.copy(o[:, 0:nb], pb[:, 0:nb], eng="act")
                        pr = self.pad_row(i * 128)
                        f.store(self.PRW[pr:pr + 128, c0:c0 + nb], o[:, 0:nb])
                    else:
                        o = o16[i % 2]
                        f.act(o, pb, AF.Sigmoid)
                        g0 = c0 - RW_COLS - SSM_COLS
                        f.store(self.GS[i * 128:(i + 1) * 128, g0:g0 + 512], o)

    def finish(self):
        self.f.finish()
        return self.nc


def _stage_R(self, l):
    f = self.f
    NT = self.NT
    nctx_t = self.n_ctx // 128
    with f.scope():
        P = {}
        def bc(name, row_ap, n):
            t = f.sb(name, [128, n])
            self.bcast_load(t, row_ap, n)
            return t
        P["MU"] = bc("MU", self.rw_shift_mu[l, :], RW_COLS)
        P["KK"] = bc("KKp", self.rw_k_k[l, :], 1024)
        P["KA"] = bc("KAp", self.rw_k_a[l, :], 1024)
        P["RK"] = bc("RKp", self.rw_r_k[l, :], 1024)
        P["LNW"] = bc("LNW", self.rw_ln_w[l, :], 1024)
        P["LNB"] = bc("LNB", self.rw_ln_b[l, :], 1024)
        g2b = f.sb("g2b", [128, 2, 1024], BF16)
        with f.scope():
            g2f = f.sb("g2f", [128, 2, 1024])
            f.load(g2f[:, 0, :], self.rw_g2[l, 0:128, :])
            f.load(g2f[0:32, 1, :], self.rw_g2[l, 128:160, :])
            f.copy(g2b[:, 0, :], g2f[:, 0, :])
            f.copy(g2b[0:32, 1, :], g2f[0:32, 1, :])
        P["g2b"] = g2b
        W = self.rw_alloc_work()
        for d in range(2):
          with f.scope():
            P["W0"] = bc("W0p%d" % d, self.rw_w0[l, d, :], 1024)
            P["A0"] = bc("A0p%d" % d, self.rw_a0[l, d, :], 1024)
            lb = f.sb("lb%d" % d, [64, 2, 1024], BF16)
            with f.scope():
                lf = f.sb("lf%d" % d, [64, 2, 1024])
                f.load(lf[:, 0, :], self.rw_w2[l, d, :, :])
                f.load(lf[:, 1, :], self.rw_a2[l, d, :, :])
                f.copy(lb, lf)
            P["w2b"], P["a2b"] = lb[:, 0, :], lb[:, 1, :]
            H32 = [f.sb("H32_%d_%d" % (d, g), [64, 4, 64]) for g in range(4)]
            H16 = [f.sb("H16_%d_%d" % (d, g), [64, 4, 64], BF16) for g in range(4)]
            for g in range(4):
                f.memset(H32[g], 0.0)
                f.memset(H16[g], 0.0, eng="pool")
            ctx_chunks = list(range(nctx_t))
            lat_chunks = list(range(nctx_t, NT))
            order = ctx_chunks + lat_chunks if d == 0 else ctx_chunks[::-1] + lat_chunks[::-1]
            for c in order:
                self.rw_chunk(l, c, d, P, W, H32, H16)


def _rw_alloc_work(self):
    f = self.f
    W = {}
    for n in ("cur", "prev", "nxt"):
        W[n] = f.sb("rw_" + n, [128, RW_COLS])
    W["y"] = f.sb("rw_y", [128, 1024])
    for j, n in enumerate(("kk", "t1", "t2")):
        W[n] = W["prev"][:, j * 1024:(j + 1) * 1024]
    for j, n in enumerate(("ar", "logw", "E")):
        W[n] = W["nxt"][:, j * 1024:(j + 1) * 1024]
    for n in ("Rt", "Kt", "Bt", "At", "V16"):
        W[n] = f.sb("rw_" + n, [128, 1024], BF16)
    for n in ("RT", "KT", "BT", "AT"):
        W[n] = f.sb("rw_" + n, [64, 16, 128], BF16)
    W["s16"] = f.sb("rw_s16", [128, 16])
    W["s16b"] = f.sb("rw_s16b", [128, 16])
    W["tw"] = f.sb("rw_tw", [128, 2, 64])
    W["lT"] = f.sb("rw_lT", [64, 2, 128], BF16)
    W["gC"] = f.sb("rw_gC", [64, 16])
    W["sets"] = []
    for si_ in range(2):
        S = {}
        for n in ("OA", "ON"):
            S[n] = [f.sb("rw_%s%d_%d" % (n, i, si_), [128, 4, 128], BF16) for i in range(5)]
        for n in ("D2s", "DT2s", "E1", "ET1", "E2", "ET2", "E4", "ET4", "Q", "QT", "P1", "P1T", "AkT", "RbT", "RkT"):
            S[n] = f.sb("rw_%s_%d" % (n, si_), [128, 4, 128], BF16)
        for n in ("X", "XT"):
            S[n] = [f.sb("rw_%s%d_%d" % (n, i, si_), [128, 4, 128], BF16) for i in range(2)]
        S["U16"] = [f.sb("rw_U16_%d_%d" % (i, si_), [128, 4, 64], BF16) for i in range(2)]
        W["sets"].append(S)
    W["sg"] = f.sb("rw_sg", [128, 160])
    W["sgT"] = f.sb("rw_sgT", [128, 2, 128], BF16)
    W["yo"] = f.sb("rw_yo", [128, 1024], BF16)
    return W


def _rw_chunk(self, l, c, d, P, W, H32, H16):
    f = self.f
    cst, cstb = self.cst, self.cstb
    pr = self.pad_row(c * 128)
    cur, prev, nxt = W["cur"], W["prev"], W["nxt"]
    f.load(cur, self.PRW[pr:pr + 128, :])
    f.load(prev, self.PRW[pr - 1:pr + 127, :])
    f.load(nxt, self.PRW[pr + 1:pr + 129, :])
    f.tt(prev, prev, nxt, ALU.add)
    f.stt(prev, prev, 0.5, cur, ALU.mult, ALU.subtract)
    f.tt(prev, prev, P["MU"], ALU.mult)
    f.tt(cur, cur, prev, ALU.add)
    s = cur
    r, k, v = s[:, 0:1024], s[:, 1152:2176], s[:, 2176:3200]
    wlo = s[:, 1024 + d * 64:1024 + (d + 1) * 64]
    alo = s[:, 3200 + d * 64:3200 + (d + 1) * 64]
    kk, t1, t2, ar, logw, E = W["kk"], W["t1"], W["t2"], W["ar"], W["logw"], W["E"]
    kd = t1
    s16, s16b = W["s16"], W["s16b"]
    h3 = lambda t: t.re("p (h n) -> p h n", h=16)
    f.tt(kk, k, P["KK"], ALU.mult)
    f.tt(t1, kk, kk, ALU.mult)
    f.reduce(s16, h3(t1), ALU.add)
    f.act(s16, s16, AF.Sqrt)
    f.ts(s16, s16, 1e-12, ALU.max)
    f.recip(s16, s16)
    f.tt(h3(kk), h3(kk), s16.re("p (h o) -> p h o", o=1).bc([128, 16, 64]), ALU.mult)
    tw, lT = W["tw"], W["lT"]
    f.act(tw[:, 0, :], wlo, AF.Tanh)
    f.copy(tw[:, 1, :], alo)
    pb = self.bank()
    for i in range(2):
        f.tr(pb[0:64, i * 128:(i + 1) * 128], tw[:, i, :], self.ident)
    f.copy(lT, pb[0:64, 0:256].re("p (i n) -> p i n", i=2), eng="act")
    for half in range(2):
        pb = self.bank()
        f.mm(pb, lT[:, 0, :], P["w2b"][:, half * 512:(half + 1) * 512])
        f.tt(t1[:, half * 512:(half + 1) * 512], pb, P["W0"][:, half * 512:(half + 1) * 512], ALU.add)
    f.act(t1, t1, AF.Sigmoid)
    f.ts(logw, t1, -0.6065306597126334, ALU.mult)
    for half in range(2):
        pb = self.bank()
        f.mm(pb, lT[:, 1, :], P["a2b"][:, half * 512:(half + 1) * 512])
        f.tt(ar[:, half * 512:(half + 1) * 512], pb, P["A0"][:, half * 512:(half + 1) * 512], ALU.add)
    f.act(ar, ar, AF.Sigmoid)
    f.stt(t2, ar, -1.0, P["KA"], ALU.add, ALU.mult)
    f.stt(kd, t2, 1.0, k, ALU.add, ALU.mult)
    incl = cst[:, 2 if d == 0 else 4, :]
    strict = cst[:, 3 if d == 0 else 5, :]
    Rt, Kt, Bt, At, V16 = W["Rt"], W["Kt"], W["Bt"], W["At"], W["V16"]
    cb = [self.bank(), self.bank()]
    for half in range(2):
        f.mm(cb[half], incl, logw[:, half * 512:(half + 1) * 512])
    for half in range(2):
        hs = slice(half * 512, (half + 1) * 512)
        f.act(E[:, hs], cb[half], AF.Exp)
        f.tt(Rt[:, hs], r[:, hs], E[:, hs], ALU.mult)
    for half in range(2):
        hs = slice(half * 512, (half + 1) * 512)
        f.act(E[:, hs], cb[half], AF.Exp, scale=-1.0)
    f.tt(Kt, kd, E, ALU.mult)
    f.tt(t2, kk, ar, ALU.mult)
    f.tt(Bt, t2, E, ALU.mult)
    cb = [self.bank(), self.bank()]
    for half in range(2):
        f.mm(cb[half], strict, logw[:, half * 512:(half + 1) * 512])
    for half in range(2):
        hs = slice(half * 512, (half + 1) * 512)
        f.act(E[:, hs], cb[half], AF.Exp)
    f.stt(At, kk, -1.0, E, ALU.mult, ALU.mult)
    f.copy(V16, v, eng="pool")
    gC = W["gC"]
    pb = self.bank()
    for h in range(16):
        f.mm(pb[0:64, h:h + 1], logw[:, h * 64:(h + 1) * 64], self.ones[:, 0:1])
    f.act(gC, pb[0:64, 0:16], AF.Exp)
    for src, dst in ((Rt, W["RT"]), (Kt, W["KT"]), (Bt, W["BT"]), (At, W["AT"])):
        for half in range(2):
            pbb = self.bank().bitcast(BF16)
            for hh in range(8):
                h = half * 8 + hh
                f.tr(pbb[0:64, hh * 128:(hh + 1) * 128], src[:, h * 64:(h + 1) * 64], self.identb)
            f.copy(dst[:, half * 8:(half + 1) * 8, :], pbb[0:64, :].re("p (h n) -> p h n", h=8), eng="act")
    RT, KT, BT, AT = W["RT"], W["KT"], W["BT"], W["AT"]
    m_strict_st = cst[:, 3 if d == 0 else 5, :]
    m_strict_ts = cst[:, 5 if d == 0 else 3, :]
    m_incl_st = cst[:, 2 if d == 0 else 4, :]
    y = W["y"]
    bc4 = lambda m: m.re("p (o n) -> p o n", o=1).bc([128, 4, 128])
    b4 = lambda b: b.re("p (h n) -> p h n", h=4)
    def grp(g, Wg):
        heads = [g * 4 + hh for hh in range(4)]
        mA = [cst[:, (8 if d == 0 else 13) + q, :] for q in range(5)]
        mN = [cst[:, (13 if d == 0 else 8) + q, :] for q in range(5)]
        idb4 = bc4(self.identb)

        def mm4(pb, L_, R_, start=True, stop=True):
            for hh in range(4):
                f.mm(pb[:, hh * 128:(hh + 1) * 128], L_[:, hh, :], R_[:, hh, :], start=start, stop=stop)

        def mm4I(pb, L_, R_, R2_):
            for hh in range(4):
                f.mm(pb[:, hh * 128:(hh + 1) * 128], L_[:, hh, :], R_[:, hh, :], start=True, stop=False)
                f.mm(pb[:, hh * 128:(hh + 1) * 128], self.identb, R2_[:, hh, :], start=False, stop=True)

        pa, pn = self.bank(), self.bank()
        for hh, h in enumerate(heads):
            f.mm(pa[:, hh * 128:(hh + 1) * 128], AT[:, h, :], BT[:, h, :])
            f.mm(pn[:, hh * 128:(hh + 1) * 128], BT[:, h, :], AT[:, h, :])
        for lv in range(5):
            f.tt(Wg["OA"][lv], b4(pa), bc4(mA[lv]), ALU.mult)
            f.tt(Wg["ON"][lv], b4(pn), bc4(mN[lv]), ALU.mult)
            yield
        for (dst, L_, R_, msk) in ((Wg["AkT"], KT, AT, m_strict_st), (Wg["RbT"], BT, RT, m_incl_st),
                                   (Wg["RkT"], KT, RT, m_incl_st)):
            pb = self.bank()
            for hh, h in enumerate(heads):
                f.mm(pb[:, hh * 128:(hh + 1) * 128], L_[:, h, :], R_[:, h, :])
            f.tt(dst, b4(pb), bc4(msk), ALU.mult)
            yield
        Dm, DTm = Wg["OA"][0], Wg["ON"][0]
        f.tt(Wg["E1"], Dm, idb4, ALU.add, eng="pool")
        f.tt(Wg["ET1"], DTm, idb4, ALU.add, eng="pool")
        p1, p2 = self.bank(), self.bank()
        mm4(p1, DTm, Dm)
        mm4(p2, Dm, DTm)
        f.copy(Wg["D2s"], b4(p1), eng="act")
        f.copy(Wg["DT2s"], b4(p2), eng="act")
        f.tt(Wg["E2"], b4(p1), idb4, ALU.add)
        f.tt(Wg["ET2"], b4(p2), idb4, ALU.add)
        yield
        p1, p2 = self.bank(), self.bank()
        mm4(p1, Wg["DT2s"], Wg["D2s"])
        mm4(p2, Wg["D2s"], Wg["DT2s"])
        f.tt(Wg["E4"], b4(p1), idb4, ALU.add)
        f.tt(Wg["ET4"], b4(p2), idb4, ALU.add)
        yield
        p1, p2 = self.bank(), self.bank()
        mm4(p1, Wg["ET2"], Wg["E4"])
        mm4(p2, Wg["E2"], Wg["ET4"])
        f.copy(Wg["Q"], b4(p1), eng="act")
        f.copy(Wg["QT"], b4(p2), eng="dve")
        yield
        p1, p2 = self.bank(), self.bank()
        mm4(p1, Wg["ET1"], Wg["Q"])
        mm4(p2, Wg["E1"], Wg["QT"])
        xi = 0
        X, XT = Wg["X"][0], Wg["XT"][0]
        f.copy(X, b4(p1), eng="act")
        f.copy(XT, b4(p2), eng="dve")
        yield
        for lv in range(1, 5):
            O, OT = Wg["OA"][lv], Wg["ON"][lv]
            p1 = self.bank()
            mm4(p1, OT, X)
            f.copy(Wg["P1"], b4(p1), eng="act")
            yield
            Xn, XTn = Wg["X"][xi ^ 1], Wg["XT"][xi ^ 1]
            if lv < 4:
                p2 = self.bank()
                mm4(p2, X, OT)
                f.copy(Wg["P1T"], b4(p2), eng="dve")
                yield
                p3 = self.bank()
                mm4I(p3, XT, Wg["P1"], X)
                f.copy(Xn, b4(p3), eng="act")
                p4 = self.bank()
                mm4I(p4, Wg["P1"], XT, XT)
                f.copy(XTn, b4(p4), eng="dve")
                yield
            else:
                p4 = self.bank()
                mm4I(p4, Wg["P1"], XT, XT)
                f.copy(XTn, b4(p4), eng="dve")
                yield
            xi ^= 1
            X, XT = Xn, XTn
        pb = self.bank()
        for hh, h in enumerate(heads):
            f.mm(pb[:, hh * 64:(hh + 1) * 64], AT[:, h, :], H16[g][:, hh, :], start=True, stop=False)
            f.mm(pb[:, hh * 64:(hh + 1) * 64], Wg["AkT"][:, hh, :], V16[:, h * 64:(h + 1) * 64], start=False, stop=True)
        f.copy(Wg["U16"][0], pb[:, 0:256].re("p (h n) -> p h n", h=4), eng="act")
        yield
        pb = self.bank()
        for hh in range(4):
            f.mm(pb[:, hh * 64:(hh + 1) * 64], XT[:, hh, :], Wg["U16"][0][:, hh, :])
        U = Wg["U16"][1]
        f.copy(U, pb[:, 0:256].re("p (h n) -> p h n", h=4), eng="act")
        yield
        pb = self.bank()
        for hh, h in enumerate(heads):
            f.mm(pb[:, hh * 64:(hh + 1) * 64], RT[:, h, :], H16[g][:, hh, :], start=True, stop=False)
            f.mm(pb[:, hh * 64:(hh + 1) * 64], Wg["RbT"][:, hh, :], U[:, hh, :], start=False, stop=False)
            f.mm(pb[:, hh * 64:(hh + 1) * 64], Wg["RkT"][:, hh, :], V16[:, h * 64:(h + 1) * 64], start=False, stop=True)
        f.copy(y[:, g * 256:(g + 1) * 256], pb[:, 0:256], eng="dve")
        yield
        pb = self.bank()
        for hh, h in enumerate(heads):
            f.mm(pb[0:64, hh * 64:(hh + 1) * 64], Bt[:, h * 64:(h + 1) * 64], U[:, hh, :], start=True, stop=False)
            f.mm(pb[0:64, hh * 64:(hh + 1) * 64], Kt[:, h * 64:(h + 1) * 64], V16[:, h * 64:(h + 1) * 64], start=False, stop=True)
        f.tt(H32[g], H32[g], pb[0:64, 0:256].re("p (h n) -> p h n", h=4), ALU.add)
        f.tt(H32[g], H32[g], gC[:, g * 4:(g + 1) * 4].re("p (h o) -> p h o", o=1).bc([64, 4, 64]), ALU.mult)
        f.copy(H16[g], H32[g], eng="pool")
    for pair in ((0, 1), (2, 3)):
        gens = [grp(g, W["sets"][g % 2]) for g in pair]
        alive = list(gens)
        while alive:
            for gen_ in list(alive):
                try:
                    next(gen_)
                except StopIteration:
                    alive.remove(gen_)
    if d == 0:
        f.store(self.YF[c * 128:(c + 1) * 128, :], y)
        return
    yf = t1
    f.load(yf, self.YF[c * 128:(c + 1) * 128, :])
    f.tt(y, y, yf, ALU.add)
    f.reduce(s16, h3(y), ALU.add)
    f.ts(s16, s16, 1.0 / 64, ALU.mult)
    f.tt(h3(y), h3(y), s16.re("p (h o) -> p h o", o=1).bc([128, 16, 64]), ALU.subtract)
    f.tt(t2, y, y, ALU.mult)
    f.reduce(s16b, h3(t2), ALU.add)
    f.ts(s16b, s16b, 1.0 / 64, ALU.mult, 64e-5, ALU.add)
    f.act(s16b, s16b, AF.Sqrt)
    f.recip(s16b, s16b)
    f.tt(h3(y), h3(y), s16b.re("p (h o) -> p h o", o=1).bc([128, 16, 64]), ALU.mult)
    f.tt(y, y, P["LNW"], ALU.mult)
    f.tt(y, y, P["LNB"], ALU.add)
    f.tt(t2, r, k, ALU.mult)
    f.tt(t2, t2, P["RK"], ALU.mult)
    f.reduce(s16, h3(t2), ALU.add)
    f.tt(h3(t2), h3(v), s16.re("p (h o) -> p h o", o=1).bc([128, 16, 64]), ALU.mult)
    f.tt(y, y, t2, ALU.add)
    sg, sgT = W["sg"], W["sgT"]
    f.act(sg, s[:, 3328:3488], AF.Sigmoid)
    pb = self.bank()
    f.tr(pb[:, 0:128], sg[:, 0:128], self.ident)
    f.tr(pb[0:32, 128:256], sg[:, 128:160], self.ident)
    f.copy(sgT[:, 0, :], pb[:, 0:128], eng="act")
    f.copy(sgT[0:32, 1, :], pb[0:32, 128:256], eng="act")
    for half in range(2):
        hs = slice(half * 512, (half + 1) * 512)
        pb = self.bank()
        f.mm(pb, sgT[:, 0, :], P["g2b"][:, 0, hs], start=True, stop=False)
        f.mm(pb, sgT[0:32, 1, :], P["g2b"][0:32, 1, hs], start=False, stop=True)
        f.tt(W["yo"][:, hs], y[:, hs], pb, ALU.mult)
    f.store(self.YRW[c * 128:(c + 1) * 128, :], W["yo"])


KB.stage_R = _stage_R
KB.rw_alloc_work = _rw_alloc_work
KB.rw_chunk = _rw_chunk


def _stage_final(self):
    f = self.f
    with f.scope():
        G = f.sb("fin_g", [128, D])
        self.bcast_load(G, self.norm_final_g, D)
        xts = [f.sb("fin_x%d" % i, [128, D]) for i in range(2)]
        sqs = f.sb("fin_sq", [128, D])
        ss = [f.sb("fin_ss%d" % i, [128, 1]) for i in range(2)]
        for i in range(self.n_lat // 128):
            xt, s1 = xts[i % 2], ss[i % 2]
            t0 = self.n_ctx + i * 128
            f.load(xt, self.XS[t0:t0 + 128, :])
            f.act(sqs, xt, AF.Square, accum=s1)
            f.ts(s1, s1, 1.0 / D, ALU.mult, 1e-6, ALU.add)
            f.act(s1, s1, AF.Sqrt)
            f.recip(s1, s1)
            f.stt(xt, xt, s1[:, 0:1], G, ALU.mult, ALU.mult)
            f.store(self.out[i * 128:(i + 1) * 128, :], xt)


KB.stage_final = _stage_final


ZOFF = RW_COLS
XOFF = RW_COLS + 2048
DOFF = RW_COLS + 2048 + 4096


def _ssm_rows(self, i):
    nctx_t = self.n_ctx // 128
    if i < nctx_t:
        return [(0, 128, i * 128, 1)]
    q = i - nctx_t
    rows = self.rows
    cpc = 128 // rows
    return [(ci * rows, rows, self.n_ctx + (q * cpc + ci), GRID_W) for ci in range(cpc)]


def _ssm_dram_rows(self, dram, i, cols=slice(None)):
    out = []
    for (p0, n, r0, stride) in self.ssm_rows(i):
        if stride == 1:
            out.append((slice(p0, p0 + n), dram[r0:r0 + n, cols]))
        else:
            lat = dram[self.n_ctx:self.T, cols].re("(r c) d -> c r d", c=GRID_W)
            out.append((slice(p0, p0 + n), lat[r0 - self.n_ctx]))
    return out


def _stage_S1(self, l):
    f = self.f
    T, NT, n_ctx = self.T, self.NT, self.n_ctx
    if not hasattr(self, "ZS"):
        self.ZS = f.dram("ZS", [T, 2048], BF16)
        self.DTA = f.dram("DTA", [T, 64])
        self.XSM = f.dram("XSM", [T, 2048], BF16)
        self.BTM = f.dram("BTM", [T, 1024], BF16)
        self.BCT = f.dram("BCT", [16, 128, T], BF16)
        self.YSF = f.dram("YSF", [T, 2048])
        self.YSSM = f.dram("YSSM", [T, 2048], BF16)
    groups = []
    nctx_t = n_ctx // 128
    i = 0
    while i < NT:
        lim = nctx_t if i < nctx_t else NT
        n = min(4, lim - i)
        groups.append((i, n))
        i += n
    with f.scope():
        self.alloc_h_tmps()
        mb = self.load_mod_bcast([0, 1])
        hTg = [f.sb("hTg%d" % gi, [128, 8, 512], BF16) for gi in range(len(groups))]
        xts = [f.sb("sxt%d" % j, [128, D]) for j in range(2)]
        for gi, (i0, n) in enumerate(groups):
            for j in range(n):
                i = i0 + j
                xt = xts[i % 2]
                for (ps_, ap) in self.ssm_dram_rows(self.XS, i):
                    f.load(xt[ps_, :], ap)
                r = 1 if i < nctx_t else 0
                self.emit_h(xt, mb[(r, 0)], mb[(r, 1)], hTg[gi][:, :, j * 128:(j + 1) * 128])
        zscope = f.scope()
        zscope.__enter__()
        wfs = [f.sb("swf%d" % j, [128, 8, 512]) for j in range(2)]
        wbs = [f.sb("swb%d" % j, [128, 8, 512], BF16) for j in range(2)]
        o16 = [f.sb("so16_%d" % j, [128, 512], BF16) for j in range(2)]
        o32 = [f.sb("so32_%d" % j, [128, 64]) for j in range(2)]
        blocks = [(ZOFF + c0, 512, "z") for c0 in range(0, 2048, 512)] + [(DOFF, 64, "dt")]
        for bi_, (c0, nb, kind) in enumerate(blocks):
            wf, wb = wfs[bi_ % 2], wbs[bi_ % 2]
            f.load(wf[:, :, 0:nb], self.w_in[l, :, c0:c0 + nb].re("(k p) n -> p k n", p=128))
            f.copy(wb[:, :, 0:nb], wf[:, :, 0:nb], eng="pool")
            for gi, (i0, n) in enumerate(groups):
                for j in range(n):
                    i = i0 + j
                    pb = self.bank()
                    for k in range(8):
                        f.mm(pb[:, 0:nb], hTg[gi][:, k, j * 128:(j + 1) * 128], wb[:, k, 0:nb], start=(k == 0), stop=(k == 7))
                    if kind == "z":
                        o = o16[i % 2]
                        f.act(o, pb, AF.Silu)
                        f.store(self.ZS[i * 128:(i + 1) * 128, c0 - ZOFF:c0 - ZOFF + 512], o)
                    else:
                        o = o32[i % 2]
                        f.copy(o, pb[:, 0:64], eng="act")
                        f.store(self.DTA[i * 128:(i + 1) * 128, :], o)
        zscope.__exit__(None, None, None)
        Lp = T + 8
        lat_off = n_ctx + 6
        XB = f.sb("XB", [128, Lp])
        CV = f.sb("CV", [128, Lp - 4])
        CVb = f.sb("CVb", [128, Lp - 4], BF16)
        f.memset(XB, 0.0)
        cws = [f.sb("cw%d" % j, [128, 6]) for j in range(2)]
        xwf = [f.sb("xwf%d" % j, [128, 8, 128]) for j in range(2)]
        xwb = [f.sb("xwb%d" % j, [128, 8, 128], BF16) for j in range(2)]
        stg = [f.sb("stg%d" % j, [128, 8, 128], BF16) for j in range(2)]
        si = 0
        for blk in range(32):
            wf, wb, cw = xwf[blk % 2], xwb[blk % 2], cws[blk % 2]
            c0 = XOFF + blk * 128
            f.load(wf, self.w_in[l, :, c0:c0 + 128].re("(k p) n -> p k n", p=128))
            f.copy(wb, wf, eng="pool")
            f.load(cw[:, 0:5], self.ssm_conv_w[l, :, blk * 128:(blk + 1) * 128].re("k c -> c k"), allow_slow_non_contiguous=True)
            f.load(cw[:, 5:6], self.ssm_conv_b[l, blk * 128:(blk + 1) * 128].re("(c o) -> c o", o=1), allow_slow_non_contiguous=True)
            for gi, (i0, n) in enumerate(groups):
                pb = self.bank()
                ng = n * 128
                for k in range(8):
                    f.mm(pb[:, 0:ng], wb[:, k, :], hTg[gi][:, k, 0:ng], start=(k == 0), stop=(k == 7))
                off = (2 if i0 < nctx_t else 6) + i0 * 128
                f.copy(XB[:, off:off + ng], pb[:, 0:ng], eng="act")
            Lc = Lp - 4
            f.ts(CV, XB[:, 0:Lc], cw[:, 0:1], ALU.mult, cw[:, 5:6], ALU.add)
            for kq in range(1, 5):
                f.stt(CV, XB[:, kq:kq + Lc], cw[:, kq:kq + 1], CV, ALU.mult, ALU.add)
            f.act(CVb, CV, AF.Silu)
            def cpos(i):
                return i * 128 if i < nctx_t else i * 128 + 4
            if blk < 24:
                dst = self.XSM if blk < 16 else self.BTM
                cc = (blk if blk < 16 else blk - 16) * 128
                for gi, (i0, n) in enumerate(groups):
                    if gi % 2 == 0:
                        pbb = self.bank().bitcast(BF16)
                        st = stg[si % 2]
                        si += 1
                    slot0 = (gi % 2) * 4
                    for j in range(n):
                        i = i0 + j
                        f.tr(pbb[:, (slot0 + j) * 128:(slot0 + j + 1) * 128], CVb[:, cpos(i):cpos(i) + 128], self.identb)
                    f.copy(st[:, slot0:slot0 + n, :], pbb[:, slot0 * 128:(slot0 + n) * 128].re("p (c n) -> p c n", n=128))
                    f.store(dst[i0 * 128:(i0 + n) * 128, cc:cc + 128].re("(c p) n -> p c n", p=128), st[:, slot0:slot0 + n, :])
            if blk >= 16:
                gidx = blk - 16
                f.store(self.BCT[gidx, :, 0:n_ctx], CVb[:, 0:n_ctx])
                f.store(self.BCT[gidx, :, n_ctx:T], CVb[:, n_ctx + 4:T + 4])


KB.ssm_rows = _ssm_rows
KB.ssm_dram_rows = _ssm_dram_rows
KB.stage_S1 = _stage_S1


def _stage_S2(self, l):
    f = self.f
    T, NT, n_ctx = self.T, self.NT, self.n_ctx
    nctx_t = n_ctx // 128
    cst = self.cst
    with f.scope():
        def bc(name, row_ap, n):
            t = f.sb(name, [128, n])
            self.bcast_load(t, row_ap, n)
            return t
        NW = bc("ssNW", self.ssm_norm_w[l, :], 2048)
        DTB = bc("ssDTB", self.ssm_dt_bias[l, :], 64)
        AL = bc("ssAL", self.ssm_a_log[l, :], 64)
        DS = bc("ssDS", self.ssm_d[l, :], 64)
        Aneg = f.sb("ssA", [128, 64])
        f.act(Aneg, AL, AF.Exp)
        f.ts(Aneg, Aneg, -1.0, ALU.mult)
        Dsk = f.sb("ssDsk", [128, 32])
        f.tt(Dsk, DS[:, 0:32], DS[:, 32:64], ALU.add)
        xs = f.sb("ss_xs", [128, 32, 64], BF16)
        Btm = f.sb("ss_Btm", [128, 1024], BF16)
        BT = f.sb("ss_BT", [128, 8, 128], BF16)
        CT = f.sb("ss_CT", [128, 8, 128], BF16)
        dtr = f.sb("ss_dtr", [128, 64])
        zs = f.sb("ss_zs", [128, 2048], BF16)
        ysf = f.sb("ss_ysf", [128, 2048])
        y = f.sb("ss_y", [128, 2048])
        yo = f.sb("ss_yo", [128, 2048], BF16)
        LT = f.sb("ss_LT", [128, 32, 128])
        xdt = f.sb("ss_xdt", [128, 32, 64], BF16)
        xe = f.sb("ss_xe", [128, 32, 64], BF16)
        dt = f.sb("ss_dt", [128, 32])
        ld = f.sb("ss_ld", [128, 32])
        cumT = f.sb("ss_cumT", [128, 32])
        dte = f.sb("ss_dte", [128, 32])
        cd = f.sb("ss_cd", [128, 32])
        SS = []
        for si_ in range(2):
            SS.append({"seg": f.sb("ss_seg%d" % si_, [128, 4, 128]), "Lm": f.sb("ss_L%d" % si_, [128, 4, 128]),
                       "Ec": f.sb("ss_Ec%d" % si_, [128, 4, 128]), "CBs": f.sb("ss_CBs%d" % si_, [128, 128]),
                       "MT": f.sb("ss_MT%d" % si_, [128, 4, 128], BF16), "CsT": f.sb("ss_CsT%d" % si_, [128, 4, 128], BF16)})
        g8 = f.sb("ss_g8", [128, 8])
        S32 = [f.sb("ss_S32_%d" % g, [128, 4, 64]) for g in range(8)]
        S16 = [f.sb("ss_S16_%d" % g, [128, 4, 64], BF16) for g in range(8)]
        bcl = lambda t: t.re("p (h o) -> p h o", o=1)
        for d in range(2):
            for g in range(8):
                f.memset(S32[g], 0.0)
                f.memset(S16[g], 0.0, eng="pool")
            ctx_chunks = list(range(nctx_t))
            lat_chunks = list(range(nctx_t, NT))
            order = ctx_chunks + lat_chunks if d == 0 else ctx_chunks[::-1] + lat_chunks[::-1]
            incl = cst[:, 2 if d == 0 else 4, :]
            negm = cst[:, 6 if d == 0 else 7, :]
            for i in order:
                u0 = i * 128
                f.load(xs, self.XSM[u0:u0 + 128, :].re("p (h n) -> p h n", h=32))
                f.load(Btm, self.BTM[u0:u0 + 128, :])
                f.load(BT, self.BCT[0:8, :, u0:u0 + 128].re("g n t -> n g t"))
                f.load(CT, self.BCT[8:16, :, u0:u0 + 128].re("g n t -> n g t"))
                f.load(dtr, self.DTA[u0:u0 + 128, :])
                if d == 1:
                    f.load(zs, self.ZS[u0:u0 + 128, :])
                    f.load(ysf, self.YSF[u0:u0 + 128, :])
                f.tt(dt, dtr[:, d * 32:(d + 1) * 32], DTB[:, d * 32:(d + 1) * 32], ALU.add)
                f.act(dt, dt, AF.Exp)
                f.ts(dt, dt, 1.0, ALU.add)
                f.act(dt, dt, AF.Ln)
                f.tt(ld, dt, Aneg[:, d * 32:(d + 1) * 32], ALU.mult)
                pb = self.bank()
                f.mm(pb[:, 0:32], incl, ld)
                f.mm(pb[:, 32:64], self.ones, ld)
                f.copy(cumT, pb[:, 0:32])
                f.tt(dte, pb[:, 32:64], cumT, ALU.subtract)
                f.act(dte, dte, AF.Exp)
                f.act(cd, pb[:, 32:64], AF.Exp)
                f.tt(LT, incl.re("p (o n) -> p o n", o=1).bc([128, 32, 128]), bcl(ld).bc([128, 32, 128]), ALU.mult)
                f.tt(xdt, xs, bcl(dt).bc([128, 32, 64]), ALU.mult)
                f.tt(xe, xdt, bcl(dte).bc([128, 32, 64]), ALU.mult)
                def sgrp(g, TS):
                    seg, Lm, Ec, CBs, MT, CsT = TS["seg"], TS["Lm"], TS["Ec"], TS["CBs"], TS["MT"], TS["CsT"]
                    hs = slice(4 * g, 4 * g + 4)
                    pc = self.bank()
                    f.mm(pc, self.ones, LT[:, hs, :].re("p h n -> p (h n)"))
                    pc4 = pc.re("p (h n) -> p h n", h=4)
                    f.tt(seg, pc4, negm.re("p (o n) -> p o n", o=1).bc([128, 4, 128]), ALU.add)
                    f.tt(seg, seg, bcl(cumT[:, hs]).bc([128, 4, 128]), ALU.subtract)
                    f.act(Lm, seg, AF.Exp)
                    f.act(Ec, pc4, AF.Exp)
                    yield
                    pcb = self.bank()
                    f.mm(pcb[:, 0:128], BT[:, g, :], CT[:, g, :])
                    f.copy(CBs, pcb[:, 0:128], eng="act")
                    f.tt(MT, Lm, CBs.re("p (o n) -> p o n", o=1).bc([128, 4, 128]), ALU.mult)
                    f.tt(CsT, Ec, CT[:, g, :].re("p (o n) -> p o n", o=1).bc([128, 4, 128]), ALU.mult)
                    yield
                    py = self.bank()
                    for hh in range(4):
                        h = 4 * g + hh
                        f.mm(py[:, hh * 64:(hh + 1) * 64], MT[:, hh, :], xdt[:, h, :], start=True, stop=False)
                        f.mm(py[:, hh * 64:(hh + 1) * 64], CsT[:, hh, :], S16[g][:, hh, :], start=False, stop=True)
                    if d == 0:
                        f.copy(y[:, g * 256:(g + 1) * 256], py[:, 0:256], eng="act")
                    else:
                        f.tt(y[:, g * 256:(g + 1) * 256], py[:, 0:256], ysf[:, g * 256:(g + 1) * 256], ALU.add)
                    pst = self.bank()
                    yield
                    f.mm(pst[:, 0:256], Btm[:, g * 128:(g + 1) * 128], xe[:, hs, :].re("p h n -> p (h n)"))
                    f.tt(S32[g], S32[g], bcl(cd[:, hs]).bc([128, 4, 64]), ALU.mult)
                    f.tt(S32[g], S32[g], pst[:, 0:256].re("p (h n) -> p h n", h=4), ALU.add)
                    f.copy(S16[g], S32[g], eng="pool")
                for pair in ((0, 1), (2, 3), (4, 5), (6, 7)):
                    alive = [sgrp(g, SS[g % 2]) for g in pair]
                    while alive:
                        for gen_ in list(alive):
                            try:
                                next(gen_)
                            except StopIteration:
                                alive.remove(gen_)
                if d == 0:
                    f.store(self.YSF[u0:u0 + 128, :], y)
                    continue
                y3 = y.re("p (h n) -> p h n", h=32)
                t3 = ysf.re("p (h n) -> p h n", h=32)
                f.tt(t3, xs, bcl(Dsk).bc([128, 32, 64]), ALU.mult)
                f.tt(y, y, ysf, ALU.add)
                f.tt(y, y, zs, ALU.mult)
                f.tt(ysf, y, y, ALU.mult)
                f.reduce(g8, ysf.re("p (g n) -> p g n", g=8), ALU.add)
                f.ts(g8, g8, 1.0 / 256, ALU.mult, 1e-5, ALU.add)
                f.act(g8, g8, AF.Sqrt)
                f.recip(g8, g8)
                f.tt(y.re("p (g n) -> p g n", g=8), y.re("p (g n) -> p g n", g=8), g8.re("p (g o) -> p g o", o=1).bc([128, 8, 256]), ALU.mult)
                f.tt(yo, y, NW, ALU.mult)
                for (ps_, ap) in self.ssm_dram_rows(self.YSSM, i):
                    f.store(ap, yo[ps_, :])


KB.stage_S2 = _stage_S2


def _load_w_bf16(self, dst, src_ap, nk, ncols, stage_tiles):
    f = self.f
    j = 0
    for k0 in range(0, nk, 8):
        kn = min(8, nk - k0)
        for c0 in range(0, ncols, 512):
            st = stage_tiles[j % 2]
            j += 1
            f.load(st[:, 0:kn, :], src_ap[k0 * 128:(k0 + kn) * 128, c0:c0 + 512].re("(k p) n -> p k n", p=128))
            f.copy(dst[:, k0:k0 + kn, c0:c0 + 512], st[:, 0:kn, :], eng="pool")


def _stage_G(self, l):
    f = self.f
    NT, n_ctx = self.NT, self.n_ctx
    with f.scope():
        Wrw = f.sb("gWrw", [128, 8, 1024], BF16)
        Wss = f.sb("gWss", [128, 16, 1024], BF16)
        Wo = f.sb("gWo", [128, 8, 1024], BF16)
        with f.scope():
            st = [f.sb("gst%d" % j, [128, 8, 512]) for j in range(2)]
            self.load_w_bf16(Wrw, self.w_branch_rw[l], 8, 1024, st)
            self.load_w_bf16(Wss, self.w_branch_ssm[l], 16, 1024, st)
            self.load_w_bf16(Wo, self.w_out[l], 8, 1024, st)
        gate = {}
        for r in range(2):
            t = f.sb("gGate%d" % r, [128, D])
            self.bcast_load(t, self.MODS[r, 2, :], D)
            gate[r] = t
        yin = [f.sb("g_yin%d" % j, [128, 3072], BF16) for j in range(2)]
        gsb = [f.sb("g_gs%d" % j, [128, 2048], BF16) for j in range(2)]
        xts = [f.sb("g_xt%d" % j, [128, D]) for j in range(2)]
        yT = f.sb("g_yT", [128, 24, 128], BF16)
        t1 = f.sb("g_t1", [128, 1024])
        t2 = f.sb("g_t2", [128, 1024])
        mg = f.sb("g_mg", [128, 1024], BF16)
        mT = f.sb("g_mT", [128, 8, 128], BF16)
        for i in range(NT):
            r = 1 if i * 128 < n_ctx else 0
            yi, gs, xt = yin[i % 2], gsb[i % 2], xts[i % 2]
            rs = slice(i * 128, (i + 1) * 128)
            f.load(yi[:, 0:1024], self.YRW[rs, :])
            f.load(yi[:, 1024:3072], self.YSSM[rs, :])
            f.load(gs, self.GS[rs, :])
            f.load(xt, self.XS[rs, :])
            for b3 in range(3):
                pbb = self.bank().bitcast(BF16)
                for j in range(8):
                    kq = b3 * 8 + j
                    f.tr(pbb[:, j * 128:(j + 1) * 128], yi[:, kq * 128:(kq + 1) * 128], self.identb)
                f.copy(yT[:, b3 * 8:(b3 + 1) * 8, :], pbb.re("p (k n) -> p k n", k=8), eng=("act" if b3 % 2 else "dve"))
            for half in range(2):
                hs = slice(half * 512, (half + 1) * 512)
                p1 = self.bank()
                for kq in range(8):
                    f.mm(p1, yT[:, kq, :], Wrw[:, kq, hs], start=(kq == 0), stop=(kq == 7))
                p2 = self.bank()
                for kq in range(16):
                    f.mm(p2, yT[:, 8 + kq, :], Wss[:, kq, hs], start=(kq == 0), stop=(kq == 15))
                f.tt(t1[:, hs], p1, gs[:, hs], ALU.mult)
                f.tt(t2[:, hs], p2, gs[:, 1024 + half * 512:1024 + (half + 1) * 512], ALU.mult)
            f.tt(mg, t1, t2, ALU.add)
            pbb = self.bank().bitcast(BF16)
            for j in range(8):
                f.tr(pbb[:, j * 128:(j + 1) * 128], mg[:, j * 128:(j + 1) * 128], self.identb)
            f.copy(mT, pbb.re("p (k n) -> p k n", k=8), eng="act")
            for half in range(2):
                hs = slice(half * 512, (half + 1) * 512)
                p1 = self.bank()
                for kq in range(8):
                    f.mm(p1, mT[:, kq, :], Wo[:, kq, hs], start=(kq == 0), stop=(kq == 7))
                f.tt(t1[:, hs], p1, gate[r][:, hs], ALU.mult)
            f.tt(xt, xt, t1, ALU.add)
            f.store(self.XS[rs, :], xt)


def _stage_F(self, l, tiles_per_pass=17):
    FDBG = ''
    f = self.f
    NT, n_ctx = self.NT, self.n_ctx
    with f.scope():
        mbg = self.load_mod_bcast([5])
        BR = f.sb("fBR", [128, 32])
        self.bcast_load(BR, self.b_router, 32)
        wr = f.sb("fwr", [128, 8, 32])
        f.load(wr, self.w_router.re("(k p) n -> p k n", p=128))
        sc = f.sb("f_sc", [128, 32])
        bi = f.sb("f_bi", [128, 32])
        m8 = f.sb("f_m8", [128, 4, 8])
        gsx = f.sb("f_gsx", [128, 4])
        gmx = f.sb("f_gmx", [128, 1])
        gm = f.sb("f_gm", [128, 4])
        tq = f.sb("f_tq", [128, 4])
        thr = f.sb("f_thr", [128, 1])
        em = f.sb("f_em", [128, 32])
        den = f.sb("f_den", [128, 1])
        for p0 in range(0, NT, tiles_per_pass):
            tiles = list(range(p0, min(NT, p0 + tiles_per_pass)))
            ng = (len(tiles) + 3) // 4
            with f.scope():
                hTg = [f.sb("f_hT%d" % gi, [128, 8, 512], BF16) for gi in range(ng)]
                acc = [f.sb("f_acc%d" % j, [128, D]) for j in range(len(tiles))]
                gw = [f.sb("f_gw%d" % j, [128, 32]) for j in range(len(tiles))]
                ph = f.scope()
                ph.__enter__()
                mb = self.load_mod_bcast([3, 4])
                self.alloc_h_tmps()
                hf = f.sb("f_hf", [128, 8, 128])
                xts = [f.sb("f_xt%d" % j, [128, D]) for j in range(2)]
                for j, i in enumerate(tiles):
                    xt = xts[j % 2]
                    r = 1 if i * 128 < n_ctx else 0
                    f.load(xt, self.XS[i * 128:(i + 1) * 128, :])
                    self.emit_h(xt, mb[(r, 3)], mb[(r, 4)], hTg[j // 4][:, :, (j % 4) * 128:(j % 4 + 1) * 128], hf_dst=hf)
                    f.memset(acc[j], 0.0, eng="pool")
                    pb = self.bank()
                    for k in range(8):
                        f.mm(pb[:, 0:32], hf[:, k, :], wr[:, k, :], start=(k == 0), stop=(k == 7))
                    f.act(sc, pb[:, 0:32], AF.Sigmoid)
                    f.tt(bi, sc, BR, ALU.add)
                    for g in range(4):
                        f.max8(m8[:, g, :], bi[:, g * 8:(g + 1) * 8])
                    f.tt(gsx, m8[:, :, 0], m8[:, :, 1], ALU.add)
                    f.reduce(gmx, gsx, ALU.max)
                    f.ts(gm, gsx, gmx[:, 0:1], ALU.is_equal)
                    f.tt(tq, gm, m8[:, :, 1], ALU.mult)
                    f.reduce(thr, tq, ALU.add)
                    f.ts(em, bi, thr[:, 0:1], ALU.is_ge)
                    f.tt(em.re("p (g n) -> p g n", g=4), em.re("p (g n) -> p g n", g=4),
                         gm.re("p (g o) -> p g o", o=1).bc([128, 4, 8]), ALU.mult)
                    f.tt(gw[j], sc, em, ALU.mult)
                    f.reduce(den, gw[j], ALU.add)
                    f.recip(den, den)
                    f.ts(gw[j], gw[j], den[:, 0:1], ALU.mult)
                ph.__exit__(None, None, None)
                ph = f.scope()
                ph.__enter__()
                w13f = [f.sb("f_w13f%d" % j, [128, 8, 512]) for j in range(2)]
                w2f = w13f[0].re("p k n -> p (k n)").re("p (k n) -> p k n", k=4)
                w1b = f.sb("f_w1b", [128, 8, 512], BF16)
                w3b = f.sb("f_w3b", [128, 8, 512], BF16)
                w2b = f.sb("f_w2b", [128, 4, 1024], BF16)
                sil = f.sb("f_sil", [128, 512], BF16)
                gT = f.sb("f_gT", [128, 4, 512], BF16)
                for e in range(0 if 'noexp' in FDBG else (2 if 'exp2' in FDBG else 32)):
                    f.load(w13f[0], self.exp_w1[l, e].re("(k p) n -> p k n", p=128))
                    f.copy(w1b, w13f[0], eng="pool")
                    f.load(w13f[1], self.exp_w3[l, e].re("(k p) n -> p k n", p=128))
                    f.copy(w3b, w13f[1], eng="pool")
                    f.load(w2f, self.exp_w2[l, e].re("(k p) n -> p k n", p=128))
                    f.copy(w2b, w2f, eng="pool")
                    for gi in range(ng):
                        nt_g = min(4, len(tiles) - gi * 4)
                        ntok = nt_g * 128
                        for fc in range(4):
                            fs = slice(fc * 128, (fc + 1) * 128)
                            pa, pb = self.bank(), self.bank()
                            for k in range(8):
                                f.mm(pa[:, 0:ntok], w1b[:, k, fs], hTg[gi][:, k, 0:ntok], start=(k == 0), stop=(k == 7))
                            for k in range(8):
                                f.mm(pb[:, 0:ntok], w3b[:, k, fs], hTg[gi][:, k, 0:ntok], start=(k == 0), stop=(k == 7))
                            f.act(sil[:, 0:ntok], pa[:, 0:ntok], AF.Silu)
                            f.tt(gT[:, fc, 0:ntok], sil[:, 0:ntok], pb[:, 0:ntok], ALU.mult)
                        for jj in range(nt_g):
                            j = gi * 4 + jj
                            for half in range(2):
                                hs = slice(half * 512, (half + 1) * 512)
                                po = self.bank()
                                for fc in range(4):
                                    f.mm(po, gT[:, fc, jj * 128:(jj + 1) * 128], w2b[:, fc, hs], start=(fc == 0), stop=(fc == 3))
                                f.stt(acc[j][:, hs], po, gw[j][:, e:e + 1], acc[j][:, hs], ALU.mult, ALU.add)
                ph.__exit__(None, None, None)
                xts = [f.sb("f_xt%d" % j, [128, D]) for j in range(2)]
                for j, i in enumerate(tiles):
                    xt = xts[j % 2]
                    r = 1 if i * 128 < n_ctx else 0
                    f.load(xt, self.XS[i * 128:(i + 1) * 128, :])
                    f.tt(acc[j], acc[j], mbg[(r, 5)], ALU.mult)
                    f.tt(xt, xt, acc[j], ALU.add)
                    f.store(self.XS[i * 128:(i + 1) * 128, :], xt)


KB.load_w_bf16 = _load_w_bf16
KB.stage_G = _stage_G
KB.stage_F = _stage_F


def build_program(n_ctx, n_lat, depth):
    K = KB(n_ctx, n_lat, depth)
    K.stage_init()
    for l in range(depth):
        K.stage_mod(l)
        K.stage_P(l)
        K.stage_R(l)
        K.stage_S1(l)
        K.stage_S2(l)
        K.stage_G(l)
        K.stage_F(l)
    K.stage_final()
    return K.finish()


def kernel(**inputs):
    from concourse.bass_utils import run_bass_kernel_spmd
    inp = {k: np.asarray(v) for k, v in inputs.items()}
    B, n_lat, _ = inp["x"].shape
    n_ctx = inp["ctx"].shape[1]
    L = inp["w_mod"].shape[0]
    nc = build_program(n_ctx, n_lat, L)
    consts = make_consts()
    n_cores = 8
    in_maps = []
    for core in range(n_cores):
        b = core // 2
        m = {}
        for k, v in inp.items():
            if k in ("x", "ctx"):
                m[k] = np.ascontiguousarray(v[b])
            elif k in ("c", "c_ctx"):
                continue
            else:
                m[k] = np.ascontiguousarray(v)
        m["c2"] = np.ascontiguousarray(np.stack([inp["c"][b], inp["c_ctx"]]))
        m["consts"] = consts
        m["rw_r_k"] = np.ascontiguousarray(inp["rw_r_k"].reshape(L, 1024))
        m["ssm_dt_bias"] = np.ascontiguousarray(inp["ssm_dt_bias"].reshape(L, 64))
        m["ssm_a_log"] = np.ascontiguousarray(inp["ssm_a_log"].reshape(L, 64))
        m["ssm_d"] = np.ascontiguousarray(inp["ssm_d"].reshape(L, 64))
        in_maps.append(m)
    res = run_bass_kernel_spmd(nc, in_maps, core_ids=list(range(n_cores)))
    out = np.stack([np.asarray(res.results[2 * b]["out"]) for b in range(B)], axis=0)
    return out.astype(np.float32)
```

```python
import numpy as np
import concourse.bass as bass
import concourse.mybir as mybir

F32 = mybir.dt.float32
BF16 = mybir.dt.bfloat16
I32 = mybir.dt.int32
AF = mybir.ActivationFunctionType
ALU = mybir.AluOpType
AX = mybir.AxisListType

N_DMA_SEMS = 6
N_ENG_SEMS = 8
SAME_ENGINE_SYNC = True


class Trk:
    __slots__ = ("w", "r")

    def __init__(self):
        self.w = None
        self.r = []


class V:
    __slots__ = ("t", "ap")

    def __init__(self, t, ap):
        self.t = t
        self.ap = ap

    def __getitem__(self, key):
        return V(self.t, self.ap[key])

    def re(self, s, **kw):
        return V(self.t, self.ap.rearrange(s, **kw))

    def bc(self, shape):
        return V(self.t, self.ap.to_broadcast(shape))

    def bitcast(self, dt):
        return V(self.t, self.ap.bitcast(dt))


class Fw:
    ENGS = ("pe", "act", "dve", "pool", "sp")

    def __init__(self, nc):
        self.nc = nc
        self.handles = {"pe": nc.tensor, "act": nc.scalar, "dve": nc.vector, "pool": nc.gpsimd, "sp": nc.sync}
        self.ops = {e: [] for e in self.ENGS}
        self.esem = {e: [nc.alloc_semaphore("s_%s%d" % (e, i)) for i in range(N_ENG_SEMS)] for e in self.ENGS}
        self.dsems = {}
        self.dcount = {}
        self.dnext = {}
        for q in ("sp", "pool", "act"):
            self.dsems[q] = [nc.alloc_semaphore("d_%s%d" % (q, i)) for i in range(N_DMA_SEMS)]
            self.dcount[q] = [0] * N_DMA_SEMS
            self.dnext[q] = 0
        self.n_alloc = 0
        self.stacks = []
        self.names = {}
        self.out_events = []

    def sb(self, name, shape, dtype=F32):
        self.n_alloc += 1
        nm = "%s_%d" % (name, self.n_alloc)
        self.names[name] = nm
        if self.stacks:
            h = self.stacks[-1].enter_context(self.nc.sbuf_tensor(nm, list(shape), dtype))
        else:
            h = self.nc.alloc_sbuf_tensor(nm, list(shape), dtype)
        return V(Trk(), h[tuple(slice(None) for _ in shape)])

    def scope(self):
        fw = self

        class _S:
            def __enter__(s):
                import contextlib
                fw.stacks.append(contextlib.ExitStack())
                return s

            def __exit__(s, *a):
                fw.barrier()
                fw.stacks.pop().close()
                return False
        return _S()

    def barrier(self):
        last = {}
        for e in self.ENGS:
            last[e] = -1
            for i in range(len(self.ops[e]) - 1, -1, -1):
                o = self.ops[e][i]
                if o["dma"] is None and o["fn"] is not None:
                    last[e] = i
                    break
        for e in self.ENGS:
            waits = {}
            for e2 in self.ENGS:
                if (e2 != e or (SAME_ENGINE_SYNC and e != 'pe')) and last[e2] >= 0:
                    waits[("e", e2)] = last[e2]
            for q in self.dsems:
                for si in range(N_DMA_SEMS):
                    if self.dcount[q][si] > 0:
                        waits[("d", q, si)] = self.dcount[q][si]
            self.ops[e].append({"waits": waits, "fn": None, "dma": None})

    def ps(self, name, shape, dtype=F32):
        self.n_alloc += 1
        h = self.nc.alloc_psum_tensor("%s_%d" % (name, self.n_alloc), list(shape), dtype)
        return V(Trk(), h[tuple(slice(None) for _ in shape)])

    def dram(self, name, shape, dtype=F32, kind="Internal"):
        h = self.nc.dram_tensor(name, list(shape), dtype, kind=kind)
        return V(Trk(), h.ap())

    def _deps(self, eng, reads, writes):
        deps = []
        for v in reads:
            if v.t.w is not None:
                deps.append(v.t.w)
        for v in writes:
            if v.t.w is not None:
                deps.append(v.t.w)
            deps.extend(v.t.r)
        out = {}
        for ev in deps:
            kind = ev[0]
            if kind == "e":
                _, e, idx = ev
                if e == eng and (not SAME_ENGINE_SYNC or e == "pe"):
                    continue
                k = ("e", e)
                out[k] = max(out.get(k, -1), idx)
            else:
                _, q, si, val = ev
                k = ("d", q, si)
                out[k] = max(out.get(k, -1), val)
        return out

    def _mark(self, ev, reads, writes):
        for v in reads:
            v.t.r.append(ev)
        for v in writes:
            v.t.w = ev
            v.t.r = []

    def op(self, eng, fn, outs, ins):
        outs = [o for o in outs if o is not None]
        ins = [i for i in ins if isinstance(i, V)]
        waits = self._deps(eng, ins, outs)
        idx = len(self.ops[eng])
        self.ops[eng].append({"waits": waits, "fn": fn, "dma": None})
        self._mark(("e", eng, idx), ins, outs)

    def dma(self, q, out, in_, **kw):
        waits = self._deps(q, [in_], [out])
        si = self.dnext[q]
        self.dnext[q] = (si + 1) % N_DMA_SEMS
        if self.dcount[q][si] > 0:
            k = ("d", q, si)
            waits[k] = max(waits.get(k, -1), self.dcount[q][si])
        self.dcount[q][si] += 1
        val = self.dcount[q][si]
        o_ap, i_ap = out.ap, in_.ap
        self.ops[q].append({"waits": waits, "fn": lambda h: h.dma_start(out=o_ap, in_=i_ap, **kw), "dma": (si, val)})
        ev = ("d", q, si, val)
        self._mark(ev, [in_], [out])
        return ev

    def finish(self):
        nc = self.nc
        need = {e: set() for e in self.ENGS}
        for e in self.ENGS:
            for o in self.ops[e]:
                for k, v in o["waits"].items():
                    if k[0] == "e":
                        need[k[1]].add(v)
        rank = {}
        for e in self.ENGS:
            r = 0
            for i in range(len(self.ops[e])):
                if i in need[e]:
                    rank[(e, i)] = (r % N_ENG_SEMS, r // N_ENG_SEMS + 1)
                    r += 1
        final_waits = []
        for q in self.dsems:
            for si in range(N_DMA_SEMS):
                if self.dcount[q][si] > 0:
                    final_waits.append((self.dsems[q][si], 16 * self.dcount[q][si]))
        ops, esem, dsems, handles = self.ops, self.esem, self.dsems, self.handles

        def replay(e, h):
            seen = {}
            for i, o in enumerate(ops[e]):
                for k, v in o["waits"].items():
                    if k[0] == "e":
                        si_, val = rank[(k[1], v)]
                        sem = esem[k[1]][si_]
                        k = ("e", k[1], si_)
                    else:
                        sem, val = dsems[k[1]][k[2]], 16 * v
                    if seen.get(k, -1) >= val:
                        continue
                    seen[k] = val
                    h.wait_ge(sem, val)
                if o["fn"] is None:
                    continue
                ins = o["fn"](h)
                if o["dma"] is not None:
                    ins.then_inc(dsems[e][o["dma"][0]], 16)
                elif (e, i) in rank:
                    ins.then_inc(esem[e][rank[(e, i)][0]], 1)

        with nc.Block() as block:
            @block.tensor
            def _(h):
                replay("pe", h)

            @block.scalar
            def _(h):
                replay("act", h)

            @block.vector
            def _(h):
                replay("dve", h)

            @block.gpsimd
            def _(h):
                replay("pool", h)

            @block.sync
            def _(h):
                replay("sp", h)
                for sem, val in final_waits:
                    h.wait_ge(sem, val)

    def mm(self, out, lhsT, rhs, start=True, stop=True):
        o, l, r = out.ap, lhsT.ap, rhs.ap
        self.op("pe", lambda h: h.matmul(o, l, r, start=start, stop=stop), [out], [lhsT, rhs] + ([] if start else [out]))

    def tr(self, out, in_, ident):
        o, i, d = out.ap, in_.ap, ident.ap
        self.op("pe", lambda h: h.transpose(o, i, d), [out], [in_, ident])

    def act(self, out, in_, func, bias=None, scale=None, accum=None, eng="act"):
        kw = {}
        if bias is not None:
            kw["bias"] = bias.ap if isinstance(bias, V) else bias
        if scale is not None:
            kw["scale"] = scale.ap if isinstance(scale, V) else scale
        if accum is not None:
            kw["accum_out"] = accum.ap
        o, i = out.ap, in_.ap
        self.op("act", lambda h: h.activation(o, i, func, **kw), [out, accum], [in_, bias, scale])

    def tt(self, out, a, b, op, eng="dve"):
        o, x, y = out.ap, a.ap, b.ap
        self.op(eng, lambda h: h.tensor_tensor(o, x, y, op), [out], [a, b])

    def ts(self, out, a, s1, op0, s2=None, op1=None, accum=None, eng="dve"):
        o, x = out.ap, a.ap
        c1 = s1.ap if isinstance(s1, V) else s1
        c2 = s2.ap if isinstance(s2, V) else s2
        kw = {}
        if op1 is not None:
            kw["op1"] = op1
        if accum is not None:
            kw["accum_out"] = accum.ap
        if op1 is None and accum is None:
            self.op(eng, lambda h: h.tensor_single_scalar(o, x, c1, op0), [out], [a, s1])
        else:
            self.op(eng, lambda h: h.tensor_scalar(o, x, c1, c2, op0, **kw), [out, accum], [a, s1, s2])

    def stt(self, out, a, s, b, op0, op1, eng="dve"):
        o, x, y = out.ap, a.ap, b.ap
        c = s.ap if isinstance(s, V) else s
        self.op(eng, lambda h: h.scalar_tensor_tensor(o, x, c, y, op0, op1), [out], [a, s, b])

    def copy(self, out, in_, eng="dve"):
        o, i = out.ap, in_.ap
        if eng == "act":
            self.op("act", lambda h: h.activation(o, i, AF.Identity), [out], [in_])
        else:
            self.op(eng, lambda h: h.tensor_copy(o, i), [out], [in_])

    def memset(self, out, val, eng="dve"):
        o = out.ap
        self.op(eng, lambda h: h.memset(o, val), [out], [])

    def reduce(self, out, in_, op, axis=AX.X, eng="dve"):
        o, i = out.ap, in_.ap
        self.op(eng, lambda h: h.tensor_reduce(o, i, axis, op), [out], [in_])

    def recip(self, out, in_):
        o, i = out.ap, in_.ap
        self.op("dve", lambda h: h.reciprocal(o, i), [out], [in_])

    def max8(self, out, in_):
        o, i = out.ap, in_.ap
        self.op("dve", lambda h: h.max(o, i), [out], [in_])

    def load(self, out, in_, q="sp", **kw):
        return self.dma(q, out, in_, **kw)

    def store(self, out, in_, q="pool", **kw):
        return self.dma(q, out, in_, **kw)


D = 1024
RW_COLS = 3488
SSM_COLS = 6208
IN_COLS = 11744
NCONST = 18
GRID_W = 64


def make_consts():
    c = np.zeros((128, NCONST, 128), np.float32)
    i = np.arange(128)
    s, t = i[:, None], i[None, :]
    c[:, 0] = np.eye(128)
    c[:, 1] = 1.0
    c[:, 2] = (s <= t)
    c[:, 3] = (s < t)
    c[:, 4] = (s >= t)
    c[:, 5] = (s > t)
    c[:, 6] = np.where(t >= s, 0.0, -1e30)
    c[:, 7] = np.where(t <= s, 0.0, -1e30)
    low = (s > t)
    c[:, 8] = low & (s // 8 == t // 8)
    for k in range(1, 5):
        bs = 8 << k
        c[:, 8 + k] = low & (s // bs == t // bs) & (s // (bs // 2) != t // (bs // 2))
    for k in range(5):
        c[:, 13 + k] = c[:, 8 + k].T
    return c


class KB:
    def __init__(self, n_ctx, n_lat, depth=2, stages="all"):
        self.n_ctx, self.n_lat, self.depth = n_ctx, n_lat, depth
        self.T = n_ctx + n_lat
        self.rows = n_lat // GRID_W
        self.NT = self.T // 128
        self.stages = stages
        nc = bass.Bass("TRN2", target_bir_lowering=False)
        self.nc = nc
        f = self.f = Fw(nc)
        T = self.T
        inp = lambda name, shape: f.dram(name, shape, F32, kind="ExternalInput")
        self.x = inp("x", [n_lat, D])
        self.ctx = inp("ctx", [n_ctx, D])
        self.c2 = inp("c2", [2, D])
        self.consts = inp("consts", [128, NCONST, 128])
        L = depth
        self.w_mod = inp("w_mod", [L, D, 6 * D])
        self.b_mod = inp("b_mod", [L, 6 * D])
        self.norm_mix_g = inp("norm_mix_g", [L, D])
        self.w_in = inp("w_in", [L, D, IN_COLS])
        self.rw_shift_mu = inp("rw_shift_mu", [L, RW_COLS])
        self.rw_w0 = inp("rw_w0", [L, 2, 1024])
        self.rw_w2 = inp("rw_w2", [L, 2, 64, 1024])
        self.rw_a0 = inp("rw_a0", [L, 2, 1024])
        self.rw_a2 = inp("rw_a2", [L, 2, 64, 1024])
        self.rw_g2 = inp("rw_g2", [L, 160, 1024])
        self.rw_k_k = inp("rw_k_k", [L, 1024])
        self.rw_k_a = inp("rw_k_a", [L, 1024])
        self.rw_r_k = inp("rw_r_k", [L, 1024])
        self.rw_ln_w = inp("rw_ln_w", [L, 1024])
        self.rw_ln_b = inp("rw_ln_b", [L, 1024])
        self.ssm_conv_w = inp("ssm_conv_w", [L, 5, 4096])
        self.ssm_conv_b = inp("ssm_conv_b", [L, 4096])
        self.ssm_dt_bias = inp("ssm_dt_bias", [L, 64])
        self.ssm_a_log = inp("ssm_a_log", [L, 64])
        self.ssm_d = inp("ssm_d", [L, 64])
        self.ssm_norm_w = inp("ssm_norm_w", [L, 2048])
        self.w_branch_rw = inp("w_branch_rw", [L, 1024, D])
        self.w_branch_ssm = inp("w_branch_ssm", [L, 2048, D])
        self.w_out = inp("w_out", [L, D, D])
        self.norm_ffn_g = inp("norm_ffn_g", [L, D])
        self.w_router = inp("w_router", [D, 32])
        self.b_router = inp("b_router", [32])
        self.exp_w1 = inp("exp_w1", [L, 32, D, 512])
        self.exp_w3 = inp("exp_w3", [L, 32, D, 512])
        self.exp_w2 = inp("exp_w2", [L, 32, 512, D])
        self.norm_final_g = inp("norm_final_g", [D])
        self.out = f.dram("out", [n_lat, D], F32, kind="ExternalOutput")
        self.XS = f.dram("XS", [T, D])
        self.MODS = f.dram("MODS", [2, 6, D])
        self.PRW = f.dram("PRW", [T + 6, RW_COLS])
        self.GS = f.dram("GS", [T, 2048], BF16)
        self.YF = f.dram("YF", [T, 1024])
        self.YRW = f.dram("YRW", [T, 1024], BF16, kind=("ExternalOutput" if stages == "debug" else "Internal"))
        self.cst = f.sb("cst", [128, NCONST, 128])
        self.cstb = f.sb("cstb", [128, NCONST, 128], BF16)
        self.banks = [f.ps("bank%d" % i, [128, 512]) for i in range(8)]
        self.bi = 0
        f.load(self.cst, self.consts)
        f.copy(self.cstb, self.cst)
        self.ident = self.cst[:, 0, :]
        self.identb = self.cstb[:, 0, :]
        self.ones = self.cst[:, 1, :]

    def bank(self):
        b = self.banks[self.bi]
        self.bi = (self.bi + 1) % 8
        return b

    def bcast_load(self, tile, row_ap, n):
        self.f.load(tile, V(row_ap.t, row_ap.ap.partition_broadcast(128)))

    def pad_row(self, t):
        return t + 1 if t < self.n_ctx else t + 3

    def stage_init(self):
        f = self.f
        f.store(self.XS[0:self.n_ctx, :], self.ctx, q="sp")
        f.store(self.XS[self.n_ctx:self.T, :], self.x, q="sp")

    def stage_mod(self, l):
        f = self.f
        with f.scope():
            cT = f.sb("cT", [128, 2, 8])
            scT = f.sb("scT", [128, 2, 8])
            with self.nc.allow_non_contiguous_dma("tiny transposed load"):
                f.load(cT, self.c2.re("r (k p) -> p r k", p=128), allow_slow_non_contiguous=True)
            f.act(scT, cT, AF.Silu)
            rows = [f.sb("mrow%d" % r, [1, 6 * D]) for r in range(2)]
            bm = f.sb("bm", [1, 6 * D])
            f.load(bm, self.b_mod[l:l + 1, :])
            g2 = f.sb("g2", [1, 2, D])
            f.load(g2[:, 0, :], self.norm_mix_g[l:l + 1, :])
            f.load(g2[:, 1, :], self.norm_ffn_g[l:l + 1, :])
            wts = [f.sb("wm%d" % i, [128, 8, 512]) for i in range(2)]
            for cb in range(12):
                wt = wts[cb % 2]
                f.load(wt, self.w_mod[l, :, cb * 512:(cb + 1) * 512].re("(k p) n -> p k n", p=128))
                for r in range(2):
                    pb = self.bank()
                    for k in range(8):
                        f.mm(pb[0:1, :], scT[:, r, k:k + 1], wt[:, k, :], start=(k == 0), stop=(k == 7))
                    f.tt(rows[r][:, cb * 512:(cb + 1) * 512], pb[0:1, :], bm[:, cb * 512:(cb + 1) * 512], ALU.add)
            for r in range(2):
                row = rows[r]
                o = f.sb("orow%d" % r, [1, 6, D])
                f.stt(o[:, 0, :], row[:, D:2 * D], 1.0, g2[:, 0, :], ALU.add, ALU.mult)
                f.copy(o[:, 1, :], row[:, 0:D])
                f.copy(o[:, 2, :], row[:, 2 * D:3 * D])
                f.stt(o[:, 3, :], row[:, 4 * D:5 * D], 1.0, g2[:, 1, :], ALU.add, ALU.mult)
                f.copy(o[:, 4, :], row[:, 3 * D:4 * D])
                f.copy(o[:, 5, :], row[:, 5 * D:6 * D])
                f.store(self.MODS[r:r + 1, :, :], o)

    def emit_h(self, xt, G, S, hT_dst, hf_dst=None):
        f = self.f
        sq, ss, hh = self.h_sq, self.h_ss, self.h_h
        f.act(sq, xt, AF.Square, accum=ss)
        f.ts(ss, ss, 1.0 / D, ALU.mult, 1e-6, ALU.add)
        f.act(ss, ss, AF.Sqrt)
        f.recip(ss, ss)
        f.stt(hh, xt, ss[:, 0:1], G, ALU.mult, ALU.mult)
        f.tt(hh, hh, S, ALU.add)
        for half in range(2):
            pb = self.bank()
            for k in range(4):
                kk = half * 4 + k
                f.tr(pb[:, k * 128:(k + 1) * 128], hh[:, kk * 128:(kk + 1) * 128], self.ident)
            if hf_dst is not None:
                f.copy(hf_dst[:, half * 4:(half + 1) * 4, :], pb.re("p (k n) -> p k n", k=4), eng="dve")
                f.copy(hT_dst[:, half * 4:(half + 1) * 4, :], hf_dst[:, half * 4:(half + 1) * 4, :], eng="act")
            else:
                f.copy(hT_dst[:, half * 4:(half + 1) * 4, :], pb.re("p (k n) -> p k n", k=4), eng="act")

    def alloc_h_tmps(self):
        f = self.f
        self.h_ss = f.sb("h_ss", [128, 1])
        self.h_h = f.sb("h_h", [128, D])
        self.h_sq = self.h_h

    def load_mod_bcast(self, idxs):
        f = self.f
        out = {}
        for r in range(2):
            for i in idxs:
                t = f.sb("mb%d_%d" % (r, i), [128, D])
                self.bcast_load(t, self.MODS[r, i, :], D)
                out[(r, i)] = t
        return out

    def stage_P(self, l):
        f = self.f
        T, NT = self.T, self.NT
        with f.scope():
            self.alloc_h_tmps()
            mb = self.load_mod_bcast([0, 1])
            hT = [f.sb("hT%d" % i, [128, 8, 128], BF16) for i in range(NT)]
            xts = [f.sb("xt%d" % i, [128, D]) for i in range(2)]
            for i in range(NT):
                xt = xts[i % 2]
                f.load(xt, self.XS[i * 128:(i + 1) * 128, :])
                r = 1 if i * 128 < self.n_ctx else 0
                self.emit_h(xt, mb[(r, 0)], mb[(r, 1)], hT[i])
            z = f.sb("zrow", [1, RW_COLS])
            f.memset(z, 0.0)
            for prow in (0, self.n_ctx + 1, self.n_ctx + 2, T + 3):
                f.store(self.PRW[prow:prow + 1, :], z)
            blocks = [(c0, min(512, RW_COLS - c0), "rw") for c0 in range(0, RW_COLS, 512)]
            blocks += [(RW_COLS + SSM_COLS + c0, 512, "gate") for c0 in range(0, 2048, 512)]
            wfs = [f.sb("wf%d" % i, [128, 8, 512]) for i in range(2)]
            wbs = [f.sb("wb%d" % i, [128, 8, 512], BF16) for i in range(2)]
            o32 = [f.sb("o32_%d" % i, [128, 512]) for i in range(2)]
            o16 = [f.sb("o16_%d" % i, [128, 512], BF16) for i in range(2)]
            for bi_, (c0, nb, kind) in enumerate(blocks):
                wf, wb = wfs[bi_ % 2], wbs[bi_ % 2]
                f.load(wf[:, :, 0:nb], self.w_in[l, :, c0:c0 + nb].re("(k p) n -> p k n", p=128))
                f.copy(wb[:, :, 0:nb], wf[:, :, 0:nb], eng="pool")
                for i in range(NT):
                    pb = self.bank()
                    for k in range(8):
                        f.mm(pb[:, 0:nb], hT[i][:, k, :], wb[:, k, 0:nb], start=(k == 0), stop=(k == 7))
                    if kind == "rw":
                        o = o32[i % 2]
                        f.copy(o[:, 0:nb], pb[:, 0:nb], eng="act")
                        pr = self.pad_row(i * 128)
                        f.store(self.PRW[pr:pr + 128, c0:c0 + nb], o[:, 0:nb])
                    else:
                        o = o16[i % 2]
                        f.act(o, pb, AF.Sigmoid)
                        g0 = c0 - RW_COLS - SSM_COLS
                        f.store(self.GS[i * 128:(i + 1) * 128, g0:g0 + 512], o)

    def finish(self):
        self.f.finish()
        return self.nc


def _stage_R(self, l):
    f = self.f
    NT = self.NT
    nctx_t = self.n_ctx // 128
    with f.scope():
        P = {}
        def bc(name, row_ap, n):
            t = f.sb(name, [128, n])
            self.bcast_load(t, row_ap, n)
            return t
        P["MU"] = bc("MU", self.rw_shift_mu[l, :], RW_COLS)
        P["KK"] = bc("KKp", self.rw_k_k[l, :], 1024)
        P["KA"] = bc("KAp", self.rw_k_a[l, :], 1024)
        P["RK"] = bc("RKp", self.rw_r_k[l, :], 1024)
        P["LNW"] = bc("LNW", self.rw_ln_w[l, :], 1024)
        P["LNB"] = bc("LNB", self.rw_ln_b[l, :], 1024)
        g2b = f.sb("g2b", [128, 2, 1024], BF16)
        with f.scope():
            g2f = f.sb("g2f", [128, 2, 1024])
            f.load(g2f[:, 0, :], self.rw_g2[l, 0:128, :])
            f.load(g2f[0:32, 1, :], self.rw_g2[l, 128:160, :])
            f.copy(g2b[:, 0, :], g2f[:, 0, :])
            f.copy(g2b[0:32, 1, :], g2f[0:32, 1, :])
        P["g2b"] = g2b
        W = self.rw_alloc_work()
        for d in range(2):
          with f.scope():
            P["W0"] = bc("W0p%d" % d, self.rw_w0[l, d, :], 1024)
            P["A0"] = bc("A0p%d" % d, self.rw_a0[l, d, :], 1024)
            lb = f.sb("lb%d" % d, [64, 2, 1024], BF16)
            with f.scope():
                lf = f.sb("lf%d" % d, [64, 2, 1024])
                f.load(lf[:, 0, :], self.rw_w2[l, d, :, :])
                f.load(lf[:, 1, :], self.rw_a2[l, d, :, :])
                f.copy(lb, lf)
            P["w2b"], P["a2b"] = lb[:, 0, :], lb[:, 1, :]
            H32 = [f.sb("H32_%d_%d" % (d, g), [64, 4, 64]) for g in range(4)]
            H16 = [f.sb("H16_%d_%d" % (d, g), [64, 4, 64], BF16) for g in range(4)]
            for g in range(4):
                f.memset(H32[g], 0.0)
                f.memset(H16[g], 0.0, eng="pool")
            ctx_chunks = list(range(nctx_t))
            lat_chunks = list(range(nctx_t, NT))
            order = ctx_chunks + lat_chunks if d == 0 else ctx_chunks[::-1] + lat_chunks[::-1]
            for c in order:
                self.rw_chunk(l, c, d, P, W, H32, H16)


def _rw_alloc_work(self):
    f = self.f
    W = {}
    for n in ("cur", "prev", "nxt"):
        W[n] = f.sb("rw_" + n, [128, RW_COLS])
    W["y"] = f.sb("rw_y", [128, 1024])
    for j, n in enumerate(("kk", "t1", "t2")):
        W[n] = W["prev"][:, j * 1024:(j + 1) * 1024]
    for j, n in enumerate(("ar", "logw", "E")):
        W[n] = W["nxt"][:, j * 1024:(j + 1) * 1024]
    for n in ("Rt", "Kt", "Bt", "At", "V16"):
        W[n] = f.sb("rw_" + n, [128, 1024], BF16)
    for n in ("RT", "KT", "BT", "AT"):
        W[n] = f.sb("rw_" + n, [64, 16, 128], BF16)
    W["s16"] = f.sb("rw_s16", [128, 16])
    W["s16b"] = f.sb("rw_s16b", [128, 16])
    W["tw"] = f.sb("rw_tw", [128, 2, 64])
    W["lT"] = f.sb("rw_lT", [64, 2, 128], BF16)
    W["gC"] = f.sb("rw_gC", [64, 16])
    W["sets"] = []
    for si_ in range(2):
        S = {}
        for n in ("OA", "ON"):
            S[n] = [f.sb("rw_%s%d_%d" % (n, i, si_), [128, 4, 128], BF16) for i in range(5)]
        for n in ("D2s", "DT2s", "E1", "ET1", "E2", "ET2", "E4", "ET4", "Q", "QT", "P1", "P1T", "AkT", "RbT", "RkT"):
            S[n] = f.sb("rw_%s_%d" % (n, si_), [128, 4, 128], BF16)
        for n in ("X", "XT"):
            S[n] = [f.sb("rw_%s%d_%d" % (n, i, si_), [128, 4, 128], BF16) for i in range(2)]
        S["U16"] = [f.sb("rw_U16_%d_%d" % (i, si_), [128, 4, 64], BF16) for i in range(2)]
        W["sets"].append(S)
    W["sg"] = f.sb("rw_sg", [128, 160])
    W["sgT"] = f.sb("rw_sgT", [128, 2, 128], BF16)
    W["yo"] = f.sb("rw_yo", [128, 1024], BF16)
    return W


def _rw_chunk(self, l, c, d, P, W, H32, H16):
    f = self.f
    cst, cstb = self.cst, self.cstb
    pr = self.pad_row(c * 128)
    cur, prev, nxt = W["cur"], W["prev"], W["nxt"]
    f.load(cur, self.PRW[pr:pr + 128, :])
    f.load(prev, self.PRW[pr - 1:pr + 127, :])
    f.load(nxt, self.PRW[pr + 1:pr + 129, :])
    f.tt(prev, prev, nxt, ALU.add)
    f.stt(prev, prev, 0.5, cur, ALU.mult, ALU.subtract)
    f.tt(prev, prev, P["MU"], ALU.mult)
    f.tt(cur, cur, prev, ALU.add)
    s = cur
    r, k, v = s[:, 0:1024], s[:, 1152:2176], s[:, 2176:3200]
    wlo = s[:, 1024 + d * 64:1024 + (d + 1) * 64]
    alo = s[:, 3200 + d * 64:3200 + (d + 1) * 64]
    kk, t1, t2, ar, logw, E = W["kk"], W["t1"], W["t2"], W["ar"], W["logw"], W["E"]
    kd = t1
    s16, s16b = W["s16"], W["s16b"]
    h3 = lambda t: t.re("p (h n) -> p h n", h=16)
    f.tt(kk, k, P["KK"], ALU.mult)
    f.tt(t1, kk, kk, ALU.mult)
    f.reduce(s16, h3(t1), ALU.add)
    f.act(s16, s16, AF.Sqrt)
    f.ts(s16, s16, 1e-12, ALU.max)
    f.recip(s16, s16)
    f.tt(h3(kk), h3(kk), s16.re("p (h o) -> p h o", o=1).bc([128, 16, 64]), ALU.mult)
    tw, lT = W["tw"], W["lT"]
    f.act(tw[:, 0, :], wlo, AF.Tanh)
    f.copy(tw[:, 1, :], alo)
    pb = self.bank()
    for i in range(2):
        f.tr(pb[0:64, i * 128:(i + 1) * 128], tw[:, i, :], self.ident)
    f.copy(lT, pb[0:64, 0:256].re("p (i n) -> p i n", i=2), eng="act")
    for half in range(2):
        pb = self.bank()
        f.mm(pb, lT[:, 0, :], P["w2b"][:, half * 512:(half + 1) * 512])
        f.tt(t1[:, half * 512:(half + 1) * 512], pb, P["W0"][:, half * 512:(half + 1) * 512], ALU.add)
    f.act(t1, t1, AF.Sigmoid)
    f.ts(logw, t1, -0.6065306597126334, ALU.mult)
    for half in range(2):
        pb = self.bank()
        f.mm(pb, lT[:, 1, :], P["a2b"][:, half * 512:(half + 1) * 512])
        f.tt(ar[:, half * 512:(half + 1) * 512], pb, P["A0"][:, half * 512:(half + 1) * 512], ALU.add)
    f.act(ar, ar, AF.Sigmoid)
    f.stt(t2, ar, -1.0, P["KA"], ALU.add, ALU.mult)
    f.stt(kd, t2, 1.0, k, ALU.add, ALU.mult)
    incl = cst[:, 2 if d == 0 else 4, :]
    strict = cst[:, 3 if d == 0 else 5, :]
    Rt, Kt, Bt, At, V16 = W["Rt"], W["Kt"], W["Bt"], W["At"], W["V16"]
    cb = [self.bank(), self.bank()]
    for half in range(2):
        f.mm(cb[half], incl, logw[:, half * 512:(half + 1) * 512])
    for half in range(2):
        hs = slice(half * 512, (half + 1) * 512)
        f.act(E[:, hs], cb[half], AF.Exp)
        f.tt(Rt[:, hs], r[:, hs], E[:, hs], ALU.mult)
    for half in range(2):
        hs = slice(half * 512, (half + 1) * 512)
        f.act(E[:, hs], cb[half], AF.Exp, scale=-1.0)
    f.tt(Kt, kd, E, ALU.mult)
    f.tt(t2, kk, ar, ALU.mult)
    f.tt(Bt, t2, E, ALU.mult)
    cb = [self.bank(), self.bank()]
    for half in range(2):
        f.mm(cb[half], strict, logw[:, half * 512:(half + 1) * 512])
    for half in range(2):
        hs = slice(half * 512, (half + 1) * 512)
        f.act(E[:, hs], cb[half], AF.Exp)
    f.stt(At, kk, -1.0, E, ALU.mult, ALU.mult)
    f.copy(V16, v, eng="pool")
    gC = W["gC"]
    pb = self.bank()
    for h in range(16):
        f.mm(pb[0:64, h:h + 1], logw[:, h * 64:(h + 1) * 64], self.ones[:, 0:1])
    f.act(gC, pb[0:64, 0:16], AF.Exp)
    for src, dst in ((Rt, W["RT"]), (Kt, W["KT"]), (Bt, W["BT"]), (At, W["AT"])):
        for half in range(2):
            pbb = self.bank().bitcast(BF16)
            for hh in range(8):
                h = half * 8 + hh
                f.tr(pbb[0:64, hh * 128:(hh + 1) * 128], src[:, h * 64:(h + 1) * 64], self.identb)
            f.copy(dst[:, half * 8:(half + 1) * 8, :], pbb[0:64, :].re("p (h n) -> p h n", h=8), eng="act")
    RT, KT, BT, AT = W["RT"], W["KT"], W["BT"], W["AT"]
    m_strict_st = cst[:, 3 if d == 0 else 5, :]
    m_strict_ts = cst[:, 5 if d == 0 else 3, :]
    m_incl_st = cst[:, 2 if d == 0 else 4, :]
    y = W["y"]
    bc4 = lambda m: m.re("p (o n) -> p o n", o=1).bc([128, 4, 128])
    b4 = lambda b: b.re("p (h n) -> p h n", h=4)
    def grp(g, Wg):
        heads = [g * 4 + hh for hh in range(4)]
        mA = [cst[:, (8 if d == 0 else 13) + q, :] for q in range(5)]
        mN = [cst[:, (13 if d == 0 else 8) + q, :] for q in range(5)]
        idb4 = bc4(self.identb)

        def mm4(pb, L_, R_, start=True, stop=True):
            for hh in range(4):
                f.mm(pb[:, hh * 128:(hh + 1) * 128], L_[:, hh, :], R_[:, hh, :], start=start, stop=stop)

        def mm4I(pb, L_, R_, R2_):
            for hh in range(4):
                f.mm(pb[:, hh * 128:(hh + 1) * 128], L_[:, hh, :], R_[:, hh, :], start=True, stop=False)
                f.mm(pb[:, hh * 128:(hh + 1) * 128], self.identb, R2_[:, hh, :], start=False, stop=True)

        pa, pn = self.bank(), self.bank()
        for hh, h in enumerate(heads):
            f.mm(pa[:, hh * 128:(hh + 1) * 128], AT[:, h, :], BT[:, h, :])
            f.mm(pn[:, hh * 128:(hh + 1) * 128], BT[:, h, :], AT[:, h, :])
        for lv in range(5):
            f.tt(Wg["OA"][lv], b4(pa), bc4(mA[lv]), ALU.mult)
            f.tt(Wg["ON"][lv], b4(pn), bc4(mN[lv]), ALU.mult)
            yield
        for (dst, L_, R_, msk) in ((Wg["AkT"], KT, AT, m_strict_st), (Wg["RbT"], BT, RT, m_incl_st),
                                   (Wg["RkT"], KT, RT, m_incl_st)):
            pb = self.bank()
            for hh, h in enumerate(heads):
                f.mm(pb[:, hh * 128:(hh + 1) * 128], L_[:, h, :], R_[:, h, :])
            f.tt(dst, b4(pb), bc4(msk), ALU.mult)
            yield
        Dm, DTm = Wg["OA"][0], Wg["ON"][0]
        f.tt(Wg["E1"], Dm, idb4, ALU.add, eng="pool")
        f.tt(Wg["ET1"], DTm, idb4, ALU.add, eng="pool")
        p1, p2 = self.bank(), self.bank()
        mm4(p1, DTm, Dm)
        mm4(p2, Dm, DTm)
        f.copy(Wg["D2s"], b4(p1), eng="act")
        f.copy(Wg["DT2s"], b4(p2), eng="act")
        f.tt(Wg["E2"], b4(p1), idb4, ALU.add)
        f.tt(Wg["ET2"], b4(p2), idb4, ALU.add)
        yield
        p1, p2 = self.bank(), self.bank()
        mm4(p1, Wg["DT2s"], Wg["D2s"])
        mm4(p2, Wg["D2s"], Wg["DT2s"])
        f.tt(Wg["E4"], b4(p1), idb4, ALU.add)
        f.tt(Wg["ET4"], b4(p2), idb4, ALU.add)
        yield
        p1, p2 = self.bank(), self.bank()
        mm4(p1, Wg["ET2"], Wg["E4"])
        mm4(p2, Wg["E2"], Wg["ET4"])
        f.copy(Wg["Q"], b4(p1), eng="act")
        f.copy(Wg["QT"], b4(p2), eng="dve")
        yield
        p1, p2 = self.bank(), self.bank()
        mm4(p1, Wg["ET1"], Wg["Q"])
        mm4(p2, Wg["E1"], Wg["QT"])
        xi = 0
        X, XT = Wg["X"][0], Wg["XT"][0]
        f.copy(X, b4(p1), eng="act")
        f.copy(XT, b4(p2), eng="dve")
        yield
        for lv in range(1, 5):
            O, OT = Wg["OA"][lv], Wg["ON"][lv]
            p1 = self.bank()
            mm4(p1, OT, X)
            f.copy(Wg["P1"], b4(p1), eng="act")
            yield
            Xn, XTn = Wg["X"][xi ^ 1], Wg["XT"][xi ^ 1]
            if lv < 4:
                p2 = self.bank()
                mm4(p2, X, OT)
                f.copy(Wg["P1T"], b4(p2), eng="dve")
                yield
                p3 = self.bank()
                mm4I(p3, XT, Wg["P1"], X)
                f.copy(Xn, b4(p3), eng="act")
                p4 = self.bank()
                mm4I(p4, Wg["P1"], XT, XT)
                f.copy(XTn, b4(p4), eng="dve")
                yield
            else:
                p4 = self.bank()
                mm4I(p4, Wg["P1"], XT, XT)
                f.copy(XTn, b4(p4), eng="dve")
                yield
            xi ^= 1
            X, XT = Xn, XTn
        pb = self.bank()
        for hh, h in enumerate(heads):
            f.mm(pb[:, hh * 64:(hh + 1) * 64], AT[:, h, :], H16[g][:, hh, :], start=True, stop=False)
            f.mm(pb[:, hh * 64:(hh + 1) * 64], Wg["AkT"][:, hh, :], V16[:, h * 64:(h + 1) * 64], start=False, stop=True)
        f.copy(Wg["U16"][0], pb[:, 0:256].re("p (h n) -> p h n", h=4), eng="act")
        yield
        pb = self.bank()
        for hh in range(4):
            f.mm(pb[:, hh * 64:(hh + 1) * 64], XT[:, hh, :], Wg["U16"][0][:, hh, :])
        U = Wg["U16"][1]
        f.copy(U, pb[:, 0:256].re("p (h n) -> p h n", h=4), eng="act")
        yield
        pb = self.bank()
        for hh, h in enumerate(heads):
            f.mm(pb[:, hh * 64:(hh + 1) * 64], RT[:, h, :], H16[g][:, hh, :], start=True, stop=False)
            f.mm(pb[:, hh * 64:(hh + 1) * 64], Wg["RbT"][:, hh, :], U[:, hh, :], start=False, stop=False)
            f.mm(pb[:, hh * 64:(hh + 1) * 64], Wg["RkT"][:, hh, :], V16[:, h * 64:(h + 1) * 64], start=False, stop=True)
        f.copy(y[:, g * 256:(g + 1) * 256], pb[:, 0:256], eng="dve")
        yield
        pb = self.bank()
        for hh, h in enumerate(heads):
            f.mm(pb[0:64, hh * 64:(hh + 1) * 64], Bt[:, h * 64:(h + 1) * 64], U[:, hh, :], start=True, stop=False)
            f.mm(pb[0:64, hh * 64:(hh + 1) * 64], Kt[:, h * 64:(h + 1) * 64], V16[:, h * 64:(h + 1) * 64], start=False, stop=True)
        f.tt(H32[g], H32[g], pb[0:64, 0:256].re("p (h n) -> p h n", h=4), ALU.add)
        f.tt(H32[g], H32[g], gC[:, g * 4:(g + 1) * 4].re("p (h o) -> p h o", o=1).bc([64, 4, 64]), ALU.mult)
        f.copy(H16[g], H32[g], eng="pool")
    for pair in ((0, 1), (2, 3)):
        gens = [grp(g, W["sets"][g % 2]) for g in pair]
        alive = list(gens)
        while alive:
            for gen_ in list(alive):
                try:
                    next(gen_)
                except StopIteration:
                    alive.remove(gen_)
    if d == 0:
        f.store(self.YF[c * 128:(c + 1) * 128, :], y)
        return
    yf = t1
    f.load(yf, self.YF[c * 128:(c + 1) * 128, :])
    f.tt(y, y, yf, ALU.add)
    f.reduce(s16, h3(y), ALU.add)
    f.ts(s16, s16, 1.0 / 64, ALU.mult)
    f.tt(h3(y), h3(y), s16.re("p (h o) -> p h o", o=1).bc([128, 16, 64]), ALU.subtract)
    f.tt(t2, y, y, ALU.mult)
    f.reduce(s16b, h3(t2), ALU.add)
    f.ts(s16b, s16b, 1.0 / 64, ALU.mult, 64e-5, ALU.add)
    f.act(s16b, s16b, AF.Sqrt)
    f.recip(s16b, s16b)
    f.tt(h3(y), h3(y), s16b.re("p (h o) -> p h o", o=1).bc([128, 16, 64]), ALU.mult)
    f.tt(y, y, P["LNW"], ALU.mult)
    f.tt(y, y, P["LNB"], ALU.add)
    f.tt(t2, r, k, ALU.mult)
    f.tt(t2, t2, P["RK"], ALU.mult)
    f.reduce(s16, h3(t2), ALU.add)
    f.tt(h3(t2), h3(v), s16.re("p (h o) -> p h o", o=1).bc([128, 16, 64]), ALU.mult)
    f.tt(y, y, t2, ALU.add)
    sg, sgT = W["sg"], W["sgT"]
    f.act(sg, s[:, 3328:3488], AF.Sigmoid)
    pb = self.bank()
    f.tr(pb[:, 0:128], sg[:, 0:128], self.ident)
    f.tr(pb[0:32, 128:256], sg[:, 128:160], self.ident)
    f.copy(sgT[:, 0, :], pb[:, 0:128], eng="act")
    f.copy(sgT[0:32, 1, :], pb[0:32, 128:256], eng="act")
    for half in range(2):
        hs = slice(half * 512, (half + 1) * 512)
        pb = self.bank()
        f.mm(pb, sgT[:, 0, :], P["g2b"][:, 0, hs], start=True, stop=False)
        f.mm(pb, sgT[0:32, 1, :], P["g2b"][0:32, 1, hs], start=False, stop=True)
        f.tt(W["yo"][:, hs], y[:, hs], pb, ALU.mult)
    f.store(self.YRW[c * 128:(c + 1) * 128, :], W["yo"])


KB.stage_R = _stage_R
KB.rw_alloc_work = _rw_alloc_work
KB.rw_chunk = _rw_chunk


def _stage_final(self):
    f = self.f
    with f.scope():
        G = f.sb("fin_g", [128, D])
        self.bcast_load(G, self.norm_final_g, D)
        xts = [f.sb("fin_x%d" % i, [128, D]) for i in range(2)]
        sqs = f.sb("fin_sq", [128, D])
        ss = [f.sb("fin_ss%d" % i, [128, 1]) for i in range(2)]
        for i in range(self.n_lat // 128):
            xt, s1 = xts[i % 2], ss[i % 2]
            t0 = self.n_ctx + i * 128
            f.load(xt, self.XS[t0:t0 + 128, :])
            f.act(sqs, xt, AF.Square, accum=s1)
            f.ts(s1, s1, 1.0 / D, ALU.mult, 1e-6, ALU.add)
            f.act(s1, s1, AF.Sqrt)
            f.recip(s1, s1)
            f.stt(xt, xt, s1[:, 0:1], G, ALU.mult, ALU.mult)
            f.store(self.out[i * 128:(i + 1) * 128, :], xt)


KB.stage_final = _stage_final


ZOFF = RW_COLS
XOFF = RW_COLS + 2048
DOFF = RW_COLS + 2048 + 4096


def _ssm_rows(self, i):
    nctx_t = self.n_ctx // 128
    if i < nctx_t:
        return [(0, 128, i * 128, 1)]
    q = i - nctx_t
    rows = self.rows
    cpc = 128 // rows
    return [(ci * rows, rows, self.n_ctx + (q * cpc + ci), GRID_W) for ci in range(cpc)]


def _ssm_dram_rows(self, dram, i, cols=slice(None)):
    out = []
    for (p0, n, r0, stride) in self.ssm_rows(i):
        if stride == 1:
            out.append((slice(p0, p0 + n), dram[r0:r0 + n, cols]))
        else:
            lat = dram[self.n_ctx:self.T, cols].re("(r c) d -> c r d", c=GRID_W)
            out.append((slice(p0, p0 + n), lat[r0 - self.n_ctx]))
    return out


def _stage_S1(self, l):
    f = self.f
    T, NT, n_ctx = self.T, self.NT, self.n_ctx
    if not hasattr(self, "ZS"):
        self.ZS = f.dram("ZS", [T, 2048], BF16)
        self.DTA = f.dram("DTA", [T, 64])
        self.XSM = f.dram("XSM", [T, 2048], BF16)
        self.BTM = f.dram("BTM", [T, 1024], BF16)
        self.BCT = f.dram("BCT", [16, 128, T], BF16)
        self.YSF = f.dram("YSF", [T, 2048])
        self.YSSM = f.dram("YSSM", [T, 2048], BF16)
    groups = []
    nctx_t = n_ctx // 128
    i = 0
    while i < NT:
        lim = nctx_t if i < nctx_t else NT
        n = min(4, lim - i)
        groups.append((i, n))
        i += n
    with f.scope():
        self.alloc_h_tmps()
        mb = self.load_mod_bcast([0, 1])
        hTg = [f.sb("hTg%d" % gi, [128, 8, 512], BF16) for gi in range(len(groups))]
        xts = [f.sb("sxt%d" % j, [128, D]) for j in range(2)]
        for gi, (i0, n) in enumerate(groups):
            for j in range(n):
                i = i0 + j
                xt = xts[i % 2]
                for (ps_, ap) in self.ssm_dram_rows(self.XS, i):
                    f.load(xt[ps_, :], ap)
                r = 1 if i < nctx_t else 0
                self.emit_h(xt, mb[(r, 0)], mb[(r, 1)], hTg[gi][:, :, j * 128:(j + 1) * 128])
        zscope = f.scope()
        zscope.__enter__()
        wfs = [f.sb("swf%d" % j, [128, 8, 512]) for j in range(2)]
        wbs = [f.sb("swb%d" % j, [128, 8, 512], BF16) for j in range(2)]
        o16 = [f.sb("so16_%d" % j, [128, 512], BF16) for j in range(2)]
        o32 = [f.sb("so32_%d" % j, [128, 64]) for j in range(2)]
        blocks = [(ZOFF + c0, 512, "z") for c0 in range(0, 2048, 512)] + [(DOFF, 64, "dt")]
        for bi_, (c0, nb, kind) in enumerate(blocks):
            wf, wb = wfs[bi_ % 2], wbs[bi_ % 2]
            f.load(wf[:, :, 0:nb], self.w_in[l, :, c0:c0 + nb].re("(k p) n -> p k n", p=128))
            f.copy(wb[:, :, 0:nb], wf[:, :, 0:nb], eng="pool")
            for gi, (i0, n) in enumerate(groups):
                for j in range(n):
                    i = i0 + j
                    pb = self.bank()
                    for k in range(8):
                        f.mm(pb[:, 0:nb], hTg[gi][:, k, j * 128:(j + 1) * 128], wb[:, k, 0:nb], start=(k == 0), stop=(k == 7))
                    if kind == "z":
                        o = o16[i % 2]
                        f.act(o, pb, AF.Silu)
                        f.store(self.ZS[i * 128:(i + 1) * 128, c0 - ZOFF:c0 - ZOFF + 512], o)
                    else:
                        o = o32[i % 2]
                        f.copy(o, pb[:, 0:64], eng="act")
                        f.store(self.DTA[i * 128:(i + 1) * 128, :], o)
        zscope.__exit__(None, None, None)
        Lp = T + 8
        lat_off = n_ctx + 6
        XB = f.sb("XB", [128, Lp])
        CV = f.sb("CV", [128, Lp - 4])
        CVb = f.sb("CVb", [128, Lp - 4], BF16)
        f.memset(XB, 0.0)
        cws = [f.sb("cw%d" % j, [128, 6]) for j in range(2)]
        xwf = [f.sb("xwf%d" % j, [128, 8, 128]) for j in range(2)]
        xwb = [f.sb("xwb%d" % j, [128, 8, 128], BF16) for j in range(2)]
        stg = [f.sb("stg%d" % j, [128, 8, 128], BF16) for j in range(2)]
        si = 0
        for blk in range(32):
            wf, wb, cw = xwf[blk % 2], xwb[blk % 2], cws[blk % 2]
            c0 = XOFF + blk * 128
            f.load(wf, self.w_in[l, :, c0:c0 + 128].re("(k p) n -> p k n", p=128))
            f.copy(wb, wf, eng="pool")
            f.load(cw[:, 0:5], self.ssm_conv_w[l, :, blk * 128:(blk + 1) * 128].re("k c -> c k"), allow_slow_non_contiguous=True)
            f.load(cw[:, 5:6], self.ssm_conv_b[l, blk * 128:(blk + 1) * 128].re("(c o) -> c o", o=1), allow_slow_non_contiguous=True)
            for gi, (i0, n) in enumerate(groups):
                pb = self.bank()
                ng = n * 128
                for k in range(8):
                    f.mm(pb[:, 0:ng], wb[:, k, :], hTg[gi][:, k, 0:ng], start=(k == 0), stop=(k == 7))
                off = (2 if i0 < nctx_t else 6) + i0 * 128
                f.copy(XB[:, off:off + ng], pb[:, 0:ng], eng="act")
            Lc = Lp - 4
            f.ts(CV, XB[:, 0:Lc], cw[:, 0:1], ALU.mult, cw[:, 5:6], ALU.add)
            for kq in range(1, 5):
                f.stt(CV, XB[:, kq:kq + Lc], cw[:, kq:kq + 1], CV, ALU.mult, ALU.add)
            f.act(CVb, CV, AF.Silu)
            def cpos(i):
                return i * 128 if i < nctx_t else i * 128 + 4
            if blk < 24:
                dst = self.XSM if blk < 16 else self.BTM
                cc = (blk if blk < 16 else blk - 16) * 128
                for gi, (i0, n) in enumerate(groups):
                    if gi % 2 == 0:
                        pbb = self.bank().bitcast(BF16)
                        st = stg[si % 2]
                        si += 1
                    slot0 = (gi % 2) * 4
                    for j in range(n):
                        i = i0 + j
                        f.tr(pbb[:, (slot0 + j) * 128:(slot0 + j + 1) * 128], CVb[:, cpos(i):cpos(i) + 128], self.identb)
                    f.copy(st[:, slot0:slot0 + n, :], pbb[:, slot0 * 128:(slot0 + n) * 128].re("p (c n) -> p c n", n=128))
                    f.store(dst[i0 * 128:(i0 + n) * 128, cc:cc + 128].re("(c p) n -> p c n", p=128), st[:, slot0:slot0 + n, :])
            if blk >= 16:
                gidx = blk - 16
                f.store(self.BCT[gidx, :, 0:n_ctx], CVb[:, 0:n_ctx])
                f.store(self.BCT[gidx, :, n_ctx:T], CVb[:, n_ctx + 4:T + 4])


KB.ssm_rows = _ssm_rows
KB.ssm_dram_rows = _ssm_dram_rows
KB.stage_S1 = _stage_S1


def _stage_S2(self, l):
    f = self.f
    T, NT, n_ctx = self.T, self.NT, self.n_ctx
    nctx_t = n_ctx // 128
    cst = self.cst
    with f.scope():
        def bc(name, row_ap, n):
            t = f.sb(name, [128, n])
            self.bcast_load(t, row_ap, n)
            return t
        NW = bc("ssNW", self.ssm_norm_w[l, :], 2048)
        DTB = bc("ssDTB", self.ssm_dt_bias[l, :], 64)
        AL = bc("ssAL", self.ssm_a_log[l, :], 64)
        DS = bc("ssDS", self.ssm_d[l, :], 64)
        Aneg = f.sb("ssA", [128, 64])
        f.act(Aneg, AL, AF.Exp)
        f.ts(Aneg, Aneg, -1.0, ALU.mult)
        Dsk = f.sb("ssDsk", [128, 32])
        f.tt(Dsk, DS[:, 0:32], DS[:, 32:64], ALU.add)
        xs = f.sb("ss_xs", [128, 32, 64], BF16)
        Btm = f.sb("ss_Btm", [128, 1024], BF16)
        BT = f.sb("ss_BT", [128, 8, 128], BF16)
        CT = f.sb("ss_CT", [128, 8, 128], BF16)
        dtr = f.sb("ss_dtr", [128, 64])
        zs = f.sb("ss_zs", [128, 2048], BF16)
        ysf = f.sb("ss_ysf", [128, 2048])
        y = f.sb("ss_y", [128, 2048])
        yo = f.sb("ss_yo", [128, 2048], BF16)
        LT = f.sb("ss_LT", [128, 32, 128])
        xdt = f.sb("ss_xdt", [128, 32, 64], BF16)
        xe = f.sb("ss_xe", [128, 32, 64], BF16)
        dt = f.sb("ss_dt", [128, 32])
        ld = f.sb("ss_ld", [128, 32])
        cumT = f.sb("ss_cumT", [128, 32])
        dte = f.sb("ss_dte", [128, 32])
        cd = f.sb("ss_cd", [128, 32])
        SS = []
        for si_ in range(4):
            SS.append({"seg": f.sb("ss_seg%d" % si_, [128, 4, 128]), "Lm": f.sb("ss_L%d" % si_, [128, 4, 128]),
                       "Ec": f.sb("ss_Ec%d" % si_, [128, 4, 128]), "CBs": f.sb("ss_CBs%d" % si_, [128, 128]),
                       "MT": f.sb("ss_MT%d" % si_, [128, 4, 128], BF16), "CsT": f.sb("ss_CsT%d" % si_, [128, 4, 128], BF16)})
        g8 = f.sb("ss_g8", [128, 8])
        S32 = [f.sb("ss_S32_%d" % g, [128, 4, 64]) for g in range(8)]
        S16 = [f.sb("ss_S16_%d" % g, [128, 4, 64], BF16) for g in range(8)]
        bcl = lambda t: t.re("p (h o) -> p h o", o=1)
        for d in range(2):
            for g in range(8):
                f.memset(S32[g], 0.0)
                f.memset(S16[g], 0.0, eng="pool")
            ctx_chunks = list(range(nctx_t))
            lat_chunks = list(range(nctx_t, NT))
            order = ctx_chunks + lat_chunks if d == 0 else ctx_chunks[::-1] + lat_chunks[::-1]
            incl = cst[:, 2 if d == 0 else 4, :]
            negm = cst[:, 6 if d == 0 else 7, :]
            for i in order:
                u0 = i * 128
                f.load(xs, self.XSM[u0:u0 + 128, :].re("p (h n) -> p h n", h=32))
                f.load(Btm, self.BTM[u0:u0 + 128, :])
                f.load(BT, self.BCT[0:8, :, u0:u0 + 128].re("g n t -> n g t"))
                f.load(CT, self.BCT[8:16, :, u0:u0 + 128].re("g n t -> n g t"))
                f.load(dtr, self.DTA[u0:u0 + 128, :])
                if d == 1:
                    f.load(zs, self.ZS[u0:u0 + 128, :])
                    f.load(ysf, self.YSF[u0:u0 + 128, :])
                f.tt(dt, dtr[:, d * 32:(d + 1) * 32], DTB[:, d * 32:(d + 1) * 32], ALU.add)
                f.act(dt, dt, AF.Exp)
                f.ts(dt, dt, 1.0, ALU.add)
                f.act(dt, dt, AF.Ln)
                f.tt(ld, dt, Aneg[:, d * 32:(d + 1) * 32], ALU.mult)
                pb = self.bank()
                f.mm(pb[:, 0:32], incl, ld)
                f.mm(pb[:, 32:64], self.ones, ld)
                f.copy(cumT, pb[:, 0:32])
                f.tt(dte, pb[:, 32:64], cumT, ALU.subtract)
                f.act(dte, dte, AF.Exp)
                f.act(cd, pb[:, 32:64], AF.Exp)
                f.tt(LT, incl.re("p (o n) -> p o n", o=1).bc([128, 32, 128]), bcl(ld).bc([128, 32, 128]), ALU.mult)
                f.tt(xdt, xs, bcl(dt).bc([128, 32, 64]), ALU.mult)
                f.tt(xe, xdt, bcl(dte).bc([128, 32, 64]), ALU.mult)
                def sgrp(g, TS):
                    seg, Lm, Ec, CBs, MT, CsT = TS["seg"], TS["Lm"], TS["Ec"], TS["CBs"], TS["MT"], TS["CsT"]
                    hs = slice(4 * g, 4 * g + 4)
                    pc = self.bank()
                    f.mm(pc, self.ones, LT[:, hs, :].re("p h n -> p (h n)"))
                    pc4 = pc.re("p (h n) -> p h n", h=4)
                    f.tt(seg, pc4, negm.re("p (o n) -> p o n", o=1).bc([128, 4, 128]), ALU.add)
                    f.tt(seg, seg, bcl(cumT[:, hs]).bc([128, 4, 128]), ALU.subtract)
                    f.act(Lm, seg, AF.Exp)
                    f.act(Ec, pc4, AF.Exp)
                    yield
                    pcb = self.bank()
                    f.mm(pcb[:, 0:128], BT[:, g, :], CT[:, g, :])
                    f.copy(CBs, pcb[:, 0:128], eng="act")
                    f.tt(MT, Lm, CBs.re("p (o n) -> p o n", o=1).bc([128, 4, 128]), ALU.mult)
                    f.tt(CsT, Ec, CT[:, g, :].re("p (o n) -> p o n", o=1).bc([128, 4, 128]), ALU.mult)
                    yield
                    py = self.bank()
                    for hh in range(4):
                        h = 4 * g + hh
                        f.mm(py[:, hh * 64:(hh + 1) * 64], MT[:, hh, :], xdt[:, h, :], start=True, stop=False)
                        f.mm(py[:, hh * 64:(hh + 1) * 64], CsT[:, hh, :], S16[g][:, hh, :], start=False, stop=True)
                    if d == 0:
                        f.copy(y[:, g * 256:(g + 1) * 256], py[:, 0:256], eng="act")
                    else:
                        f.tt(y[:, g * 256:(g + 1) * 256], py[:, 0:256], ysf[:, g * 256:(g + 1) * 256], ALU.add)
                    pst = self.bank()
                    yield
                    f.mm(pst[:, 0:256], Btm[:, g * 128:(g + 1) * 128], xe[:, hs, :].re("p h n -> p (h n)"))
                    f.tt(S32[g], S32[g], bcl(cd[:, hs]).bc([128, 4, 64]), ALU.mult)
                    f.tt(S32[g], S32[g], pst[:, 0:256].re("p (h n) -> p h n", h=4), ALU.add)
                    f.copy(S16[g], S32[g], eng="pool")
                for pair in ((0, 1, 2, 3), (4, 5, 6, 7)):
                    alive = [sgrp(g, SS[g % 4]) for g in pair]
                    while alive:
                        for gen_ in list(alive):
                            try:
                                next(gen_)
                            except StopIteration:
                                alive.remove(gen_)
                if d == 0:
                    f.store(self.YSF[u0:u0 + 128, :], y)
                    continue
                y3 = y.re("p (h n) -> p h n", h=32)
                t3 = ysf.re("p (h n) -> p h n", h=32)
                f.tt(t3, xs, bcl(Dsk).bc([128, 32, 64]), ALU.mult)
                f.tt(y, y, ysf, ALU.add)
                f.tt(y, y, zs, ALU.mult)
                f.tt(ysf, y, y, ALU.mult)
                f.reduce(g8, ysf.re("p (g n) -> p g n", g=8), ALU.add)
                f.ts(g8, g8, 1.0 / 256, ALU.mult, 1e-5, ALU.add)
                f.act(g8, g8, AF.Sqrt)
                f.recip(g8, g8)
                f.tt(y.re("p (g n) -> p g n", g=8), y.re("p (g n) -> p g n", g=8), g8.re("p (g o) -> p g o", o=1).bc([128, 8, 256]), ALU.mult)
                f.tt(yo, y, NW, ALU.mult)
                for (ps_, ap) in self.ssm_dram_rows(self.YSSM, i):
                    f.store(ap, yo[ps_, :])


KB.stage_S2 = _stage_S2


def _load_w_bf16(self, dst, src_ap, nk, ncols, stage_tiles):
    f = self.f
    j = 0
    for k0 in range(0, nk, 8):
        kn = min(8, nk - k0)
        for c0 in range(0, ncols, 512):
            st = stage_tiles[j % 2]
            j += 1
            f.load(st[:, 0:kn, :], src_ap[k0 * 128:(k0 + kn) * 128, c0:c0 + 512].re("(k p) n -> p k n", p=128))
            f.copy(dst[:, k0:k0 + kn, c0:c0 + 512], st[:, 0:kn, :], eng="pool")


def _stage_G(self, l):
    f = self.f
    NT, n_ctx = self.NT, self.n_ctx
    with f.scope():
        Wrw = f.sb("gWrw", [128, 8, 1024], BF16)
        Wss = f.sb("gWss", [128, 16, 1024], BF16)
        Wo = f.sb("gWo", [128, 8, 1024], BF16)
        with f.scope():
            st = [f.sb("gst%d" % j, [128, 8, 512]) for j in range(2)]
            self.load_w_bf16(Wrw, self.w_branch_rw[l], 8, 1024, st)
            self.load_w_bf16(Wss, self.w_branch_ssm[l], 16, 1024, st)
            self.load_w_bf16(Wo, self.w_out[l], 8, 1024, st)
        gate = {}
        for r in range(2):
            t = f.sb("gGate%d" % r, [128, D])
            self.bcast_load(t, self.MODS[r, 2, :], D)
            gate[r] = t
        yin = [f.sb("g_yin%d" % j, [128, 3072], BF16) for j in range(2)]
        gsb = [f.sb("g_gs%d" % j, [128, 2048], BF16) for j in range(2)]
        xts = [f.sb("g_xt%d" % j, [128, D]) for j in range(2)]
        yT = f.sb("g_yT", [128, 24, 128], BF16)
        t1 = f.sb("g_t1", [128, 1024])
        t2 = f.sb("g_t2", [128, 1024])
        mg = f.sb("g_mg", [128, 1024], BF16)
        mT = f.sb("g_mT", [128, 8, 128], BF16)
        for i in range(NT):
            r = 1 if i * 128 < n_ctx else 0
            yi, gs, xt = yin[i % 2], gsb[i % 2], xts[i % 2]
            rs = slice(i * 128, (i + 1) * 128)
            f.load(yi[:, 0:1024], self.YRW[rs, :])
            f.load(yi[:, 1024:3072], self.YSSM[rs, :])
            f.load(gs, self.GS[rs, :])
            f.load(xt, self.XS[rs, :])
            for b3 in range(3):
                pbb = self.bank().bitcast(BF16)
                for j in range(8):
                    kq = b3 * 8 + j
                    f.tr(pbb[:, j * 128:(j + 1) * 128], yi[:, kq * 128:(kq + 1) * 128], self.identb)
                f.copy(yT[:, b3 * 8:(b3 + 1) * 8, :], pbb.re("p (k n) -> p k n", k=8), eng=("act" if b3 % 2 else "dve"))
            for half in range(2):
                hs = slice(half * 512, (half + 1) * 512)
                p1 = self.bank()
                for kq in range(8):
                    f.mm(p1, yT[:, kq, :], Wrw[:, kq, hs], start=(kq == 0), stop=(kq == 7))
                p2 = self.bank()
                for kq in range(16):
                    f.mm(p2, yT[:, 8 + kq, :], Wss[:, kq, hs], start=(kq == 0), stop=(kq == 15))
                f.tt(t1[:, hs], p1, gs[:, hs], ALU.mult)
                f.tt(t2[:, hs], p2, gs[:, 1024 + half * 512:1024 + (half + 1) * 512], ALU.mult)
            f.tt(mg, t1, t2, ALU.add)
            pbb = self.bank().bitcast(BF16)
            for j in range(8):
                f.tr(pbb[:, j * 128:(j + 1) * 128], mg[:, j * 128:(j + 1) * 128], self.identb)
            f.copy(mT, pbb.re("p (k n) -> p k n", k=8), eng="act")
            for half in range(2):
                hs = slice(half * 512, (half + 1) * 512)
                p1 = self.bank()
                for kq in range(8):
                    f.mm(p1, mT[:, kq, :], Wo[:, kq, hs], start=(kq == 0), stop=(kq == 7))
                f.tt(t1[:, hs], p1, gate[r][:, hs], ALU.mult)
            f.tt(xt, xt, t1, ALU.add)
            f.store(self.XS[rs, :], xt)


def _stage_F(self, l, tiles_per_pass=17):
    FDBG = ''
    f = self.f
    NT, n_ctx = self.NT, self.n_ctx
    with f.scope():
        mbg = self.load_mod_bcast([5])
        BR = f.sb("fBR", [128, 32])
        self.bcast_load(BR, self.b_router, 32)
        wr = f.sb("fwr", [128, 8, 32])
        f.load(wr, self.w_router.re("(k p) n -> p k n", p=128))
        sc = f.sb("f_sc", [128, 32])
        bi = f.sb("f_bi", [128, 32])
        m8 = f.sb("f_m8", [128, 4, 8])
        gsx = f.sb("f_gsx", [128, 4])
        gmx = f.sb("f_gmx", [128, 1])
        gm = f.sb("f_gm", [128, 4])
        tq = f.sb("f_tq", [128, 4])
        thr = f.sb("f_thr", [128, 1])
        em = f.sb("f_em", [128, 32])
        den = f.sb("f_den", [128, 1])
        for p0 in range(0, NT, tiles_per_pass):
            tiles = list(range(p0, min(NT, p0 + tiles_per_pass)))
            ng = (len(tiles) + 3) // 4
            with f.scope():
                hTg = [f.sb("f_hT%d" % gi, [128, 8, 512], BF16) for gi in range(ng)]
                acc = [f.sb("f_acc%d" % j, [128, D]) for j in range(len(tiles))]
                gw = [f.sb("f_gw%d" % j, [128, 32]) for j in range(len(tiles))]
                ph = f.scope()
                ph.__enter__()
                mb = self.load_mod_bcast([3, 4])
                self.alloc_h_tmps()
                hf = f.sb("f_hf", [128, 8, 128])
                xts = [f.sb("f_xt%d" % j, [128, D]) for j in range(2)]
                for j, i in enumerate(tiles):
                    xt = xts[j % 2]
                    r = 1 if i * 128 < n_ctx else 0
                    f.load(xt, self.XS[i * 128:(i + 1) * 128, :])
                    self.emit_h(xt, mb[(r, 3)], mb[(r, 4)], hTg[j // 4][:, :, (j % 4) * 128:(j % 4 + 1) * 128], hf_dst=hf)
                    f.memset(acc[j], 0.0, eng="pool")
                    pb = self.bank()
                    for k in range(8):
                        f.mm(pb[:, 0:32], hf[:, k, :], wr[:, k, :], start=(k == 0), stop=(k == 7))
                    f.act(sc, pb[:, 0:32], AF.Sigmoid)
                    f.tt(bi, sc, BR, ALU.add)
                    for g in range(4):
                        f.max8(m8[:, g, :], bi[:, g * 8:(g + 1) * 8])
                    f.tt(gsx, m8[:, :, 0], m8[:, :, 1], ALU.add)
                    f.reduce(gmx, gsx, ALU.max)
                    f.ts(gm, gsx, gmx[:, 0:1], ALU.is_equal)
                    f.tt(tq, gm, m8[:, :, 1], ALU.mult)
                    f.reduce(thr, tq, ALU.add)
                    f.ts(em, bi, thr[:, 0:1], ALU.is_ge)
                    f.tt(em.re("p (g n) -> p g n", g=4), em.re("p (g n) -> p g n", g=4),
                         gm.re("p (g o) -> p g o", o=1).bc([128, 4, 8]), ALU.mult)
                    f.tt(gw[j], sc, em, ALU.mult)
                    f.reduce(den, gw[j], ALU.add)
                    f.recip(den, den)
                    f.ts(gw[j], gw[j], den[:, 0:1], ALU.mult)
                ph.__exit__(None, None, None)
                ph = f.scope()
                ph.__enter__()
                w13f = [f.sb("f_w13f%d" % j, [128, 8, 512]) for j in range(2)]
                w2f = w13f[0].re("p k n -> p (k n)").re("p (k n) -> p k n", k=4)
                w1b = f.sb("f_w1b", [128, 8, 512], BF16)
                w3b = f.sb("f_w3b", [128, 8, 512], BF16)
                w2b = f.sb("f_w2b", [128, 4, 1024], BF16)
                sil = f.sb("f_sil", [128, 512], BF16)
                gT = f.sb("f_gT", [128, 4, 512], BF16)
                for e in range(0 if 'noexp' in FDBG else (2 if 'exp2' in FDBG else 32)):
                    f.load(w13f[0], self.exp_w1[l, e].re("(k p) n -> p k n", p=128))
                    f.copy(w1b, w13f[0], eng="pool")
                    f.load(w13f[1], self.exp_w3[l, e].re("(k p) n -> p k n", p=128))
                    f.copy(w3b, w13f[1], eng="pool")
                    f.load(w2f, self.exp_w2[l, e].re("(k p) n -> p k n", p=128))
                    f.copy(w2b, w2f, eng="pool")
                    for gi in range(ng):
                        nt_g = min(4, len(tiles) - gi * 4)
                        ntok = nt_g * 128
                        for fc in range(4):
                            fs = slice(fc * 128, (fc + 1) * 128)
                            pa, pb = self.bank(), self.bank()
                            for k in range(8):
                                f.mm(pa[:, 0:ntok], w1b[:, k, fs], hTg[gi][:, k, 0:ntok], start=(k == 0), stop=(k == 7))
                            for k in range(8):
                                f.mm(pb[:, 0:ntok], w3b[:, k, fs], hTg[gi][:, k, 0:ntok], start=(k == 0), stop=(k == 7))
                            f.act(sil[:, 0:ntok], pa[:, 0:ntok], AF.Silu)
                            f.tt(gT[:, fc, 0:ntok], sil[:, 0:ntok], pb[:, 0:ntok], ALU.mult)
                        for jj in range(nt_g):
                            j = gi * 4 + jj
                            for half in range(2):
                                hs = slice(half * 512, (half + 1) * 512)
                                po = self.bank()
                                for fc in range(4):
                                    f.mm(po, gT[:, fc, jj * 128:(jj + 1) * 128], w2b[:, fc, hs], start=(fc == 0), stop=(fc == 3))
                                f.stt(acc[j][:, hs], po, gw[j][:, e:e + 1], acc[j][:, hs], ALU.mult, ALU.add)
                ph.__exit__(None, None, None)
                xts = [f.sb("f_xt%d" % j, [128, D]) for j in range(2)]
                for j, i in enumerate(tiles):
                    xt = xts[j % 2]
                    r = 1 if i * 128 < n_ctx else 0
                    f.load(xt, self.XS[i * 128:(i + 1) * 128, :])
                    f.tt(acc[j], acc[j], mbg[(r, 5)], ALU.mult)
                    f.tt(xt, xt, acc[j], ALU.add)
                    f.store(self.XS[i * 128:(i + 1) * 128, :], xt)


KB.load_w_bf16 = _load_w_bf16
KB.stage_G = _stage_G
KB.stage_F = _stage_F


def build_program(n_ctx, n_lat, depth):
    K = KB(n_ctx, n_lat, depth)
    K.stage_init()
    for l in range(depth):
        K.stage_mod(l)
        K.stage_P(l)
        K.stage_R(l)
        K.stage_S1(l)
        K.stage_S2(l)
        K.stage_G(l)
        K.stage_F(l)
    K.stage_final()
    return K.finish()


def kernel(**inputs):
    from concourse.bass_utils import run_bass_kernel_spmd
    inp = {k: np.asarray(v) for k, v in inputs.items()}
    B, n_lat, _ = inp["x"].shape
    n_ctx = inp["ctx"].shape[1]
    L = inp["w_mod"].shape[0]
    nc = build_program(n_ctx, n_lat, L)
    consts = make_consts()
    n_cores = 8
    in_maps = []
    for core in range(n_cores):
        b = core // 2
        m = {}
        for k, v in inp.items():
            if k in ("x", "ctx"):
                m[k] = np.ascontiguousarray(v[b])
            elif k in ("c", "c_ctx"):
                continue
            else:
                m[k] = np.ascontiguousarray(v)
        m["c2"] = np.ascontiguousarray(np.stack([inp["c"][b], inp["c_ctx"]]))
        m["consts"] = consts
        m["rw_r_k"] = np.ascontiguousarray(inp["rw_r_k"].reshape(L, 1024))
        m["ssm_dt_bias"] = np.ascontiguousarray(inp["ssm_dt_bias"].reshape(L, 64))
        m["ssm_a_log"] = np.ascontiguousarray(inp["ssm_a_log"].reshape(L, 64))
        m["ssm_d"] = np.ascontiguousarray(inp["ssm_d"].reshape(L, 64))
        in_maps.append(m)
    res = run_bass_kernel_spmd(nc, in_maps, core_ids=list(range(n_cores)))
    out = np.stack([np.asarray(res.results[2 * b]["out"]) for b in range(B)], axis=0)
    return out.astype(np.float32)
```

```python
import numpy as np
import concourse.bass as bass
import concourse.mybir as mybir

F32 = mybir.dt.float32
BF16 = mybir.dt.bfloat16
I32 = mybir.dt.int32
AF = mybir.ActivationFunctionType
ALU = mybir.AluOpType
AX = mybir.AxisListType

N_DMA_SEMS = 6
N_ENG_SEMS = 8
SAME_ENGINE_SYNC = True


class Trk:
    __slots__ = ("w", "r")

    def __init__(self):
        self.w = None
        self.r = []


class V:
    __slots__ = ("t", "ap")

    def __init__(self, t, ap):
        self.t = t
        self.ap = ap

    def __getitem__(self, key):
        return V(self.t, self.ap[key])

    def re(self, s, **kw):
        return V(self.t, self.ap.rearrange(s, **kw))

    def bc(self, shape):
        return V(self.t, self.ap.to_broadcast(shape))

    def bitcast(self, dt):
        return V(self.t, self.ap.bitcast(dt))


class Fw:
    ENGS = ("pe", "act", "dve", "pool", "sp")

    def __init__(self, nc):
        self.nc = nc
        self.handles = {"pe": nc.tensor, "act": nc.scalar, "dve": nc.vector, "pool": nc.gpsimd, "sp": nc.sync}
        self.ops = {e: [] for e in self.ENGS}
        self.esem = {e: [nc.alloc_semaphore("s_%s%d" % (e, i)) for i in range(N_ENG_SEMS)] for e in self.ENGS}
        self.dsems = {}
        self.dcount = {}
        self.dnext = {}
        for q in ("sp", "pool", "act"):
            self.dsems[q] = [nc.alloc_semaphore("d_%s%d" % (q, i)) for i in range(N_DMA_SEMS)]
            self.dcount[q] = [0] * N_DMA_SEMS
            self.dnext[q] = 0
        self.n_alloc = 0
        self.stacks = []
        self.names = {}
        self.out_events = []

    def sb(self, name, shape, dtype=F32):
        self.n_alloc += 1
        nm = "%s_%d" % (name, self.n_alloc)
        self.names[name] = nm
        if self.stacks:
            h = self.stacks[-1].enter_context(self.nc.sbuf_tensor(nm, list(shape), dtype))
        else:
            h = self.nc.alloc_sbuf_tensor(nm, list(shape), dtype)
        return V(Trk(), h[tuple(slice(None) for _ in shape)])

    def scope(self):
        fw = self

        class _S:
            def __enter__(s):
                import contextlib
                fw.stacks.append(contextlib.ExitStack())
                return s

            def __exit__(s, *a):
                fw.barrier()
                fw.stacks.pop().close()
                return False
        return _S()

    def barrier(self):
        last = {}
        for e in self.ENGS:
            last[e] = -1
            for i in range(len(self.ops[e]) - 1, -1, -1):
                o = self.ops[e][i]
                if o["dma"] is None and o["fn"] is not None:
                    last[e] = i
                    break
        for e in self.ENGS:
            waits = {}
            for e2 in self.ENGS:
                if (e2 != e or (SAME_ENGINE_SYNC and e != 'pe')) and last[e2] >= 0:
                    waits[("e", e2)] = last[e2]
            for q in self.dsems:
                for si in range(N_DMA_SEMS):
                    if self.dcount[q][si] > 0:
                        waits[("d", q, si)] = self.dcount[q][si]
            self.ops[e].append({"waits": waits, "fn": None, "dma": None})

    def ps(self, name, shape, dtype=F32):
        self.n_alloc += 1
        h = self.nc.alloc_psum_tensor("%s_%d" % (name, self.n_alloc), list(shape), dtype)
        return V(Trk(), h[tuple(slice(None) for _ in shape)])

    def dram(self, name, shape, dtype=F32, kind="Internal"):
        h = self.nc.dram_tensor(name, list(shape), dtype, kind=kind)
        return V(Trk(), h.ap())

    def _deps(self, eng, reads, writes):
        deps = []
        for v in reads:
            if v.t.w is not None:
                deps.append(v.t.w)
        for v in writes:
            if v.t.w is not None:
                deps.append(v.t.w)
            deps.extend(v.t.r)
        out = {}
        for ev in deps:
            kind = ev[0]
            if kind == "e":
                _, e, idx = ev
                if e == eng and (not SAME_ENGINE_SYNC or e == "pe"):
                    continue
                k = ("e", e)
                out[k] = max(out.get(k, -1), idx)
            else:
                _, q, si, val = ev
                k = ("d", q, si)
                out[k] = max(out.get(k, -1), val)
        return out

    def _mark(self, ev, reads, writes):
        for v in reads:
            v.t.r.append(ev)
        for v in writes:
            v.t.w = ev
            v.t.r = []

    def op(self, eng, fn, outs, ins):
        outs = [o for o in outs if o is not None]
        ins = [i for i in ins if isinstance(i, V)]
        waits = self._deps(eng, ins, outs)
        idx = len(self.ops[eng])
        self.ops[eng].append({"waits": waits, "fn": fn, "dma": None})
        self._mark(("e", eng, idx), ins, outs)

    def dma(self, q, out, in_, **kw):
        waits = self._deps(q, [in_], [out])
        si = self.dnext[q]
        self.dnext[q] = (si + 1) % N_DMA_SEMS
        if self.dcount[q][si] > 0:
            k = ("d", q, si)
            waits[k] = max(waits.get(k, -1), self.dcount[q][si])
        self.dcount[q][si] += 1
        val = self.dcount[q][si]
        o_ap, i_ap = out.ap, in_.ap
        self.ops[q].append({"waits": waits, "fn": lambda h: h.dma_start(out=o_ap, in_=i_ap, **kw), "dma": (si, val)})
        ev = ("d", q, si, val)
        self._mark(ev, [in_], [out])
        return ev

    def finish(self):
        nc = self.nc
        need = {e: set() for e in self.ENGS}
        for e in self.ENGS:
            for o in self.ops[e]:
                for k, v in o["waits"].items():
                    if k[0] == "e":
                        need[k[1]].add(v)
        rank = {}
        for e in self.ENGS:
            r = 0
            for i in range(len(self.ops[e])):
                if i in need[e]:
                    rank[(e, i)] = (r % N_ENG_SEMS, r // N_ENG_SEMS + 1)
                    r += 1
        final_waits = []
        for q in self.dsems:
            for si in range(N_DMA_SEMS):
                if self.dcount[q][si] > 0:
                    final_waits.append((self.dsems[q][si], 16 * self.dcount[q][si]))
        ops, esem, dsems, handles = self.ops, self.esem, self.dsems, self.handles

        def replay(e, h):
            seen = {}
            for i, o in enumerate(ops[e]):
                for k, v in o["waits"].items():
                    if k[0] == "e":
                        si_, val = rank[(k[1], v)]
                        sem = esem[k[1]][si_]
                        k = ("e", k[1], si_)
                    else:
                        sem, val = dsems[k[1]][k[2]], 16 * v
                    if seen.get(k, -1) >= val:
                        continue
                    seen[k] = val
                    h.wait_ge(sem, val)
                if o["fn"] is None:
                    continue
                ins = o["fn"](h)
                if o["dma"] is not None:
                    ins.then_inc(dsems[e][o["dma"][0]], 16)
                elif (e, i) in rank:
                    ins.then_inc(esem[e][rank[(e, i)][0]], 1)

        with nc.Block() as block:
            @block.tensor
            def _(h):
                replay("pe", h)

            @block.scalar
            def _(h):
                replay("act", h)

            @block.vector
            def _(h):
                replay("dve", h)

            @block.gpsimd
            def _(h):
                replay("pool", h)

            @block.sync
            def _(h):
                replay("sp", h)
                for sem, val in final_waits:
                    h.wait_ge(sem, val)

    def mm(self, out, lhsT, rhs, start=True, stop=True):
        o, l, r = out.ap, lhsT.ap, rhs.ap
        self.op("pe", lambda h: h.matmul(o, l, r, start=start, stop=stop), [out], [lhsT, rhs] + ([] if start else [out]))

    def tr(self, out, in_, ident):
        o, i, d = out.ap, in_.ap, ident.ap
        self.op("pe", lambda h: h.transpose(o, i, d), [out], [in_, ident])

    def act(self, out, in_, func, bias=None, scale=None, accum=None, eng="act"):
        kw = {}
        if bias is not None:
            kw["bias"] = bias.ap if isinstance(bias, V) else bias
        if scale is not None:
            kw["scale"] = scale.ap if isinstance(scale, V) else scale
        if accum is not None:
            kw["accum_out"] = accum.ap
        o, i = out.ap, in_.ap
        self.op("act", lambda h: h.activation(o, i, func, **kw), [out, accum], [in_, bias, scale])

    def tt(self, out, a, b, op, eng="dve"):
        o, x, y = out.ap, a.ap, b.ap
        self.op(eng, lambda h: h.tensor_tensor(o, x, y, op), [out], [a, b])

    def ts(self, out, a, s1, op0, s2=None, op1=None, accum=None, eng="dve"):
        o, x = out.ap, a.ap
        c1 = s1.ap if isinstance(s1, V) else s1
        c2 = s2.ap if isinstance(s2, V) else s2
        kw = {}
        if op1 is not None:
            kw["op1"] = op1
        if accum is not None:
            kw["accum_out"] = accum.ap
        if op1 is None and accum is None:
            self.op(eng, lambda h: h.tensor_single_scalar(o, x, c1, op0), [out], [a, s1])
        else:
            self.op(eng, lambda h: h.tensor_scalar(o, x, c1, c2, op0, **kw), [out, accum], [a, s1, s2])

    def stt(self, out, a, s, b, op0, op1, eng="dve"):
        o, x, y = out.ap, a.ap, b.ap
        c = s.ap if isinstance(s, V) else s
        self.op(eng, lambda h: h.scalar_tensor_tensor(o, x, c, y, op0, op1), [out], [a, s, b])

    def copy(self, out, in_, eng="dve"):
        o, i = out.ap, in_.ap
        if eng == "act":
            self.op("act", lambda h: h.activation(o, i, AF.Identity), [out], [in_])
        else:
            self.op(eng, lambda h: h.tensor_copy(o, i), [out], [in_])

    def memset(self, out, val, eng="dve"):
        o = out.ap
        self.op(eng, lambda h: h.memset(o, val), [out], [])

    def reduce(self, out, in_, op, axis=AX.X, eng="dve"):
        o, i = out.ap, in_.ap
        self.op(eng, lambda h: h.tensor_reduce(o, i, axis, op), [out], [in_])

    def recip(self, out, in_):
        o, i = out.ap, in_.ap
        self.op("dve", lambda h: h.reciprocal(o, i), [out], [in_])

    def max8(self, out, in_):
        o, i = out.ap, in_.ap
        self.op("dve", lambda h: h.max(o, i), [out], [in_])

    def load(self, out, in_, q="sp", **kw):
        return self.dma(q, out, in_, **kw)

    def store(self, out, in_, q="pool", **kw):
        return self.dma(q, out, in_, **kw)


D = 1024
RW_COLS = 3488
SSM_COLS = 6208
IN_COLS = 11744
NCONST = 18
GRID_W = 64


def make_consts():
    c = np.zeros((128, NCONST, 128), np.float32)
    i = np.arange(128)
    s, t = i[:, None], i[None, :]
    c[:, 0] = np.eye(128)
    c[:, 1] = 1.0
    c[:, 2] = (s <= t)
    c[:, 3] = (s < t)
    c[:, 4] = (s >= t)
    c[:, 5] = (s > t)
    c[:, 6] = np.where(t >= s, 0.0, -1e30)
    c[:, 7] = np.where(t <= s, 0.0, -1e30)
    low = (s > t)
    c[:, 8] = low & (s // 8 == t // 8)
    for k in range(1, 5):
        bs = 8 << k
        c[:, 8 + k] = low & (s // bs == t // bs) & (s // (bs // 2) != t // (bs // 2))
    for k in range(5):
        c[:, 13 + k] = c[:, 8 + k].T
    return c


class KB:
    def __init__(self, n_ctx, n_lat, depth=2, stages="all"):
        self.n_ctx, self.n_lat, self.depth = n_ctx, n_lat, depth
        self.T = n_ctx + n_lat
        self.rows = n_lat // GRID_W
        self.NT = self.T // 128
        self.stages = stages
        nc = bass.Bass("TRN2", target_bir_lowering=False)
        self.nc = nc
        f = self.f = Fw(nc)
        T = self.T
        inp = lambda name, shape: f.dram(name, shape, F32, kind="ExternalInput")
        self.x = inp("x", [n_lat, D])
        self.ctx = inp("ctx", [n_ctx, D])
        self.c2 = inp("c2", [2, D])
        self.consts = inp("consts", [128, NCONST, 128])
        L = depth
        self.w_mod = inp("w_mod", [L, D, 6 * D])
        self.b_mod = inp("b_mod", [L, 6 * D])
        self.norm_mix_g = inp("norm_mix_g", [L, D])
        self.w_in = inp("w_in", [L, D, IN_COLS])
        self.rw_shift_mu = inp("rw_shift_mu", [L, RW_COLS])
        self.rw_w0 = inp("rw_w0", [L, 2, 1024])
        self.rw_w2 = inp("rw_w2", [L, 2, 64, 1024])
        self.rw_a0 = inp("rw_a0", [L, 2, 1024])
        self.rw_a2 = inp("rw_a2", [L, 2, 64, 1024])
        self.rw_g2 = inp("rw_g2", [L, 160, 1024])
        self.rw_k_k = inp("rw_k_k", [L, 1024])
        self.rw_k_a = inp("rw_k_a", [L, 1024])
        self.rw_r_k = inp("rw_r_k", [L, 1024])
        self.rw_ln_w = inp("rw_ln_w", [L, 1024])
        self.rw_ln_b = inp("rw_ln_b", [L, 1024])
        self.ssm_conv_w = inp("ssm_conv_w", [L, 5, 4096])
        self.ssm_conv_b = inp("ssm_conv_b", [L, 4096])
        self.ssm_dt_bias = inp("ssm_dt_bias", [L, 64])
        self.ssm_a_log = inp("ssm_a_log", [L, 64])
        self.ssm_d = inp("ssm_d", [L, 64])
        self.ssm_norm_w = inp("ssm_norm_w", [L, 2048])
        self.w_branch_rw = inp("w_branch_rw", [L, 1024, D])
        self.w_branch_ssm = inp("w_branch_ssm", [L, 2048, D])
        self.w_out = inp("w_out", [L, D, D])
        self.norm_ffn_g = inp("norm_ffn_g", [L, D])
        self.w_router = inp("w_router", [D, 32])
        self.b_router = inp("b_router", [32])
        self.exp_w1 = inp("exp_w1", [L, 32, D, 512])
        self.exp_w3 = inp("exp_w3", [L, 32, D, 512])
        self.exp_w2 = inp("exp_w2", [L, 32, 512, D])
        self.norm_final_g = inp("norm_final_g", [D])
        self.out = f.dram("out", [n_lat, D], F32, kind="ExternalOutput")
        self.XS = f.dram("XS", [T, D])
        self.MODS = f.dram("MODS", [2, 6, D])
        self.PRW = f.dram("PRW", [T + 6, RW_COLS])
        self.GS = f.dram("GS", [T, 2048], BF16)
        self.YF = f.dram("YF", [T, 1024])
        self.YRW = f.dram("YRW", [T, 1024], BF16, kind=("ExternalOutput" if stages == "debug" else "Internal"))
        self.cst = f.sb("cst", [128, NCONST, 128])
        self.cstb = f.sb("cstb", [128, NCONST, 128], BF16)
        self.banks = [f.ps("bank%d" % i, [128, 512]) for i in range(8)]
        self.bi = 0
        f.load(self.cst, self.consts)
        f.copy(self.cstb, self.cst)
        self.ident = self.cst[:, 0, :]
        self.identb = self.cstb[:, 0, :]
        self.ones = self.cst[:, 1, :]

    def bank(self):
        b = self.banks[self.bi]
        self.bi = (self.bi + 1) % 8
        return b

    def bcast_load(self, tile, row_ap, n):
        self.f.load(tile, V(row_ap.t, row_ap.ap.partition_broadcast(128)))

    def pad_row(self, t):
        return t + 1 if t < self.n_ctx else t + 3

    def stage_init(self):
        f = self.f
        f.store(self.XS[0:self.n_ctx, :], self.ctx, q="sp")
        f.store(self.XS[self.n_ctx:self.T, :], self.x, q="sp")

    def stage_mod(self, l):
        f = self.f
        with f.scope():
            cT = f.sb("cT", [128, 2, 8])
            scT = f.sb("scT", [128, 2, 8])
            with self.nc.allow_non_contiguous_dma("tiny transposed load"):
                f.load(cT, self.c2.re("r (k p) -> p r k", p=128), allow_slow_non_contiguous=True)
            f.act(scT, cT, AF.Silu)
            rows = [f.sb("mrow%d" % r, [1, 6 * D]) for r in range(2)]
            bm = f.sb("bm", [1, 6 * D])
            f.load(bm, self.b_mod[l:l + 1, :])
            g2 = f.sb("g2", [1, 2, D])
            f.load(g2[:, 0, :], self.norm_mix_g[l:l + 1, :])
            f.load(g2[:, 1, :], self.norm_ffn_g[l:l + 1, :])
            wts = [f.sb("wm%d" % i, [128, 8, 512]) for i in range(2)]
            for cb in range(12):
                wt = wts[cb % 2]
                f.load(wt, self.w_mod[l, :, cb * 512:(cb + 1) * 512].re("(k p) n -> p k n", p=128))
                for r in range(2):
                    pb = self.bank()
                    for k in range(8):
                        f.mm(pb[0:1, :], scT[:, r, k:k + 1], wt[:, k, :], start=(k == 0), stop=(k == 7))
                    f.tt(rows[r][:, cb * 512:(cb + 1) * 512], pb[0:1, :], bm[:, cb * 512:(cb + 1) * 512], ALU.add)
            for r in range(2):
                row = rows[r]
                o = f.sb("orow%d" % r, [1, 6, D])
                f.stt(o[:, 0, :], row[:, D:2 * D], 1.0, g2[:, 0, :], ALU.add, ALU.mult)
                f.copy(o[:, 1, :], row[:, 0:D])
                f.copy(o[:, 2, :], row[:, 2 * D:3 * D])
                f.stt(o[:, 3, :], row[:, 4 * D:5 * D], 1.0, g2[:, 1, :], ALU.add, ALU.mult)
                f.copy(o[:, 4, :], row[:, 3 * D:4 * D])
                f.copy(o[:, 5, :], row[:, 5 * D:6 * D])
                f.store(self.MODS[r:r + 1, :, :], o)

    def emit_h(self, xt, G, S, hT_dst, hf_dst=None):
        f = self.f
        sq, ss, hh = self.h_sq, self.h_ss, self.h_h
        f.act(sq, xt, AF.Square, accum=ss)
        f.ts(ss, ss, 1.0 / D, ALU.mult, 1e-6, ALU.add)
        f.act(ss, ss, AF.Sqrt)
        f.recip(ss, ss)
        f.stt(hh, xt, ss[:, 0:1], G, ALU.mult, ALU.mult)
        f.tt(hh, hh, S, ALU.add)
        for half in range(2):
            pb = self.bank()
            for k in range(4):
                kk = half * 4 + k
                f.tr(pb[:, k * 128:(k + 1) * 128], hh[:, kk * 128:(kk + 1) * 128], self.ident)
            if hf_dst is not None:
                f.copy(hf_dst[:, half * 4:(half + 1) * 4, :], pb.re("p (k n) -> p k n", k=4), eng="dve")
                f.copy(hT_dst[:, half * 4:(half + 1) * 4, :], hf_dst[:, half * 4:(half + 1) * 4, :], eng="act")
            else:
                f.copy(hT_dst[:, half * 4:(half + 1) * 4, :], pb.re("p (k n) -> p k n", k=4), eng="act")

    def alloc_h_tmps(self):
        f = self.f
        self.h_ss = f.sb("h_ss", [128, 1])
        self.h_h = f.sb("h_h", [128, D])
        self.h_sq = self.h_h

    def load_mod_bcast(self, idxs):
        f = self.f
        out = {}
        for r in range(2):
            for i in idxs:
                t = f.sb("mb%d_%d" % (r, i), [128, D])
                self.bcast_load(t, self.MODS[r, i, :], D)
                out[(r, i)] = t
        return out

    def stage_P(self, l):
        f = self.f
        T, NT = self.T, self.NT
        with f.scope():
            self.alloc_h_tmps()
            mb = self.load_mod_bcast([0, 1])
            hT = [f.sb("hT%d" % i, [128, 8, 128], BF16) for i in range(NT)]
            xts = [f.sb("xt%d" % i, [128, D]) for i in range(2)]
            for i in range(NT):
                xt = xts[i % 2]
                f.load(xt, self.XS[i * 128:(i + 1) * 128, :])
                r = 1 if i * 128 < self.n_ctx else 0
                self.emit_h(xt, mb[(r, 0)], mb[(r, 1)], hT[i])
            z = f.sb("zrow", [1, RW_COLS])
            f.memset(z, 0.0)
            for prow in (0, self.n_ctx + 1, self.n_ctx + 2, T + 3):
                f.store(self.PRW[prow:prow + 1, :], z)
            blocks = [(c0, min(512, RW_COLS - c0), "rw") for c0 in range(0, RW_COLS, 512)]
            blocks += [(RW_COLS + SSM_COLS + c0, 512, "gate") for c0 in range(0, 2048, 512)]
            wfs = [f.sb("wf%d" % i, [128, 8, 512]) for i in range(2)]
            wbs = [f.sb("wb%d" % i, [128, 8, 512], BF16) for i in range(2)]
            o32 = [f.sb("o32_%d" % i, [128, 512]) for i in range(2)]
            o16 = [f.sb("o16_%d" % i, [128, 512], BF16) for i in range(2)]
            for bi_, (c0, nb, kind) in enumerate(blocks):
                wf, wb = wfs[bi_ % 2], wbs[bi_ % 2]
                f.load(wf[:, :, 0:nb], self.w_in[l, :, c0:c0 + nb].re("(k p) n -> p k n", p=128))
                f.copy(wb[:, :, 0:nb], wf[:, :, 0:nb], eng="pool")
                for i in range(NT):
                    pb = self.bank()
                    for k in range(8):
                        f.mm(pb[:, 0:nb], hT[i][:, k, :], wb[:, k, 0:nb], start=(k == 0), stop=(k == 7))
                    if kind == "rw":
                        o = o32[i % 2]
                        f.copy(o[:, 0:nb], pb[:, 0:nb], eng="act")
                        pr = self.pad_row(i * 128)
                        f.store(self.PRW[pr:pr + 128, c0:c0 + nb], o[:, 0:nb])
                    else:
                        o = o16[i % 2]
                        f.act(o, pb, AF.Sigmoid)
                        g0 = c0 - RW_COLS - SSM_COLS
                        f.store(self.GS[i * 128:(i + 1) * 128, g0:g0 + 512], o)

    def finish(self):
        self.f.finish()
        return self.nc


def _stage_R(self, l):
    f = self.f
    NT = self.NT
    nctx_t = self.n_ctx // 128
    with f.scope():
        P = {}
        def bc(name, row_ap, n):
            t = f.sb(name, [128, n])
            self.bcast_load(t, row_ap, n)
            return t
        P["MU"] = bc("MU", self.rw_shift_mu[l, :], RW_COLS)
        P["KK"] = bc("KKp", self.rw_k_k[l, :], 1024)
        P["KA"] = bc("KAp", self.rw_k_a[l, :], 1024)
        P["RK"] = bc("RKp", self.rw_r_k[l, :], 1024)
        P["LNW"] = bc("LNW", self.rw_ln_w[l, :], 1024)
        P["LNB"] = bc("LNB", self.rw_ln_b[l, :], 1024)
        g2b = f.sb("g2b", [128, 2, 1024], BF16)
        with f.scope():
            g2f = f.sb("g2f", [128, 2, 1024])
            f.load(g2f[:, 0, :], self.rw_g2[l, 0:128, :])
            f.load(g2f[0:32, 1, :], self.rw_g2[l, 128:160, :])
            f.copy(g2b[:, 0, :], g2f[:, 0, :])
            f.copy(g2b[0:32, 1, :], g2f[0:32, 1, :])
        P["g2b"] = g2b
        W = self.rw_alloc_work()
        for d in range(2):
          with f.scope():
            P["W0"] = bc("W0p%d" % d, self.rw_w0[l, d, :], 1024)
            P["A0"] = bc("A0p%d" % d, self.rw_a0[l, d, :], 1024)
            lb = f.sb("lb%d" % d, [64, 2, 1024], BF16)
            with f.scope():
                lf = f.sb("lf%d" % d, [64, 2, 1024])
                f.load(lf[:, 0, :], self.rw_w2[l, d, :, :])
                f.load(lf[:, 1, :], self.rw_a2[l, d, :, :])
                f.copy(lb, lf)
            P["w2b"], P["a2b"] = lb[:, 0, :], lb[:, 1, :]
            H32 = [f.sb("H32_%d_%d" % (d, g), [64, 4, 64]) for g in range(4)]
            H16 = [f.sb("H16_%d_%d" % (d, g), [64, 4, 64], BF16) for g in range(4)]
            for g in range(4):
                f.memset(H32[g], 0.0)
                f.memset(H16[g], 0.0, eng="pool")
            ctx_chunks = list(range(nctx_t))
            lat_chunks = list(range(nctx_t, NT))
            order = ctx_chunks + lat_chunks if d == 0 else ctx_chunks[::-1] + lat_chunks[::-1]
            for c in order:
                self.rw_chunk(l, c, d, P, W, H32, H16)


def _rw_alloc_work(self):
    f = self.f
    W = {}
    for n in ("cur", "prev", "nxt"):
        W[n] = f.sb("rw_" + n, [128, RW_COLS])
    W["y"] = f.sb("rw_y", [128, 1024])
    for j, n in enumerate(("kk", "t1", "t2")):
        W[n] = W["prev"][:, j * 1024:(j + 1) * 1024]
    for j, n in enumerate(("ar", "logw", "E")):
        W[n] = W["nxt"][:, j * 1024:(j + 1) * 1024]
    for n in ("Rt", "Kt", "Bt", "At", "V16"):
        W[n] = f.sb("rw_" + n, [128, 1024], BF16)
    for n in ("RT", "KT", "BT", "AT"):
        W[n] = f.sb("rw_" + n, [64, 16, 128], BF16)
    W["s16"] = f.sb("rw_s16", [128, 16])
    W["s16b"] = f.sb("rw_s16b", [128, 16])
    W["tw"] = f.sb("rw_tw", [128, 2, 64])
    W["lT"] = f.sb("rw_lT", [64, 2, 128], BF16)
    W["gC"] = f.sb("rw_gC", [64, 16])
    W["sets"] = []
    for si_ in range(2):
        S = {}
        for n in ("OA", "ON"):
            S[n] = [f.sb("rw_%s%d_%d" % (n, i, si_), [128, 4, 128], BF16) for i in range(5)]
        for n in ("D2s", "DT2s", "E1", "ET1", "E2", "ET2", "E4", "ET4", "Q", "QT", "P1", "P1T", "AkT", "RbT", "RkT"):
            S[n] = f.sb("rw_%s_%d" % (n, si_), [128, 4, 128], BF16)
        for n in ("X", "XT"):
            S[n] = [f.sb("rw_%s%d_%d" % (n, i, si_), [128, 4, 128], BF16) for i in range(2)]
        S["U16"] = [f.sb("rw_U16_%d_%d" % (i, si_), [128, 4, 64], BF16) for i in range(2)]
        W["sets"].append(S)
    W["sg"] = f.sb("rw_sg", [128, 160])
    W["sgT"] = f.sb("rw_sgT", [128, 2, 128], BF16)
    W["yo"] = f.sb("rw_yo", [128, 1024], BF16)
    return W


def _rw_chunk(self, l, c, d, P, W, H32, H16):
    f = self.f
    cst, cstb = self.cst, self.cstb
    pr = self.pad_row(c * 128)
    cur, prev, nxt = W["cur"], W["prev"], W["nxt"]
    f.load(cur, self.PRW[pr:pr + 128, :])
    f.load(prev, self.PRW[pr - 1:pr + 127, :])
    f.load(nxt, self.PRW[pr + 1:pr + 129, :])
    f.tt(prev, prev, nxt, ALU.add)
    f.stt(prev, prev, 0.5, cur, ALU.mult, ALU.subtract)
    f.tt(prev, prev, P["MU"], ALU.mult)
    f.tt(cur, cur, prev, ALU.add)
    s = cur
    r, k, v = s[:, 0:1024], s[:, 1152:2176], s[:, 2176:3200]
    wlo = s[:, 1024 + d * 64:1024 + (d + 1) * 64]
    alo = s[:, 3200 + d * 64:3200 + (d + 1) * 64]
    kk, t1, t2, ar, logw, E = W["kk"], W["t1"], W["t2"], W["ar"], W["logw"], W["E"]
    kd = t1
    s16, s16b = W["s16"], W["s16b"]
    h3 = lambda t: t.re("p (h n) -> p h n", h=16)
    f.tt(kk, k, P["KK"], ALU.mult)
    f.tt(t1, kk, kk, ALU.mult)
    f.reduce(s16, h3(t1), ALU.add)
    f.act(s16, s16, AF.Sqrt)
    f.ts(s16, s16, 1e-12, ALU.max)
    f.recip(s16, s16)
    f.tt(h3(kk), h3(kk), s16.re("p (h o) -> p h o", o=1).bc([128, 16, 64]), ALU.mult)
    tw, lT = W["tw"], W["lT"]
    f.act(tw[:, 0, :], wlo, AF.Tanh)
    f.copy(tw[:, 1, :], alo)
    pb = self.bank()
    for i in range(2):
        f.tr(pb[0:64, i * 128:(i + 1) * 128], tw[:, i, :], self.ident)
    f.copy(lT, pb[0:64, 0:256].re("p (i n) -> p i n", i=2), eng="act")
    for half in range(2):
        pb = self.bank()
        f.mm(pb, lT[:, 0, :], P["w2b"][:, half * 512:(half + 1) * 512])
        f.tt(t1[:, half * 512:(half + 1) * 512], pb, P["W0"][:, half * 512:(half + 1) * 512], ALU.add)
    f.act(t1, t1, AF.Sigmoid)
    f.ts(logw, t1, -0.6065306597126334, ALU.mult)
    for half in range(2):
        pb = self.bank()
        f.mm(pb, lT[:, 1, :], P["a2b"][:, half * 512:(half + 1) * 512])
        f.tt(ar[:, half * 512:(half + 1) * 512], pb, P["A0"][:, half * 512:(half + 1) * 512], ALU.add)
    f.act(ar, ar, AF.Sigmoid)
    f.stt(t2, ar, -1.0, P["KA"], ALU.add, ALU.mult)
    f.stt(kd, t2, 1.0, k, ALU.add, ALU.mult)
    incl = cst[:, 2 if d == 0 else 4, :]
    strict = cst[:, 3 if d == 0 else 5, :]
    Rt, Kt, Bt, At, V16 = W["Rt"], W["Kt"], W["Bt"], W["At"], W["V16"]
    cb = [self.bank(), self.bank()]
    for half in range(2):
        f.mm(cb[half], incl, logw[:, half * 512:(half + 1) * 512])
    for half in range(2):
        hs = slice(half * 512, (half + 1) * 512)
        f.act(E[:, hs], cb[half], AF.Exp)
        f.tt(Rt[:, hs], r[:, hs], E[:, hs], ALU.mult)
    for half in range(2):
        hs = slice(half * 512, (half + 1) * 512)
        f.act(E[:, hs], cb[half], AF.Exp, scale=-1.0)
    f.tt(Kt, kd, E, ALU.mult)
    f.tt(t2, kk, ar, ALU.mult)
    f.tt(Bt, t2, E, ALU.mult)
    cb = [self.bank(), self.bank()]
    for half in range(2):
        f.mm(cb[half], strict, logw[:, half * 512:(half + 1) * 512])
    for half in range(2):
        hs = slice(half * 512, (half + 1) * 512)
        f.act(E[:, hs], cb[half], AF.Exp)
    f.stt(At, kk, -1.0, E, ALU.mult, ALU.mult)
    f.copy(V16, v, eng="pool")
    gC = W["gC"]
    pb = self.bank()
    for h in range(16):
        f.mm(pb[0:64, h:h + 1], logw[:, h * 64:(h + 1) * 64], self.ones[:, 0:1])
    f.act(gC, pb[0:64, 0:16], AF.Exp)
    for src, dst in ((Rt, W["RT"]), (Kt, W["KT"]), (Bt, W["BT"]), (At, W["AT"])):
        for half in range(2):
            pbb = self.bank().bitcast(BF16)
            for hh in range(8):
                h = half * 8 + hh
                f.tr(pbb[0:64, hh * 128:(hh + 1) * 128], src[:, h * 64:(h + 1) * 64], self.identb)
            f.copy(dst[:, half * 8:(half + 1) * 8, :], pbb[0:64, :].re("p (h n) -> p h n", h=8), eng="act")
    RT, KT, BT, AT = W["RT"], W["KT"], W["BT"], W["AT"]
    m_strict_st = cst[:, 3 if d == 0 else 5, :]
    m_strict_ts = cst[:, 5 if d == 0 else 3, :]
    m_incl_st = cst[:, 2 if d == 0 else 4, :]
    y = W["y"]
    bc4 = lambda m: m.re("p (o n) -> p o n", o=1).bc([128, 4, 128])
    b4 = lambda b: b.re("p (h n) -> p h n", h=4)
    def grp(g, Wg):
        heads = [g * 4 + hh for hh in range(4)]
        mA = [cst[:, (8 if d == 0 else 13) + q, :] for q in range(5)]
        mN = [cst[:, (13 if d == 0 else 8) + q, :] for q in range(5)]
        idb4 = bc4(self.identb)

        def mm4(pb, L_, R_, start=True, stop=True):
            for hh in range(4):
                f.mm(pb[:, hh * 128:(hh + 1) * 128], L_[:, hh, :], R_[:, hh, :], start=start, stop=stop)

        def mm4I(pb, L_, R_, R2_):
            for hh in range(4):
                f.mm(pb[:, hh * 128:(hh + 1) * 128], L_[:, hh, :], R_[:, hh, :], start=True, stop=False)
                f.mm(pb[:, hh * 128:(hh + 1) * 128], self.identb, R2_[:, hh, :], start=False, stop=True)

        pa, pn = self.bank(), self.bank()
        for hh, h in enumerate(heads):
            f.mm(pa[:, hh * 128:(hh + 1) * 128], AT[:, h, :], BT[:, h, :])
            f.mm(pn[:, hh * 128:(hh + 1) * 128], BT[:, h, :], AT[:, h, :])
        for lv in range(5):
            f.tt(Wg["OA"][lv], b4(pa), bc4(mA[lv]), ALU.mult)
            f.tt(Wg["ON"][lv], b4(pn), bc4(mN[lv]), ALU.mult)
            yield
        for (dst, L_, R_, msk) in ((Wg["AkT"], KT, AT, m_strict_st), (Wg["RbT"], BT, RT, m_incl_st),
                                   (Wg["RkT"], KT, RT, m_incl_st)):
            pb = self.bank()
            for hh, h in enumerate(heads):
                f.mm(pb[:, hh * 128:(hh + 1) * 128], L_[:, h, :], R_[:, h, :])
            f.tt(dst, b4(pb), bc4(msk), ALU.mult)
            yield
        Dm, DTm = Wg["OA"][0], Wg["ON"][0]
        f.tt(Wg["E1"], Dm, idb4, ALU.add, eng="pool")
        f.tt(Wg["ET1"], DTm, idb4, ALU.add, eng="pool")
        p1, p2 = self.bank(), self.bank()
        mm4(p1, DTm, Dm)
        mm4(p2, Dm, DTm)
        f.copy(Wg["D2s"], b4(p1), eng="act")
        f.copy(Wg["DT2s"], b4(p2), eng="act")
        f.tt(Wg["E2"], b4(p1), idb4, ALU.add)
        f.tt(Wg["ET2"], b4(p2), idb4, ALU.add)
        yield
        p1, p2 = self.bank(), self.bank()
        mm4(p1, Wg["DT2s"], Wg["D2s"])
        mm4(p2, Wg["D2s"], Wg["DT2s"])
        f.tt(Wg["E4"], b4(p1), idb4, ALU.add)
        f.tt(Wg["ET4"], b4(p2), idb4, ALU.add)
        yield
        p1, p2 = self.bank(), self.bank()
        mm4(p1, Wg["ET2"], Wg["E4"])
        mm4(p2, Wg["E2"], Wg["ET4"])
        f.copy(Wg["Q"], b4(p1), eng="act")
        f.copy(Wg["QT"], b4(p2), eng="dve")
        yield
        p1, p2 = self.bank(), self.bank()
        mm4(p1, Wg["ET1"], Wg["Q"])
        mm4(p2, Wg["E1"], Wg["QT"])
        xi = 0
        X, XT = Wg["X"][0], Wg["XT"][0]
        f.copy(X, b4(p1), eng="act")
        f.copy(XT, b4(p2), eng="dve")
        yield
        for lv in range(1, 5):
            O, OT = Wg["OA"][lv], Wg["ON"][lv]
            p1 = self.bank()
            mm4(p1, OT, X)
            f.copy(Wg["P1"], b4(p1), eng="act")
            yield
            Xn, XTn = Wg["X"][xi ^ 1], Wg["XT"][xi ^ 1]
            if lv < 4:
                p2 = self.bank()
                mm4(p2, X, OT)
                f.copy(Wg["P1T"], b4(p2), eng="dve")
                yield
                p3 = self.bank()
                mm4I(p3, XT, Wg["P1"], X)
                f.copy(Xn, b4(p3), eng="act")
                p4 = self.bank()
                mm4I(p4, Wg["P1"], XT, XT)
                f.copy(XTn, b4(p4), eng="dve")
                yield
            else:
                p4 = self.bank()
                mm4I(p4, Wg["P1"], XT, XT)
                f.copy(XTn, b4(p4), eng="dve")
                yield
            xi ^= 1
            X, XT = Xn, XTn
        pb = self.bank()
        for hh, h in enumerate(heads):
            f.mm(pb[:, hh * 64:(hh + 1) * 64], AT[:, h, :], H16[g][:, hh, :], start=True, stop=False)
            f.mm(pb[:, hh * 64:(hh + 1) * 64], Wg["AkT"][:, hh, :], V16[:, h * 64:(h + 1) * 64], start=False, stop=True)
        f.copy(Wg["U16"][0], pb[:, 0:256].re("p (h n) -> p h n", h=4), eng="act")
        yield
        pb = self.bank()
        for hh in range(4):
            f.mm(pb[:, hh * 64:(hh + 1) * 64], XT[:, hh, :], Wg["U16"][0][:, hh, :])
        U = Wg["U16"][1]
        f.copy(U, pb[:, 0:256].re("p (h n) -> p h n", h=4), eng="act")
        yield
        pb = self.bank()
        for hh, h in enumerate(heads):
            f.mm(pb[:, hh * 64:(hh + 1) * 64], RT[:, h, :], H16[g][:, hh, :], start=True, stop=False)
            f.mm(pb[:, hh * 64:(hh + 1) * 64], Wg["RbT"][:, hh, :], U[:, hh, :], start=False, stop=False)
            f.mm(pb[:, hh * 64:(hh + 1) * 64], Wg["RkT"][:, hh, :], V16[:, h * 64:(h + 1) * 64], start=False, stop=True)
        f.copy(y[:, g * 256:(g + 1) * 256], pb[:, 0:256], eng="dve")
        yield
        pb = self.bank()
        for hh, h in enumerate(heads):
            f.mm(pb[0:64, hh * 64:(hh + 1) * 64], Bt[:, h * 64:(h + 1) * 64], U[:, hh, :], start=True, stop=False)
            f.mm(pb[0:64, hh * 64:(hh + 1) * 64], Kt[:, h * 64:(h + 1) * 64], V16[:, h * 64:(h + 1) * 64], start=False, stop=True)
        f.tt(H32[g], H32[g], pb[0:64, 0:256].re("p (h n) -> p h n", h=4), ALU.add)
        f.tt(H32[g], H32[g], gC[:, g * 4:(g + 1) * 4].re("p (h o) -> p h o", o=1).bc([64, 4, 64]), ALU.mult)
        f.copy(H16[g], H32[g], eng="pool")
    for pair in ((0, 1), (2, 3)):
        gens = [grp(g, W["sets"][g % 2]) for g in pair]
        alive = list(gens)
        while alive:
            for gen_ in list(alive):
                try:
                    next(gen_)
                except StopIteration:
                    alive.remove(gen_)
    if d == 0:
        f.store(self.YF[c * 128:(c + 1) * 128, :], y)
        return
    yf = t1
    f.load(yf, self.YF[c * 128:(c + 1) * 128, :])
    f.tt(y, y, yf, ALU.add)
    f.reduce(s16, h3(y), ALU.add)
    f.ts(s16, s16, 1.0 / 64, ALU.mult)
    f.tt(h3(y), h3(y), s16.re("p (h o) -> p h o", o=1).bc([128, 16, 64]), ALU.subtract)
    f.tt(t2, y, y, ALU.mult)
    f.reduce(s16b, h3(t2), ALU.add)
    f.ts(s16b, s16b, 1.0 / 64, ALU.mult, 64e-5, ALU.add)
    f.act(s16b, s16b, AF.Sqrt)
    f.recip(s16b, s16b)
    f.tt(h3(y), h3(y), s16b.re("p (h o) -> p h o", o=1).bc([128, 16, 64]), ALU.mult)
    f.tt(y, y, P["LNW"], ALU.mult)
    f.tt(y, y, P["LNB"], ALU.add)
    f.tt(t2, r, k, ALU.mult)
    f.tt(t2, t2, P["RK"], ALU.mult)
    f.reduce(s16, h3(t2), ALU.add)
    f.tt(h3(t2), h3(v), s16.re("p (h o) -> p h o", o=1).bc([128, 16, 64]), ALU.mult)
    f.tt(y, y, t2, ALU.add)
    sg, sgT = W["sg"], W["sgT"]
    f.act(sg, s[:, 3328:3488], AF.Sigmoid)
    pb = self.bank()
    f.tr(pb[:, 0:128], sg[:, 0:128], self.ident)
    f.tr(pb[0:32, 128:256], sg[:, 128:160], self.ident)
    f.copy(sgT[:, 0, :], pb[:, 0:128], eng="act")
    f.copy(sgT[0:32, 1, :], pb[0:32, 128:256], eng="act")
    for half in range(2):
        hs = slice(half * 512, (half + 1) * 512)
        pb = self.bank()
        f.mm(pb, sgT[:, 0, :], P["g2b"][:, 0, hs], start=True, stop=False)
        f.mm(pb, sgT[0:32, 1, :], P["g2b"][0:32, 1, hs], start=False, stop=True)
        f.tt(W["yo"][:, hs], y[:, hs], pb, ALU.mult)
    f.store(self.YRW[c * 128:(c + 1) * 128, :], W["yo"])


KB.stage_R = _stage_R
KB.rw_alloc_work = _rw_alloc_work
KB.rw_chunk = _rw_chunk


def _stage_final(self):
    f = self.f
    with f.scope():
        G = f.sb("fin_g", [128, D])
        self.bcast_load(G, self.norm_final_g, D)
        xts = [f.sb("fin_x%d" % i, [128, D]) for i in range(2)]
        sqs = f.sb("fin_sq", [128, D])
        ss = [f.sb("fin_ss%d" % i, [128, 1]) for i in range(2)]
        for i in range(self.n_lat // 128):
            xt, s1 = xts[i % 2], ss[i % 2]
            t0 = self.n_ctx + i * 128
            f.load(xt, self.XS[t0:t0 + 128, :])
            f.act(sqs, xt, AF.Square, accum=s1)
            f.ts(s1, s1, 1.0 / D, ALU.mult, 1e-6, ALU.add)
            f.act(s1, s1, AF.Sqrt)
            f.recip(s1, s1)
            f.stt(xt, xt, s1[:, 0:1], G, ALU.mult, ALU.mult)
            f.store(self.out[i * 128:(i + 1) * 128, :], xt)


KB.stage_final = _stage_final


ZOFF = RW_COLS
XOFF = RW_COLS + 2048
DOFF = RW_COLS + 2048 + 4096


def _ssm_rows(self, i):
    nctx_t = self.n_ctx // 128
    if i < nctx_t:
        return [(0, 128, i * 128, 1)]
    q = i - nctx_t
    rows = self.rows
    cpc = 128 // rows
    return [(ci * rows, rows, self.n_ctx + (q * cpc + ci), GRID_W) for ci in range(cpc)]


def _ssm_dram_rows(self, dram, i, cols=slice(None)):
    out = []
    for (p0, n, r0, stride) in self.ssm_rows(i):
        if stride == 1:
            out.append((slice(p0, p0 + n), dram[r0:r0 + n, cols]))
        else:
            lat = dram[self.n_ctx:self.T, cols].re("(r c) d -> c r d", c=GRID_W)
            out.append((slice(p0, p0 + n), lat[r0 - self.n_ctx]))
    return out


def _stage_S1(self, l):
    f = self.f
    T, NT, n_ctx = self.T, self.NT, self.n_ctx
    if not hasattr(self, "ZS"):
        self.ZS = f.dram("ZS", [T, 2048], BF16)
        self.DTA = f.dram("DTA", [T, 64])
        self.XSM = f.dram("XSM", [T, 2048], BF16)
        self.BTM = f.dram("BTM", [T, 1024], BF16)
        self.BCT = f.dram("BCT", [16, 128, T], BF16)
        self.YSF = f.dram("YSF", [T, 2048])
        self.YSSM = f.dram("YSSM", [T, 2048], BF16)
    groups = []
    nctx_t = n_ctx // 128
    i = 0
    while i < NT:
        lim = nctx_t if i < nctx_t else NT
        n = min(4, lim - i)
        groups.append((i, n))
        i += n
    with f.scope():
        self.alloc_h_tmps()
        mb = self.load_mod_bcast([0, 1])
        hTg = [f.sb("hTg%d" % gi, [128, 8, 512], BF16) for gi in range(len(groups))]
        xts = [f.sb("sxt%d" % j, [128, D]) for j in range(2)]
        for gi, (i0, n) in enumerate(groups):
            for j in range(n):
                i = i0 + j
                xt = xts[i % 2]
                for (ps_, ap) in self.ssm_dram_rows(self.XS, i):
                    f.load(xt[ps_, :], ap)
                r = 1 if i < nctx_t else 0
                self.emit_h(xt, mb[(r, 0)], mb[(r, 1)], hTg[gi][:, :, j * 128:(j + 1) * 128])
        zscope = f.scope()
        zscope.__enter__()
        wfs = [f.sb("swf%d" % j, [128, 8, 512]) for j in range(2)]
        wbs = [f.sb("swb%d" % j, [128, 8, 512], BF16) for j in range(2)]
        o16 = [f.sb("so16_%d" % j, [128, 512], BF16) for j in range(2)]
        o32 = [f.sb("so32_%d" % j, [128, 64]) for j in range(2)]
        blocks = [(ZOFF + c0, 512, "z") for c0 in range(0, 2048, 512)] + [(DOFF, 64, "dt")]
        for bi_, (c0, nb, kind) in enumerate(blocks):
            wf, wb = wfs[bi_ % 2], wbs[bi_ % 2]
            f.load(wf[:, :, 0:nb], self.w_in[l, :, c0:c0 + nb].re("(k p) n -> p k n", p=128))
            f.copy(wb[:, :, 0:nb], wf[:, :, 0:nb], eng="pool")
            for gi, (i0, n) in enumerate(groups):
                for j in range(n):
                    i = i0 + j
                    pb = self.bank()
                    for k in range(8):
                        f.mm(pb[:, 0:nb], hTg[gi][:, k, j * 128:(j + 1) * 128], wb[:, k, 0:nb], start=(k == 0), stop=(k == 7))
                    if kind == "z":
                        o = o16[i % 2]
                        f.act(o, pb, AF.Silu)
                        f.store(self.ZS[i * 128:(i + 1) * 128, c0 - ZOFF:c0 - ZOFF + 512], o)
                    else:
                        o = o32[i % 2]
                        f.copy(o, pb[:, 0:64], eng="act")
                        f.store(self.DTA[i * 128:(i + 1) * 128, :], o)
        zscope.__exit__(None, None, None)
        Lp = T + 8
        lat_off = n_ctx + 6
        XB = f.sb("XB", [128, Lp])
        CV = f.sb("CV", [128, Lp - 4])
        CVb = f.sb("CVb", [128, Lp - 4], BF16)
        f.memset(XB, 0.0)
        cws = [f.sb("cw%d" % j, [128, 6]) for j in range(2)]
        xwf = [f.sb("xwf%d" % j, [128, 8, 128]) for j in range(2)]
        xwb = [f.sb("xwb%d" % j, [128, 8, 128], BF16) for j in range(2)]
        stg = [f.sb("stg%d" % j, [128, 8, 128], BF16) for j in range(2)]
        si = 0
        for blk in range(32):
            wf, wb, cw = xwf[blk % 2], xwb[blk % 2], cws[blk % 2]
            c0 = XOFF + blk * 128
            f.load(wf, self.w_in[l, :, c0:c0 + 128].re("(k p) n -> p k n", p=128))
            f.copy(wb, wf, eng="pool")
            f.load(cw[:, 0:5], self.ssm_conv_w[l, :, blk * 128:(blk + 1) * 128].re("k c -> c k"), allow_slow_non_contiguous=True)
            f.load(cw[:, 5:6], self.ssm_conv_b[l, blk * 128:(blk + 1) * 128].re("(c o) -> c o", o=1), allow_slow_non_contiguous=True)
            for gi, (i0, n) in enumerate(groups):
                pb = self.bank()
                ng = n * 128
                for k in range(8):
                    f.mm(pb[:, 0:ng], wb[:, k, :], hTg[gi][:, k, 0:ng], start=(k == 0), stop=(k == 7))
                off = (2 if i0 < nctx_t else 6) + i0 * 128
                f.copy(XB[:, off:off + ng], pb[:, 0:ng], eng="act")
            Lc = Lp - 4
            f.ts(CV, XB[:, 0:Lc], cw[:, 0:1], ALU.mult, cw[:, 5:6], ALU.add)
            for kq in range(1, 5):
                f.stt(CV, XB[:, kq:kq + Lc], cw[:, kq:kq + 1], CV, ALU.mult, ALU.add)
            f.act(CVb, CV, AF.Silu)
            def cpos(i):
                return i * 128 if i < nctx_t else i * 128 + 4
            if blk < 24:
                dst = self.XSM if blk < 16 else self.BTM
                cc = (blk if blk < 16 else blk - 16) * 128
                for gi, (i0, n) in enumerate(groups):
                    if gi % 2 == 0:
                        pbb = self.bank().bitcast(BF16)
                        st = stg[si % 2]
                        si += 1
                    slot0 = (gi % 2) * 4
                    for j in range(n):
                        i = i0 + j
                        f.tr(pbb[:, (slot0 + j) * 128:(slot0 + j + 1) * 128], CVb[:, cpos(i):cpos(i) + 128], self.identb)
                    f.copy(st[:, slot0:slot0 + n, :], pbb[:, slot0 * 128:(slot0 + n) * 128].re("p (c n) -> p c n", n=128))
                    f.store(dst[i0 * 128:(i0 + n) * 128, cc:cc + 128].re("(c p) n -> p c n", p=128), st[:, slot0:slot0 + n, :])
            if blk >= 16:
                gidx = blk - 16
                f.store(self.BCT[gidx, :, 0:n_ctx], CVb[:, 0:n_ctx])
                f.store(self.BCT[gidx, :, n_ctx:T], CVb[:, n_ctx + 4:T + 4])


KB.ssm_rows = _ssm_rows
KB.ssm_dram_rows = _ssm_dram_rows
KB.stage_S1 = _stage_S1


def _stage_S2(self, l):
    f = self.f
    T, NT, n_ctx = self.T, self.NT, self.n_ctx
    nctx_t = n_ctx // 128
    cst = self.cst
    with f.scope():
        def bc(name, row_ap, n):
            t = f.sb(name, [128, n])
            self.bcast_load(t, row_ap, n)
            return t
        NW = bc("ssNW", self.ssm_norm_w[l, :], 2048)
        DTB = bc("ssDTB", self.ssm_dt_bias[l, :], 64)
        AL = bc("ssAL", self.ssm_a_log[l, :], 64)
        DS = bc("ssDS", self.ssm_d[l, :], 64)
        Aneg = f.sb("ssA", [128, 64])
        f.act(Aneg, AL, AF.Exp)
        f.ts(Aneg, Aneg, -1.0, ALU.mult)
        Dsk = f.sb("ssDsk", [128, 32])
        f.tt(Dsk, DS[:, 0:32], DS[:, 32:64], ALU.add)
        xs = f.sb("ss_xs", [128, 32, 64], BF16)
        Btm = f.sb("ss_Btm", [128, 1024], BF16)
        BT = f.sb("ss_BT", [128, 8, 128], BF16)
        CT = f.sb("ss_CT", [128, 8, 128], BF16)
        dtr = f.sb("ss_dtr", [128, 64])
        zs = f.sb("ss_zs", [128, 2048], BF16)
        ysf = f.sb("ss_ysf", [128, 2048])
        y = f.sb("ss_y", [128, 2048])
        yo = f.sb("ss_yo", [128, 2048], BF16)
        LT = f.sb("ss_LT", [128, 32, 128])
        xdt = f.sb("ss_xdt", [128, 32, 64], BF16)
        xe = f.sb("ss_xe", [128, 32, 64], BF16)
        dt = f.sb("ss_dt", [128, 32])
        ld = f.sb("ss_ld", [128, 32])
        cumT = f.sb("ss_cumT", [128, 32])
        dte = f.sb("ss_dte", [128, 32])
        cd = f.sb("ss_cd", [128, 32])
        SS = []
        for si_ in range(4):
            SS.append({"seg": f.sb("ss_seg%d" % si_, [128, 4, 128]), "Lm": f.sb("ss_L%d" % si_, [128, 4, 128]),
                       "Ec": f.sb("ss_Ec%d" % si_, [128, 4, 128]), "CBs": f.sb("ss_CBs%d" % si_, [128, 128]),
                       "MT": f.sb("ss_MT%d" % si_, [128, 4, 128], BF16), "CsT": f.sb("ss_CsT%d" % si_, [128, 4, 128], BF16)})
        g8 = f.sb("ss_g8", [128, 8])
        S32 = [f.sb("ss_S32_%d" % g, [128, 4, 64]) for g in range(8)]
        S16 = [f.sb("ss_S16_%d" % g, [128, 4, 64], BF16) for g in range(8)]
        bcl = lambda t: t.re("p (h o) -> p h o", o=1)
        for d in range(2):
            for g in range(8):
                f.memset(S32[g], 0.0)
                f.memset(S16[g], 0.0, eng="pool")
            ctx_chunks = list(range(nctx_t))
            lat_chunks = list(range(nctx_t, NT))
            order = ctx_chunks + lat_chunks if d == 0 else ctx_chunks[::-1] + lat_chunks[::-1]
            incl = cst[:, 2 if d == 0 else 4, :]
            negm = cst[:, 6 if d == 0 else 7, :]
            for i in order:
                u0 = i * 128
                f.load(xs, self.XSM[u0:u0 + 128, :].re("p (h n) -> p h n", h=32))
                f.load(Btm, self.BTM[u0:u0 + 128, :])
                f.load(BT, self.BCT[0:8, :, u0:u0 + 128].re("g n t -> n g t"))
                f.load(CT, self.BCT[8:16, :, u0:u0 + 128].re("g n t -> n g t"))
                f.load(dtr, self.DTA[u0:u0 + 128, :])
                if d == 1:
                    f.load(zs, self.ZS[u0:u0 + 128, :])
                    f.load(ysf, self.YSF[u0:u0 + 128, :])
                f.tt(dt, dtr[:, d * 32:(d + 1) * 32], DTB[:, d * 32:(d + 1) * 32], ALU.add)
                f.act(dt, dt, AF.Exp)
                f.ts(dt, dt, 1.0, ALU.add)
                f.act(dt, dt, AF.Ln)
                f.tt(ld, dt, Aneg[:, d * 32:(d + 1) * 32], ALU.mult)
                pb = self.bank()
                f.mm(pb[:, 0:32], incl, ld)
                f.mm(pb[:, 32:64], self.ones, ld)
                f.copy(cumT, pb[:, 0:32])
                f.tt(dte, pb[:, 32:64], cumT, ALU.subtract)
                f.act(dte, dte, AF.Exp)
                f.act(cd, pb[:, 32:64], AF.Exp)
                f.tt(LT, incl.re("p (o n) -> p o n", o=1).bc([128, 32, 128]), bcl(ld).bc([128, 32, 128]), ALU.mult)
                f.tt(xdt, xs, bcl(dt).bc([128, 32, 64]), ALU.mult)
                f.tt(xe, xdt, bcl(dte).bc([128, 32, 64]), ALU.mult)
                def sgrp(g, TS):
                    seg, Lm, Ec, CBs, MT, CsT = TS["seg"], TS["Lm"], TS["Ec"], TS["CBs"], TS["MT"], TS["CsT"]
                    hs = slice(4 * g, 4 * g + 4)
                    pc = self.bank()
                    f.mm(pc, self.ones, LT[:, hs, :].re("p h n -> p (h n)"))
                    pc4 = pc.re("p (h n) -> p h n", h=4)
                    f.tt(seg, pc4, negm.re("p (o n) -> p o n", o=1).bc([128, 4, 128]), ALU.add)
                    f.tt(seg, seg, bcl(cumT[:, hs]).bc([128, 4, 128]), ALU.subtract)
                    f.act(Lm, seg, AF.Exp)
                    f.act(Ec, pc4, AF.Exp)
                    yield
                    pcb = self.bank()
                    f.mm(pcb[:, 0:128], BT[:, g, :], CT[:, g, :])
                    f.copy(CBs, pcb[:, 0:128], eng="act")
                    f.tt(MT, Lm, CBs.re("p (o n) -> p o n", o=1).bc([128, 4, 128]), ALU.mult)
                    f.tt(CsT, Ec, CT[:, g, :].re("p (o n) -> p o n", o=1).bc([128, 4, 128]), ALU.mult)
                    yield
                    py = self.bank()
                    for hh in range(4):
                        h = 4 * g + hh
                        f.mm(py[:, hh * 64:(hh + 1) * 64], MT[:, hh, :], xdt[:, h, :], start=True, stop=False)
                        f.mm(py[:, hh * 64:(hh + 1) * 64], CsT[:, hh, :], S16[g][:, hh, :], start=False, stop=True)
                    if d == 0:
                        f.copy(y[:, g * 256:(g + 1) * 256], py[:, 0:256], eng="act")
                    else:
                        f.tt(y[:, g * 256:(g + 1) * 256], py[:, 0:256], ysf[:, g * 256:(g + 1) * 256], ALU.add)
                    pst = self.bank()
                    yield
                    f.mm(pst[:, 0:256], Btm[:, g * 128:(g + 1) * 128], xe[:, hs, :].re("p h n -> p (h n)"))
                    f.tt(S32[g], S32[g], bcl(cd[:, hs]).bc([128, 4, 64]), ALU.mult)
                    f.tt(S32[g], S32[g], pst[:, 0:256].re("p (h n) -> p h n", h=4), ALU.add)
                    f.copy(S16[g], S32[g], eng="pool")
                for pair in ((0, 1, 2, 3), (4, 5, 6, 7)):
                    alive = [sgrp(g, SS[g % 4]) for g in pair]
                    while alive:
                        for gen_ in list(alive):
                            try:
                                next(gen_)
                            except StopIteration:
                                alive.remove(gen_)
                if d == 0:
                    f.store(self.YSF[u0:u0 + 128, :], y)
                    continue
                y3 = y.re("p (h n) -> p h n", h=32)
                t3 = ysf.re("p (h n) -> p h n", h=32)
                f.tt(t3, xs, bcl(Dsk).bc([128, 32, 64]), ALU.mult)
                f.tt(y, y, ysf, ALU.add)
                f.tt(y, y, zs, ALU.mult)
                f.tt(ysf, y, y, ALU.mult)
                f.reduce(g8, ysf.re("p (g n) -> p g n", g=8), ALU.add)
                f.ts(g8, g8, 1.0 / 256, ALU.mult, 1e-5, ALU.add)
                f.act(g8, g8, AF.Sqrt)
                f.recip(g8, g8)
                f.tt(y.re("p (g n) -> p g n", g=8), y.re("p (g n) -> p g n", g=8), g8.re("p (g o) -> p g o", o=1).bc([128, 8, 256]), ALU.mult)
                f.tt(yo, y, NW, ALU.mult)
                for (ps_, ap) in self.ssm_dram_rows(self.YSSM, i):
                    f.store(ap, yo[ps_, :])


KB.stage_S2 = _stage_S2


def _load_w_bf16(self, dst, src_ap, nk, ncols, stage_tiles):
    f = self.f
    j = 0
    for k0 in range(0, nk, 8):
        kn = min(8, nk - k0)
        for c0 in range(0, ncols, 512):
            st = stage_tiles[j % 2]
            j += 1
            f.load(st[:, 0:kn, :], src_ap[k0 * 128:(k0 + kn) * 128, c0:c0 + 512].re("(k p) n -> p k n", p=128))
            f.copy(dst[:, k0:k0 + kn, c0:c0 + 512], st[:, 0:kn, :], eng="pool")


def _stage_G(self, l):
    f = self.f
    NT, n_ctx = self.NT, self.n_ctx
    with f.scope():
        Wrw = f.sb("gWrw", [128, 8, 1024], BF16)
        Wss = f.sb("gWss", [128, 16, 1024], BF16)
        Wo = f.sb("gWo", [128, 8, 1024], BF16)
        with f.scope():
            st = [f.sb("gst%d" % j, [128, 8, 512]) for j in range(2)]
            self.load_w_bf16(Wrw, self.w_branch_rw[l], 8, 1024, st)
            self.load_w_bf16(Wss, self.w_branch_ssm[l], 16, 1024, st)
            self.load_w_bf16(Wo, self.w_out[l], 8, 1024, st)
        gate = {}
        for r in range(2):
            t = f.sb("gGate%d" % r, [128, D])
            self.bcast_load(t, self.MODS[r, 2, :], D)
            gate[r] = t
        yin = [f.sb("g_yin%d" % j, [128, 3072], BF16) for j in range(2)]
        gsb = [f.sb("g_gs%d" % j, [128, 2048], BF16) for j in range(2)]
        xts = [f.sb("g_xt%d" % j, [128, D]) for j in range(2)]
        yT = f.sb("g_yT", [128, 24, 128], BF16)
        t1 = f.sb("g_t1", [128, 1024])
        t2 = f.sb("g_t2", [128, 1024])
        mg = f.sb("g_mg", [128, 1024], BF16)
        mT = f.sb("g_mT", [128, 8, 128], BF16)
        for i in range(NT):
            r = 1 if i * 128 < n_ctx else 0
            yi, gs, xt = yin[i % 2], gsb[i % 2], xts[i % 2]
            rs = slice(i * 128, (i + 1) * 128)
            f.load(yi[:, 0:1024], self.YRW[rs, :])
            f.load(yi[:, 1024:3072], self.YSSM[rs, :])
            f.load(gs, self.GS[rs, :])
            f.load(xt, self.XS[rs, :])
            for b3 in range(3):
                pbb = self.bank().bitcast(BF16)
                for j in range(8):
                    kq = b3 * 8 + j
                    f.tr(pbb[:, j * 128:(j + 1) * 128], yi[:, kq * 128:(kq + 1) * 128], self.identb)
                f.copy(yT[:, b3 * 8:(b3 + 1) * 8, :], pbb.re("p (k n) -> p k n", k=8), eng=("act" if b3 % 2 else "dve"))
            for half in range(2):
                hs = slice(half * 512, (half + 1) * 512)
                p1 = self.bank()
                for kq in range(8):
                    f.mm(p1, yT[:, kq, :], Wrw[:, kq, hs], start=(kq == 0), stop=(kq == 7))
                p2 = self.bank()
                for kq in range(16):
                    f.mm(p2, yT[:, 8 + kq, :], Wss[:, kq, hs], start=(kq == 0), stop=(kq == 15))
                f.tt(t1[:, hs], p1, gs[:, hs], ALU.mult)
                f.tt(t2[:, hs], p2, gs[:, 1024 + half * 512:1024 + (half + 1) * 512], ALU.mult)
            f.tt(mg, t1, t2, ALU.add)
            pbb = self.bank().bitcast(BF16)
            for j in range(8):
                f.tr(pbb[:, j * 128:(j + 1) * 128], mg[:, j * 128:(j + 1) * 128], self.identb)
            f.copy(mT, pbb.re("p (k n) -> p k n", k=8), eng="act")
            for half in range(2):
                hs = slice(half * 512, (half + 1) * 512)
                p1 = self.bank()
                for kq in range(8):
                    f.mm(p1, mT[:, kq, :], Wo[:, kq, hs], start=(kq == 0), stop=(kq == 7))
                f.tt(t1[:, hs], p1, gate[r][:, hs], ALU.mult)
            f.tt(xt, xt, t1, ALU.add)
            f.store(self.XS[rs, :], xt)


def _stage_F(self, l, tiles_per_pass=17):
    FDBG = ''
    f = self.f
    NT, n_ctx = self.NT, self.n_ctx
    with f.scope():
        mbg = self.load_mod_bcast([5])
        BR = f.sb("fBR", [128, 32])
        self.bcast_load(BR, self.b_router, 32)
        wr = f.sb("fwr", [128, 8, 32])
        f.load(wr, self.w_router.re("(k p) n -> p k n", p=128))
        sc = f.sb("f_sc", [128, 32])
        bi = f.sb("f_bi", [128, 32])
        m8 = f.sb("f_m8", [128, 4, 8])
        gsx = f.sb("f_gsx", [128, 4])
        gmx = f.sb("f_gmx", [128, 1])
        gm = f.sb("f_gm", [128, 4])
        tq = f.sb("f_tq", [128, 4])
        thr = f.sb("f_thr", [128, 1])
        em = f.sb("f_em", [128, 32])
        den = f.sb("f_den", [128, 1])
        for p0 in range(0, NT, tiles_per_pass):
            tiles = list(range(p0, min(NT, p0 + tiles_per_pass)))
            ng = (len(tiles) + 3) // 4
            with f.scope():
                hTg = [f.sb("f_hT%d" % gi, [128, 8, 512], BF16) for gi in range(ng)]
                acc = [f.sb("f_acc%d" % j, [128, D]) for j in range(len(tiles))]
                gw = [f.sb("f_gw%d" % j, [128, 32]) for j in range(len(tiles))]
                ph = f.scope()
                ph.__enter__()
                mb = self.load_mod_bcast([3, 4])
                self.alloc_h_tmps()
                hf = f.sb("f_hf", [128, 8, 128])
                xts = [f.sb("f_xt%d" % j, [128, D]) for j in range(2)]
                for j, i in enumerate(tiles):
                    xt = xts[j % 2]
                    r = 1 if i * 128 < n_ctx else 0
                    f.load(xt, self.XS[i * 128:(i + 1) * 128, :])
                    self.emit_h(xt, mb[(r, 3)], mb[(r, 4)], hTg[j // 4][:, :, (j % 4) * 128:(j % 4 + 1) * 128], hf_dst=hf)
                    f.memset(acc[j], 0.0, eng="pool")
                    pb = self.bank()
                    for k in range(8):
                        f.mm(pb[:, 0:32], hf[:, k, :], wr[:, k, :], start=(k == 0), stop=(k == 7))
                    f.act(sc, pb[:, 0:32], AF.Sigmoid)
                    f.tt(bi, sc, BR, ALU.add)
                    for g in range(4):
                        f.max8(m8[:, g, :], bi[:, g * 8:(g + 1) * 8])
                    f.tt(gsx, m8[:, :, 0], m8[:, :, 1], ALU.add)
                    f.reduce(gmx, gsx, ALU.max)
                    f.ts(gm, gsx, gmx[:, 0:1], ALU.is_equal)
                    f.tt(tq, gm, m8[:, :, 1], ALU.mult)
                    f.reduce(thr, tq, ALU.add)
                    f.ts(em, bi, thr[:, 0:1], ALU.is_ge)
                    f.tt(em.re("p (g n) -> p g n", g=4), em.re("p (g n) -> p g n", g=4),
                         gm.re("p (g o) -> p g o", o=1).bc([128, 4, 8]), ALU.mult)
                    f.tt(gw[j], sc, em, ALU.mult)
                    f.reduce(den, gw[j], ALU.add)
                    f.recip(den, den)
                    f.ts(gw[j], gw[j], den[:, 0:1], ALU.mult)
                ph.__exit__(None, None, None)
                ph = f.scope()
                ph.__enter__()
                w13f = [f.sb("f_w13f%d" % j, [128, 8, 512]) for j in range(2)]
                w2f = w13f[0].re("p k n -> p (k n)").re("p (k n) -> p k n", k=4)
                w1b = f.sb("f_w1b", [128, 8, 512], BF16)
                w3b = f.sb("f_w3b", [128, 8, 512], BF16)
                w2b = f.sb("f_w2b", [128, 4, 1024], BF16)
                sils = [f.sb("f_sil%d" % j, [128, 512], BF16) for j in range(2)]
                gTs = [f.sb("f_gT%d" % j, [128, 4, 512], BF16) for j in range(2)]
                for e in range(0 if 'noexp' in FDBG else (2 if 'exp2' in FDBG else 32)):
                    f.load(w13f[0], self.exp_w1[l, e].re("(k p) n -> p k n", p=128))
                    f.copy(w1b, w13f[0], eng="pool")
                    f.load(w13f[1], self.exp_w3[l, e].re("(k p) n -> p k n", p=128))
                    f.copy(w3b, w13f[1], eng="pool")
                    f.load(w2f, self.exp_w2[l, e].re("(k p) n -> p k n", p=128))
                    f.copy(w2b, w2f, eng="pool")
                    def up_proj(gi):
                        nt_g = min(4, len(tiles) - gi * 4)
                        ntok = nt_g * 128
                        gTc, silc = gTs[gi % 2], sils[gi % 2]
                        for fc in range(4):
                            fs = slice(fc * 128, (fc + 1) * 128)
                            pa, pb = self.bank(), self.bank()
                            for k in range(8):
                                f.mm(pa[:, 0:ntok], w1b[:, k, fs], hTg[gi][:, k, 0:ntok], start=(k == 0), stop=(k == 7))
                            for k in range(8):
                                f.mm(pb[:, 0:ntok], w3b[:, k, fs], hTg[gi][:, k, 0:ntok], start=(k == 0), stop=(k == 7))
                            f.act(silc[:, 0:ntok], pa[:, 0:ntok], AF.Silu)
                            f.tt(gTc[:, fc, 0:ntok], silc[:, 0:ntok], pb[:, 0:ntok], ALU.mult)

                    def down_proj(gi):
                        nt_g = min(4, len(tiles) - gi * 4)
                        gTc = gTs[gi % 2]
                        for jj in range(nt_g):
                            j = gi * 4 + jj
                            for half in range(2):
                                hs = slice(half * 512, (half + 1) * 512)
                                po = self.bank()
                                for fc in range(4):
                                    f.mm(po, gTc[:, fc, jj * 128:(jj + 1) * 128], w2b[:, fc, hs], start=(fc == 0), stop=(fc == 3))
                                f.stt(acc[j][:, hs], po, gw[j][:, e:e + 1], acc[j][:, hs], ALU.mult, ALU.add)

                    up_proj(0)
                    for gi in range(ng):
                        if gi + 1 < ng:
                            up_proj(gi + 1)
                        down_proj(gi)
                ph.__exit__(None, None, None)
                xts = [f.sb("f_xt%d" % j, [128, D]) for j in range(2)]
                for j, i in enumerate(tiles):
                    xt = xts[j % 2]
                    r = 1 if i * 128 < n_ctx else 0
                    f.load(xt, self.XS[i * 128:(i + 1) * 128, :])
                    f.tt(acc[j], acc[j], mbg[(r, 5)], ALU.mult)
                    f.tt(xt, xt, acc[j], ALU.add)
                    f.store(self.XS[i * 128:(i + 1) * 128, :], xt)


KB.load_w_bf16 = _load_w_bf16
KB.stage_G = _stage_G
KB.stage_F = _stage_F


def build_program(n_ctx, n_lat, depth):
    K = KB(n_ctx, n_lat, depth)
    K.stage_init()
    for l in range(depth):
        K.stage_mod(l)
        K.stage_P(l)
        K.stage_R(l)
        K.stage_S1(l)
        K.stage_S2(l)
        K.stage_G(l)
        K.stage_F(l)
    K.stage_final()
    return K.finish()


def kernel(**inputs):
    from concourse.bass_utils import run_bass_kernel_spmd
    inp = {k: np.asarray(v) for k, v in inputs.items()}
    B, n_lat, _ = inp["x"].shape
    n_ctx = inp["ctx"].shape[1]
    L = inp["w_mod"].shape[0]
    nc = build_program(n_ctx, n_lat, L)
    consts = make_consts()
    n_cores = 8
    in_maps = []
    for core in range(n_cores):
        b = core // 2
        m = {}
        for k, v in inp.items():
            if k in ("x", "ctx"):
                m[k] = np.ascontiguousarray(v[b])
            elif k in ("c", "c_ctx"):
                continue
            else:
                m[k] = np.ascontiguousarray(v)
        m["c2"] = np.ascontiguousarray(np.stack([inp["c"][b], inp["c_ctx"]]))
        m["consts"] = consts
        m["rw_r_k"] = np.ascontiguousarray(inp["rw_r_k"].reshape(L, 1024))
        m["ssm_dt_bias"] = np.ascontiguousarray(inp["ssm_dt_bias"].reshape(L, 64))
        m["ssm_a_log"] = np.ascontiguousarray(inp["ssm_a_log"].reshape(L, 64))
        m["ssm_d"] = np.ascontiguousarray(inp["ssm_d"].reshape(L, 64))
        in_maps.append(m)
    res = run_bass_kernel_spmd(nc, in_maps, core_ids=list(range(n_cores)))
    out = np.stack([np.asarray(res.results[2 * b]["out"]) for b in range(B)], axis=0)
    return out.astype(np.float32)
```
